# Optimizing a Trainium2 kernel written in Bass

```python
import jax, jax.numpy as jnp
from jax import lax
import numpy as np

D_MODEL = 1024
BATCH = 2
SEQ = 16384
DEPTH = 2

EPS = 1e-6
ROPE_THETA = 500000.0
POOL_GROUPS = 4
POOL_WINDOWS = (2, 4, 8, 16)
POOL_GROUP_DIM = D_MODEL // 8
POOL_WIDTH = POOL_GROUPS * POOL_GROUP_DIM
NSA_HEADS = D_MODEL // 128
NSA_KV_GROUPS = 2
NSA_HEAD_DIM = 64
NSA_ROT_DIM = NSA_HEAD_DIM // 4
NSA_WIDTH = NSA_HEADS * NSA_HEAD_DIM
NSA_KV_WIDTH = NSA_KV_GROUPS * NSA_HEAD_DIM
CMP_BLOCK = 32
CMP_STRIDE = 16
SEL_BLOCK = 64
SEL_TOPK = 16
WINDOW = 512
Q_BLOCK = 128
GLA_HEADS = 4
GLA_DK = D_MODEL // 16
GLA_DV = D_MODEL // 8
GLA_GATE_RANK = 16
GLA_TAU = 16.0
GLA_CHUNK = 64
GLA_WIDTH = GLA_HEADS * GLA_DV
N_BRANCHES = 3
BRANCH_WIDTH = D_MODEL // 2
D_FF = 4 * D_MODEL
PLE_DIM = 256
IN_SIZES = (POOL_WIDTH, NSA_WIDTH, 6 * NSA_KV_WIDTH, 3 * NSA_HEADS, GLA_HEADS * GLA_DK, GLA_HEADS * GLA_DK, GLA_WIDTH, GLA_GATE_RANK, GLA_WIDTH, N_BRANCHES * D_MODEL)
IN_WIDTH = sum(IN_SIZES)

kernel_name = "hybrid_pool_nsa_gla_gated_merge"


def rmsnorm(x, g):
    xf = x.astype(jnp.float32)
    y = xf * lax.rsqrt(jnp.mean(xf * xf, axis=-1, keepdims=True) + EPS)
    return (y * g.astype(jnp.float32)).astype(x.dtype)


def partial_rope(x, positions):
    half = NSA_ROT_DIM // 2
    inv_freq = jnp.power(ROPE_THETA, -jnp.arange(half, dtype=jnp.float32) * (2.0 / NSA_ROT_DIM))
    ang = positions.astype(jnp.float32)[..., None] * inv_freq
    cos = jnp.cos(ang)[:, :, None, :]
    sin = jnp.sin(ang)[:, :, None, :]
    xr = x[..., :NSA_ROT_DIM].astype(jnp.float32)
    x1, x2 = xr[..., :half], xr[..., half:]
    rot = jnp.concatenate([x1 * cos - x2 * sin, x2 * cos + x1 * sin], axis=-1).astype(x.dtype)
    return jnp.concatenate([rot, x[..., NSA_ROT_DIM:]], axis=-1)


def masked_softmax(s, mask):
    s = jnp.where(mask, s.astype(jnp.float32), -jnp.inf)
    m = jnp.max(s, axis=-1, keepdims=True)
    m = jnp.where(jnp.isfinite(m), m, 0.0)
    e = jnp.exp(s - m)
    return e / jnp.maximum(jnp.sum(e, axis=-1, keepdims=True), 1e-30)


def pool_mixer(u, w_pool, scale):
    B, S, _ = u.shape
    ug = u.reshape(B, S, POOL_GROUPS, POOL_GROUP_DIM).astype(jnp.float32)
    cs = jnp.concatenate([jnp.zeros_like(ug[:, :1]), jnp.cumsum(ug, axis=1)], axis=1)
    t = jnp.arange(S)
    win = jnp.array(POOL_WINDOWS, dtype=jnp.int32)
    start = jnp.maximum(t[:, None] + 1 - win[None, :], 0)
    cnt = (t[:, None] + 1 - start).astype(jnp.float32)
    g_idx = jnp.arange(POOL_GROUPS)[None, :]
    lower = cs[:, start, g_idx, :]
    pooled = (cs[:, 1:] - lower) / cnt[None, :, :, None] - ug
    y = jnp.einsum('bsgc,gcd->bsgd', pooled, w_pool.astype(jnp.float32))
    y = y * scale.astype(jnp.float32).reshape(POOL_GROUPS, POOL_GROUP_DIM)
    return y.reshape(B, S, POOL_WIDTH).astype(u.dtype)


def nsa_mixer(q, kv, gate_logits, positions, cmp_pos_k, cmp_w_k, cmp_pos_v, cmp_w_v):
    B, S, _ = q.shape
    H, G, Dh = NSA_HEADS, NSA_KV_GROUPS, NSA_HEAD_DIM
    HG = H // G
    dt = q.dtype
    q = partial_rope(q.reshape(B, S, H, Dh), positions) * (Dh ** -0.5)
    kv = kv.reshape(B, S, 6, G, Dh)
    kc = partial_rope(kv[:, :, 0], positions)
    vc = kv[:, :, 1]
    ks = partial_rope(kv[:, :, 2], positions)
    vs = kv[:, :, 3]
    kw = partial_rope(kv[:, :, 4], positions)
    vw = kv[:, :, 5]

    n_cmp = (S - CMP_BLOCK) // CMP_STRIDE + 1
    blk_start = jnp.arange(n_cmp) * CMP_STRIDE
    idx = blk_start[:, None] + jnp.arange(CMP_BLOCK)[None, :]

    def compress(t, pos, w):
        blk = t[:, idx] + pos[None, None, :, None, :]
        blk = jnp.moveaxis(blk, 3, 2).reshape(B, n_cmp, G, CMP_BLOCK * Dh)
        return jnp.einsum('bngf,fd->bngd', blk, w)

    kc_cmp = compress(kc, cmp_pos_k, cmp_w_k)
    vc_cmp = compress(vc, cmp_pos_v, cmp_w_v)
    cmp_end = blk_start + CMP_BLOCK - 1

    n_sel = S // SEL_BLOCK
    sel_start = jnp.arange(n_sel) * SEL_BLOCK
    overlap = ((blk_start[:, None] < sel_start[None, :] + SEL_BLOCK) & (blk_start[:, None] + CMP_BLOCK > sel_start[None, :])).astype(jnp.float32)
    top_k = min(SEL_TOPK, n_sel)

    ks_g = ks.transpose(0, 2, 1, 3)
    vs_g = vs.transpose(0, 2, 1, 3)
    kw_pad = jnp.pad(kw, ((0, 0), (WINDOW, 0), (0, 0), (0, 0)))
    vw_pad = jnp.pad(vw, ((0, 0), (WINDOW, 0), (0, 0), (0, 0)))
    gates = jax.nn.sigmoid(gate_logits.astype(jnp.float32)).astype(dt).reshape(B, S, H, 3)
    j_sel = jnp.arange(n_sel)
    n_keys = top_k * SEL_BLOCK

    def query_block(qb):
        t0 = qb * Q_BLOCK
        qblk = lax.dynamic_slice_in_dim(q, t0, Q_BLOCK, axis=1).reshape(B, Q_BLOCK, G, HG, Dh)
        tq = t0 + jnp.arange(Q_BLOCK)
        s = jnp.einsum('bqghd,bngd->bghqn', qblk, kc_cmp)
        p_cmp = masked_softmax(s, cmp_end[None, :] <= tq[:, None])
        o_cmp = jnp.einsum('bghqn,bngd->bqghd', p_cmp.astype(dt), vc_cmp)
        imp = jnp.einsum('bghqn,nj->bgqj', p_cmp, overlap)
        cur = tq // SEL_BLOCK
        valid = j_sel[None, :] <= cur[:, None]
        forced = (j_sel[None, :] == 0) | (j_sel[None, :] == cur[:, None]) | (j_sel[None, :] == cur[:, None] - 1)
        score = jnp.where(forced, jnp.inf, jnp.where(valid, imp, -jnp.inf))
        _, sel = lax.top_k(score, top_k)
        tok = (sel[..., None] * SEL_BLOCK + jnp.arange(SEL_BLOCK)).reshape(B, G, Q_BLOCK * n_keys)
        k_sel = jnp.take_along_axis(ks_g, tok[..., None], axis=2).reshape(B, G, Q_BLOCK, n_keys, Dh)
        v_sel = jnp.take_along_axis(vs_g, tok[..., None], axis=2).reshape(B, G, Q_BLOCK, n_keys, Dh)
        tok_r = tok.reshape(B, G, Q_BLOCK, n_keys)
        s = jnp.einsum('bqghd,bgqkd->bghqk', qblk, k_sel)
        p_slc = masked_softmax(s, (tok_r <= tq[:, None])[:, :, None])
        o_slc = jnp.einsum('bghqk,bgqkd->bqghd', p_slc.astype(dt), v_sel)
        kwb = lax.dynamic_slice_in_dim(kw_pad, t0, Q_BLOCK + WINDOW, axis=1)
        vwb = lax.dynamic_slice_in_dim(vw_pad, t0, Q_BLOCK + WINDOW, axis=1)
        kpos = t0 - WINDOW + jnp.arange(Q_BLOCK + WINDOW)
        diff = tq[:, None] - kpos[None, :]
        wmask = (diff >= 0) & (diff < WINDOW) & (kpos[None, :] >= 0)
        s = jnp.einsum('bqghd,bkgd->bghqk', qblk, kwb)
        o_win = jnp.einsum('bghqk,bkgd->bqghd', masked_softmax(s, wmask).astype(dt), vwb)
        g = lax.dynamic_slice_in_dim(gates, t0, Q_BLOCK, axis=1).reshape(B, Q_BLOCK, G, HG, 3)
        o = g[..., 0:1] * o_cmp + g[..., 1:2] * o_slc + g[..., 2:3] * o_win
        return o.reshape(B, Q_BLOCK, H * Dh)

    out = lax.map(query_block, jnp.arange(S // Q_BLOCK))
    return out.transpose(1, 0, 2, 3).reshape(B, S, NSA_WIDTH)


def gla_mixer(q, k, v, gate_low, r, w_gate2, b_gate, norm_g):
    B, S, _ = q.shape
    H, Dk, Dv, C = GLA_HEADS, GLA_DK, GLA_DV, GLA_CHUNK
    nC = S // C
    f32 = jnp.float32
    z = jnp.einsum('bsr,rd->bsd', gate_low, w_gate2) + b_gate
    log_a = (jax.nn.log_sigmoid(z.astype(f32)) / GLA_TAU).reshape(B, nC, C, H, Dk)
    bcum = jnp.cumsum(log_a, axis=2)
    b_last = bcum[:, :, -1]
    qf = q.astype(f32).reshape(B, nC, C, H, Dk) * (Dk ** -0.5)
    kf = k.astype(f32).reshape(B, nC, C, H, Dk)
    vf = v.astype(f32).reshape(B, nC, C, H, Dv)
    q_s = qf * jnp.exp(bcum)
    k_s = kf * jnp.exp(-bcum)
    k_t = kf * jnp.exp(b_last[:, :, None] - bcum)
    causal = jnp.tril(jnp.ones((C, C), dtype=bool))
    A = jnp.where(causal, jnp.einsum('bnihd,bnjhd->bnhij', q_s, k_s), 0.0)
    o_intra = jnp.einsum('bnhij,bnjhv->bnihv', A, vf)

    def step(state, xs):
        qn, kn, vn, bl = xs
        o = jnp.einsum('bihd,bhdv->bihv', qn, state)
        state = state * jnp.exp(bl)[..., None] + jnp.einsum('bjhd,bjhv->bhdv', kn, vn)
        return state, o

    xs = (jnp.moveaxis(q_s, 1, 0), jnp.moveaxis(k_t, 1, 0), jnp.moveaxis(vf, 1, 0), jnp.moveaxis(b_last, 1, 0))
    _, o_inter = lax.scan(step, jnp.zeros((B, H, Dk, Dv), f32), xs)
    o = (o_intra + jnp.moveaxis(o_inter, 0, 1)).reshape(B, S, H, Dv)
    o = rmsnorm(o, norm_g).reshape(B, S, GLA_WIDTH)
    return (o * jax.nn.silu(r.astype(f32))).astype(q.dtype)


def setup_inputs(seed: int = 0) -> dict:
    key = jax.random.key(seed)
    ks = jax.random.split(key, 24)
    f32 = jnp.float32

    def nrm(k, shape, scale):
        return jax.random.normal(k, shape, f32) * scale

    def gain(k, shape):
        return 1.0 + 0.02 * jax.random.normal(k, shape, f32)

    return {
        "x": nrm(ks[0], (BATCH, SEQ, D_MODEL), 1.0),
        "p": nrm(ks[1], (DEPTH, BATCH, SEQ, PLE_DIM), 1.0),
        "positions": jnp.broadcast_to(jnp.arange(SEQ, dtype=jnp.int32), (BATCH, SEQ)),
        "norm_mix": gain(ks[2], (DEPTH, D_MODEL)),
        "w_in": nrm(ks[3], (DEPTH, D_MODEL, IN_WIDTH), D_MODEL ** -0.5),
        "pool_w": nrm(ks[4], (DEPTH, POOL_GROUPS, POOL_GROUP_DIM, POOL_GROUP_DIM), POOL_GROUP_DIM ** -0.5),
        "pool_scale": gain(ks[5], (DEPTH, POOL_WIDTH)),
        "cmp_pos_k": nrm(ks[6], (DEPTH, CMP_BLOCK, NSA_HEAD_DIM), 0.1),
        "cmp_w_k": nrm(ks[7], (DEPTH, CMP_BLOCK * NSA_HEAD_DIM, NSA_HEAD_DIM), (CMP_BLOCK * NSA_HEAD_DIM) ** -0.5),
        "cmp_pos_v": nrm(ks[8], (DEPTH, CMP_BLOCK, NSA_HEAD_DIM), 0.1),
        "cmp_w_v": nrm(ks[9], (DEPTH, CMP_BLOCK * NSA_HEAD_DIM, NSA_HEAD_DIM), (CMP_BLOCK * NSA_HEAD_DIM) ** -0.5),
        "gla_w_gate": nrm(ks[10], (DEPTH, GLA_GATE_RANK, GLA_HEADS * GLA_DK), GLA_GATE_RANK ** -0.5),
        "gla_b_gate": nrm(ks[11], (DEPTH, GLA_HEADS * GLA_DK), 0.1),
        "gla_norm": gain(ks[12], (DEPTH, GLA_DV)),
        "w_branch": nrm(ks[13], (DEPTH, N_BRANCHES, BRANCH_WIDTH, D_MODEL), BRANCH_WIDTH ** -0.5),
        "w_out": nrm(ks[14], (DEPTH, D_MODEL, D_MODEL), D_MODEL ** -0.5),
        "norm_ffn": gain(ks[15], (DEPTH, D_MODEL)),
        "w_ff1": nrm(ks[16], (DEPTH, D_MODEL, D_FF), D_MODEL ** -0.5),
        "w_ff2": nrm(ks[17], (DEPTH, D_FF, D_MODEL), 0.5 * D_FF ** -0.5),
        "norm_ple": gain(ks[18], (DEPTH, D_MODEL)),
        "w_ple_gate": nrm(ks[19], (DEPTH, D_MODEL, D_MODEL), D_MODEL ** -0.5),
        "w_ple_proj": nrm(ks[20], (DEPTH, PLE_DIM, D_MODEL), PLE_DIM ** -0.5),
        "norm_final": gain(ks[21], (D_MODEL,)),
    }


def reference(x, p, positions, norm_mix, w_in, pool_w, pool_scale, cmp_pos_k, cmp_w_k, cmp_pos_v, cmp_w_v, gla_w_gate, gla_b_gate, gla_norm, w_branch, w_out, norm_ffn, w_ff1, w_ff2, norm_ple, w_ple_gate, w_ple_proj, norm_final):
    B, S, D = x.shape
    h = x
    for i in range(DEPTH):
        a = rmsnorm(h, norm_mix[i])
        proj = jnp.einsum('bsd,df->bsf', a, w_in[i])
        parts = []
        off = 0
        for sz in IN_SIZES:
            parts.append(proj[..., off:off + sz])
            off += sz
        u_pool, q_nsa, kv_nsa, g_nsa, q_gla, k_gla, v_gla, a_gla, r_gla, g_merge = parts

        y_a = pool_mixer(u_pool, pool_w[i], pool_scale[i])
        y_b = nsa_mixer(q_nsa, kv_nsa, g_nsa, positions, cmp_pos_k[i], cmp_w_k[i], cmp_pos_v[i], cmp_w_v[i])
        y_c = gla_mixer(q_gla, k_gla, v_gla, a_gla, r_gla, gla_w_gate[i], gla_b_gate[i], gla_norm[i])

        gm = jax.nn.sigmoid(g_merge.astype(jnp.float32)).astype(x.dtype).reshape(B, S, N_BRANCHES, D)
        merged = gm[:, :, 0] * jnp.einsum('bsw,wd->bsd', y_a, w_branch[i, 0])
        merged = merged + gm[:, :, 1] * jnp.einsum('bsw,wd->bsd', y_b, w_branch[i, 1])
        merged = merged + gm[:, :, 2] * jnp.einsum('bsw,wd->bsd', y_c, w_branch[i, 2])
        h = h + jnp.einsum('bsd,de->bse', merged, w_out[i])

        f = rmsnorm(h, norm_ffn[i])
        f = jnp.square(jax.nn.relu(jnp.einsum('bsd,df->bsf', f, w_ff1[i])))
        h = h + jnp.einsum('bsf,fd->bsd', f, w_ff2[i])

        gate = jax.nn.sigmoid(jnp.einsum('bsd,de->bse', rmsnorm(h, norm_ple[i]), w_ple_gate[i]).astype(jnp.float32)).astype(x.dtype)
        h = h + gate * jnp.einsum('bsk,kd->bsd', p[i], w_ple_proj[i])
    return rmsnorm(h, norm_final)
```

```python
import numpy as np
import concourse.bass as bass
import concourse.mybir as mybir
from concourse.bass_utils import run_bass_kernel_spmd
from contextlib import ExitStack

F32 = mybir.dt.float32
BF16 = mybir.dt.bfloat16
I32 = mybir.dt.int32
AF = mybir.ActivationFunctionType
ALU = mybir.AluOpType

D = 1024
DEPTH = 2
NEG = -30000.0
BIG = 1.0e4
EPS = 1e-6
INW = 6440
PI = float(np.pi)
NPC = 2061 + 6 * 65
ENGS = ("pe", "act", "dve", "pool", "sp")


class Op:
    __slots__ = ("eng", "fn", "deps", "stream", "inc", "count", "marked", "idx", "ninst")


class Prog:
    def __init__(self, nc):
        self.nc = nc
        self.g = ExitStack()
        self.sems = []
        self.semcnt = []
        self.ccsems = []
        self.nphase = 0
        self.begin()

    def begin(self):
        self.ops = []
        self.last_w = {}
        self.readers = {}
        self.last_dma = {}
        self.st = ExitStack()
        self.uses_pid = False
        self.pid = None
        self.npool = 0

    def gsb(self, name, shape, dt):
        return self.g.enter_context(self.nc.sbuf_tensor(name, list(shape), dt))

    def sb(self, name, shape, dt):
        return self.st.enter_context(self.nc.sbuf_tensor("%s_p%d" % (name, self.nphase), list(shape), dt))

    def ps(self, name, shape, dt=F32):
        return self.st.enter_context(self.nc.psum_tensor("%s_p%d" % (name, self.nphase), list(shape), dt))

    def _add(self, eng, fn, reads, writes, stream, inc, ninst=1):
        op = Op()
        op.eng, op.fn, op.stream, op.inc = eng, fn, stream, inc
        op.marked = False
        op.count = 0
        op.idx = len(self.ops)
        op.ninst = ninst
        deps = set()
        for r in reads:
            w = self.last_w.get(r)
            if w is not None:
                deps.add(w)
        for w_ in writes:
            w = self.last_w.get(w_)
            if w is not None:
                deps.add(w)
            for ri in self.readers.get(w_, {}).values():
                deps.add(ri)
        op.deps = deps
        for r in reads:
            self.readers.setdefault(r, {})[stream] = op.idx
        for w_ in writes:
            self.last_w[w_] = op.idx
            self.readers[w_] = {}
        self.ops.append(op)
        return op

    def op(self, eng, fn, reads=(), writes=()):
        return self._add(eng, fn, reads, writes, ("eng", eng), 1)

    def dma(self, fns, reads=(), writes=(), key=None, queue="sp", inc=16):
        if not isinstance(fns, (list, tuple)):
            fns = [fns]
        op = self._add(queue, fns, reads, writes, ("dma", key), inc, ninst=len(fns))
        prev = self.last_dma.get(key)
        if prev is not None:
            op.deps.add(prev)
        self.last_dma[key] = op.idx
        op.marked = True
        return op

    def end(self):
        nc = self.nc
        ops = self.ops
        last_eng = {}
        for op in ops:
            for d in op.deps:
                ops[d].marked = True
            if op.stream[0] == "eng":
                last_eng[op.eng] = op
        for op in last_eng.values():
            op.marked = True
        sidx = {}
        counts = {}
        for op in ops:
            if op.marked:
                if op.stream not in sidx:
                    if op.stream[0] == "dma" and str(op.stream[1]).startswith("cc"):
                        self.ccsems.append(self.g.enter_context(nc.semaphore("ccsem%d" % len(self.ccsems))))
                        sidx[op.stream] = -len(self.ccsems)
                        counts[op.stream] = 0
                    else:
                        i = self.npool
                        self.npool += 1
                        sidx[op.stream] = i
                        while len(self.sems) <= i:
                            self.sems.append(self.g.enter_context(nc.semaphore("sem%d" % len(self.sems))))
                            self.semcnt.append(0)
                        counts[op.stream] = self.semcnt[i]
                counts[op.stream] += op.inc * op.ninst
                op.count = counts[op.stream]
        per_eng = {e: [] for e in ENGS}
        for op in ops:
            per_eng[op.eng].append(op)
        sems = {s: (self.sems[i] if i >= 0 else self.ccsems[-i - 1]) for s, i in sidx.items()}
        final = dict(counts)

        def run(eng_name, eng):
            waited = {}
            if eng_name == "sp" and self.uses_pid:
                self.pid = eng.partition_id()
            for op in per_eng[eng_name]:
                need = {}
                for d in op.deps:
                    p = ops[d]
                    if p.stream == ("eng", "pe") and eng_name == "pe" and op.stream[0] == "eng":
                        continue
                    if need.get(p.stream, 0) < p.count:
                        need[p.stream] = p.count
                for s, c in need.items():
                    if waited.get(s, 0) < c:
                        eng.wait_ge(sems[s], c)
                        waited[s] = c
                if op.stream[0] == "dma":
                    for f in op.fn:
                        f(eng).then_inc(sems[op.stream], op.inc)
                else:
                    ins = op.fn(eng)
                    if op.marked:
                        ins.then_inc(sems[op.stream], 1)
            for s, c in final.items():
                if waited.get(s, 0) < c:
                    eng.wait_ge(sems[s], c)

        with nc.Block() as block:
            @block.tensor
            def _(e):
                run("pe", e)

            @block.scalar
            def _(e):
                run("act", e)

            @block.vector
            def _(e):
                run("dve", e)

            @block.gpsimd
            def _(e):
                run("pool", e)

            @block.sync
            def _(e):
                run("sp", e)
        for s, i in sidx.items():
            if i >= 0:
                self.semcnt[i] = final[s]
        self.st.close()
        self.nphase += 1
        self.begin()

    def finish(self):
        self.g.close()


def _o(c, expr):
    return (expr + c) if c else expr


class Ring:
    def __init__(self, P, name, n, shape, dt, psum=False):
        self.n = n
        self.i = 0
        self.t = [(P.ps if psum else P.sb)("%s%d" % (name, j), shape, dt) for j in range(n)]
        self.k = ["%s%d" % (name, j) for j in range(n)]

    def next(self):
        j = self.i % self.n
        self.i += 1
        return self.t[j], self.k[j]


class Pack:
    def __init__(self):
        self.items = []
        self.off = {}
        self.n = 0

    def add(self, name, a):
        a = np.asarray(a, np.float32)
        rows = a.shape[0]
        flat = a.reshape(rows, -1)
        buf = np.zeros((128, flat.shape[1]), np.float32)
        buf[:rows] = flat
        self.off[name] = (self.n, rows, a.shape[1:])
        self.n += flat.shape[1]
        self.items.append(buf)

    def array(self):
        return np.ascontiguousarray(np.concatenate(self.items, axis=1))


def f32_consts(S):
    NSEL = S // 64
    p = Pack()
    j = np.arange(128)[:, None]
    i = np.arange(128)[None, :]
    same = (j // 64) == (i // 64)
    p.add("ident", np.eye(128))
    p.add("ones", np.ones((128, 128)))
    p.add("tri", (same & (j <= i)).astype(np.float32))
    p.add("m3", -(same & (j > i)).astype(np.float32))
    hi = (np.arange(128) >= 64).astype(np.float32)
    x = np.arange(2 * NSEL)[None, :]
    d = x - NSEL - hi[:, None]
    at = np.where(d > 0, -BIG, np.where(d >= -1, BIG, 0.0))
    p.add("at", at)
    half = 8
    invf = np.power(np.float32(500000.0), -np.arange(half, dtype=np.float32) * np.float32(2.0 / 16)).astype(np.float32)
    p.add("invf", np.concatenate([invf, invf])[:, None])
    p.add("sgn", np.concatenate([-np.ones(8), np.ones(8)])[:, None])
    rsw = np.zeros((64, 16), np.float32)
    for ii in range(16):
        rsw[(ii + 8) % 16, ii] = 1.0
    p.add("rsw", rsw)
    selg = np.zeros((32, 6, 65), np.float32)
    for br in range(3):
        for hh in range(2):
            selg[hh * 3 + br, br * 2 + hh, 64] = 1.0
    p.add("selg", selg)
    return p


def bf_consts(S):
    NM = S // 16
    NMT = max(1, NM // 128)
    NSEL = S // 64
    p = Pack()
    j = np.arange(128)[:, None]
    t = np.arange(128)[None, :]
    p.add("identb", np.eye(128))
    p.add("onesb", np.ones((128, 8)))
    c = np.where(j <= t, 0.0, NEG)
    p.add("causal2", np.stack([c, c], 1))
    a = np.where(j > t, 0.0, NEG)
    p.add("antic2", np.stack([a, a], 1))
    fl = np.floor((np.arange(128) - 31) / 16.0)[None, :]
    cm = np.zeros((128, 17, 2, 128), np.float32)
    for o in range(17):
        m = np.where(j - fl <= 8 * o, 0.0, NEG)
        cm[:, o, 0] = m
        cm[:, o, 1] = m
    p.add("cmpmask", cm)
    n = np.arange(NMT * 128)
    ncmp = NM - 1
    bs = n * 16
    ss = np.arange(NSEL) * 64
    ov = ((bs[:, None] < ss[None, :] + 64) & (bs[:, None] + 32 > ss[None, :]) & (n[:, None] < ncmp)).astype(np.float32)
    p.add("ov", ov.reshape(NMT, 128, NSEL).transpose(1, 0, 2))
    return p


def epat_const(S):
    key = np.arange(S)
    r = np.arange(64)[:, None]
    return (((key // 64) % 64)[None, :] == r).astype(np.float32)


VEC_ITEMS = ["gmix", "gffn", "gple", "gfin", "pscale", "gnorm", "posFk", "posGk", "posFv", "posGv"]


def vec_pack(inp, l):
    p = Pack()
    fm = lambda v: np.asarray(v, np.float32).reshape(-1, 128).T
    p.add("gmix", fm(inp["norm_mix"][l]))
    p.add("gffn", fm(inp["norm_ffn"][l]))
    p.add("gple", fm(inp["norm_ple"][l]))
    p.add("gfin", fm(inp["norm_final"]))
    p.add("pscale", fm(inp["pool_scale"][l]))
    p.add("gnorm", np.asarray(inp["gla_norm"][l], np.float32)[:, None])
    p.add("posk", np.asarray(inp["cmp_pos_k"][l], np.float32).T)
    p.add("posv", np.asarray(inp["cmp_pos_v"][l], np.float32).T)
    return p


class Builder:
    def __init__(self, S, depth=DEPTH, debug=False, stop_after=None):
        self.S = S
        self.T = S // 4
        self.depth = depth
        self.debug = debug
        self.stop_after = stop_after
        self.nc = bass.Bass("TRN2", target_bir_lowering=False)
        self.P = Prog(self.nc)
        self.cf_off = f32_consts(S)
        self.cb_off = bf_consts(S)
        self.build()

    def din(self, name, shape, dt=F32):
        return self.nc.dram_tensor(name, list(shape), dt, kind="ExternalInput")

    def scratch(self, name, shape, dt, collective=False):
        if self.debug and not collective:
            return self.nc.dram_tensor(name, list(shape), dt, kind="ExternalOutput")
        return self.nc.dram_tensor(name, list(shape), dt)

    def xf(self, r0, n):
        i = r0 // self.CR
        assert (r0 + n - 1) // self.CR == i
        lo = r0 - i * self.CR
        return self.XFc[i][lo:lo + n, :]

    def gf(self, s, blk):
        r0 = blk * 128
        i = r0 // self.CR
        rows = min(self.CR, 1280 - i * self.CR)
        lo = r0 - i * self.CR
        return self.GFc[i][s * rows + lo:s * rows + lo + 128, :]

    def mm(self, out, lhsT, rhs, start, stop, rd, wr):
        self.P.op("pe", lambda e: e.matmul(out, lhsT=lhsT, rhs=rhs, start=start, stop=stop), rd, wr)

    def tp(self, out, in_, ident, rd, wr):
        self.P.op("pe", lambda e: e.transpose(out=out, in_=in_, identity=ident), rd, wr)

    def act(self, out, in_, func, rd, wr, scale=None, bias=None):
        kw = {}
        if scale is not None:
            kw["scale"] = scale
        if bias is not None:
            kw["bias"] = bias
        self.P.op("act", lambda e: e.activation(out=out, in_=in_, func=func, **kw), rd, wr)

    def tt(self, eng, out, in0, in1, op, rd, wr):
        self.P.op(eng, lambda e: e.tensor_tensor(out=out, in0=in0, in1=in1, op=op), rd, wr)

    def ts(self, eng, out, in0, s1, s2, op0, op1, rd, wr):
        if op1 is None:
            self.P.op(eng, lambda e: e.tensor_scalar(out=out, in0=in0, scalar1=s1, scalar2=None, op0=op0), rd, wr)
        else:
            self.P.op(eng, lambda e: e.tensor_scalar(out=out, in0=in0, scalar1=s1, scalar2=s2, op0=op0, op1=op1), rd, wr)

    def stt(self, out, in0, scalar, in1, op0, op1, rd, wr):
        self.P.op("dve", lambda e: e.scalar_tensor_tensor(out=out, in0=in0, scalar=scalar, in1=in1, op0=op0, op1=op1), rd, wr)

    def cp(self, eng, out, in_, rd, wr):
        if eng == "act":
            self.P.op("act", lambda e: e.copy(out=out, in_=in_), rd, wr)
        else:
            self.P.op(eng, lambda e: e.tensor_copy(out=out, in_=in_), rd, wr)

    def memset(self, eng, ap, val, wr):
        self.P.op(eng, lambda e: e.memset(ap, val), (), wr)

    def ld(self, out, in_, wr, key, queue="sp", rd=()):
        self.P.dma(lambda e: e.dma_start(out=out, in_=in_), rd, wr, key=key, queue=queue)

    def stq(self, out, in_, rd, key, wr=()):
        self.P.dma(lambda e: e.dma_start(out=out, in_=in_), rd, wr, key=key, queue="pool")

    def dyn(self, mk, wr, key, rd=()):
        self.P.uses_pid = True
        self.P.dma(lambda e: mk(e, self.P.pid), rd, wr, key=key, queue="sp")

    def cfv(self, name):
        off, rows, shp = self.cf_off.off[name]
        n = int(np.prod(shp))
        ap = self.cf[0:rows, off:off + n]
        if len(shp) == 2:
            ap = ap.rearrange("p (a b) -> p a b", b=shp[1])
        return ap

    def cbv(self, name):
        off, rows, shp = self.cb_off.off[name]
        n = int(np.prod(shp))
        ap = self.cb[0:rows, off:off + n]
        if len(shp) == 2:
            ap = ap.rearrange("p (a b) -> p a b", b=shp[1])
        elif len(shp) == 3:
            ap = ap.rearrange("p (a b c) -> p a b c", b=shp[1], c=shp[2])
        return ap

    def vv(self, name):
        off, rows, shp = self.vec_off.off[name]
        n = int(np.prod(shp))
        return self.vec[0:rows, off:off + n]

    def build(self):
        nc, P, S, T = self.nc, self.P, self.S, self.T
        depth = self.depth
        self.xT = self.din("xT", [D, T])
        self.pT = self.din("pT", [depth, 256, T])
        self.pos = self.din("pos", [1, T], I32)
        self.w_in = self.din("w_in", [depth, D, INW])
        self.pool_w = self.din("pool_w", [depth, 4, 128, 128])
        self.cmp_w_k = self.din("cmp_w_k", [depth, 2048, 64])
        self.cmp_w_v = self.din("cmp_w_v", [depth, 2048, 64])
        self.wga = self.din("wga", [depth, 32, 256])
        self.w_branch = self.din("w_branch", [depth, 3, 512, D])
        self.w_out = self.din("w_out", [depth, D, D])
        self.w_ff1 = self.din("w_ff1", [depth, D, 4 * D])
        self.w_ff2 = self.din("w_ff2", [depth, 4 * D, D])
        self.w_ple_gate = self.din("w_ple_gate", [depth, D, D])
        self.w_ple_proj = self.din("w_ple_proj", [depth, 256, D])
        self.vec_off = vec_pack_offsets()
        self.vecs = self.din("vecs", [depth, 128, self.vec_off.n])
        self.cf_in = self.din("cf", [128, self.cf_off.n])
        self.cb_in = self.din("cb", [128, self.cb_off.n])
        self.epat_in = self.din("epat", [64, S])
        self.pc_in = self.din("pc", [128, NPC])
        self.pcb_in = self.din("pcb", [128, 192])
        self.out = nc.dram_tensor("outT", [D, T], F32, kind="ExternalOutput")
        sc = self.scratch
        self.hT = sc("hT", [D, T], F32)
        self.uT = sc("uT", [512, T], F32)
        self.rT = sc("rT", [512, T], BF16)
        self.gmT = sc("gmT", [3 * D, T], BF16)
        self.tabs = sc("tabs", [2, 16, T], F32)
        self.CR = max(128, min(1024, (524288 // T) // 128 * 128))
        self.XFc, self.GFc = [], []
        r = 0
        while r < 1280:
            n = min(self.CR, 1280 - r)
            self.XFc.append(sc("X_F%d" % len(self.XFc), [n, T], BF16, True))
            self.GFc.append(sc("G_F%d" % len(self.GFc), [4 * n, T], BF16, True))
            r += n
        self.XTc = [sc("X_T%d" % j, [512, 1024], BF16, True) for j in range(T // 512)]
        self.GTc = [sc("G_T%d" % j, [4 * 512, 1024], BF16, True) for j in range(T // 512)]
        self.X_fg = sc("X_fg", [512, T // 16], F32, True)
        self.X_gn = sc("X_gn", [32, T], F32, True)
        self.X_ge = sc("X_ge", [256, T // 64], F32, True)
        self.X_u = sc("X_u", [512, 16], F32, True)
        self.G_fg = sc("G_fg", [4 * 512, T // 16], F32, True)
        self.G_gn = sc("G_gn", [4 * 32, T], F32, True)
        self.G_ge = sc("G_ge", [4 * 256, T // 64], F32, True)
        self.G_u = sc("G_u", [4 * 512, 16], F32, True)
        self.Yc = [[sc("Y%d_%d" % (bc, sg), [128, T], BF16, True) for sg in range(4)] for bc in range(2)]
        self.GYc = [[sc("G_Y%d_%d" % (bc, sg), [4 * 128, T], BF16, True) for sg in range(4)] for bc in range(2)]
        if self.debug:
            self.dbgY = nc.dram_tensor("dbgY", [8, 4 * 128, T], BF16, kind="ExternalOutput")
            self.dbgXF = nc.dram_tensor("dbgXF", [1280, T], BF16, kind="ExternalOutput")
            self.dbgXT = nc.dram_tensor("dbgXT", [T, 1024], BF16, kind="ExternalOutput")
            self.dbgfg = nc.dram_tensor("dbgfg", [512, T // 16], F32, kind="ExternalOutput")
            self.dbggn = nc.dram_tensor("dbggn", [32, T], F32, kind="ExternalOutput")
            self.dbgge = nc.dram_tensor("dbgge", [256, T // 64], F32, kind="ExternalOutput")
        self.cf = P.gsb("cf_sb", [128, self.cf_off.n], F32)
        self.epsb = P.gsb("epsb", [128, 1], F32)

        self.phase_setup()
        stages = []
        for l in range(depth):
            stages += [("A", l), ("AG1", l), ("NSA", l), ("GLA", l), ("AG2", l), ("C1", l), ("C2", l), ("C3", l)]
        for nm, l in stages:
            getattr(self, "phase_" + nm)(l)
            P.end()
            if self.stop_after == (nm, l):
                break
        P.finish()

    def phase_setup(self):
        P, T = self.P, self.T
        self.ld(self.cf[:], self.cf_in[:, :], ["cf"], "cf")
        self.memset("dve", self.epsb[:], EPS, ["epsb"])
        posi = P.sb("posi", [16, 512], I32)
        ang = P.sb("ang", [16, 512], F32)
        red = P.sb("red", [16, 512], F32)
        xs = P.sb("xs", [16, 512], F32)
        ki = P.sb("ki", [16, 512], I32)
        tb = P.sb("tb", [16, 2, 512], F32)
        invf = self.cfv("invf")
        sgn = self.cfv("sgn")
        for c in range(T // 512):
            sl = slice(c * 512, (c + 1) * 512)
            self.ld(posi[:], self.pos[0:1, sl].partition_broadcast(16), ["posi"], "posi")
            self.cp("dve", ang[:], posi[:], ["posi"], ["ang"])
            self.ts("dve", ang[:], ang[:], invf[:, 0:1], None, ALU.mult, None, ["ang", "cf"], ["ang"])
            C1, C2 = 6.28125, 2 * PI - 6.28125
            for a_, shift in ((0, 0.5 * PI), (1, 0.0)):
                self.ts("dve", xs[:], ang[:], shift, None, ALU.add, None, ["ang"], ["xs"])
                self.ts("dve", red[:], xs[:], 1.0 / (2 * PI), None, ALU.mult, None, ["xs"], ["red"])
                self.cp("dve", ki[:], red[:], ["red"], ["ki"])
                self.cp("dve", red[:], ki[:], ["ki"], ["red"])
                self.stt(xs[:], red[:], -C1, xs[:], ALU.mult, ALU.add, ["red", "xs"], ["xs"])
                self.stt(xs[:], red[:], -C2, xs[:], ALU.mult, ALU.add, ["red", "xs"], ["xs"])
                self.ts("dve", red[:], xs[:], PI, -2 * PI, ALU.is_gt, ALU.mult, ["xs"], ["red"])
                self.tt("dve", xs[:], xs[:], red[:], ALU.add, ["xs", "red"], ["xs"])
                self.ts("dve", red[:], xs[:], -PI, 2 * PI, ALU.is_lt, ALU.mult, ["xs"], ["red"])
                self.tt("dve", xs[:], xs[:], red[:], ALU.add, ["xs", "red"], ["xs"])
                self.act(tb[:, a_, :], xs[:], AF.Sin, ["xs"], ["tb"])
            self.ts("dve", tb[:, 1, :], tb[:, 1, :], sgn[:, 0:1], None, ALU.mult, None, ["tb", "cf"], ["tb"])
            self.stq(self.tabs.ap().rearrange("a p t -> p a t")[:, 0:2, sl], tb[:], ["tb"], "tb_st")
        P.end()

    def load_vec(self, l):
        P = self.P
        self.vec = P.sb("vec", [128, self.vec_off.n], F32)
        self.ld(self.vec[:], self.vecs[l, :, :], ["vec"], "vec")

    def load_pc(self):
        P = self.P
        self.pc = P.sb("pc", [128, NPC], F32)
        self.ld(self.pc[:], self.pc_in[:, :], ["pc"], "pc")
        self.pcb = P.sb("pcb", [128, 192], BF16)
        self.ld(self.pcb[:], self.pcb_in[:, :], ["pcb"], "pcb", queue="pool")
        self.oh_r = [self.pc[:, 2049 + i:2050 + i] for i in range(4)]
        self.oh_g = [self.pc[:, 2053 + i:2054 + i] for i in range(2)]
        self.oh_hp = [self.pc[:, 2055 + i:2056 + i] for i in range(2)]
        self.oh_prev = [self.pc[:, 2057 + i:2058 + i] for i in range(4)]

    def select(self, out, terms, rd, wr):
        (a0, s0) = terms[0]
        n = a0.shape[0]
        fix = lambda s: s if s.shape[0] == n else s[0:n, :]
        self.ts("dve", out, a0, fix(s0), None, ALU.mult, None, rd + ["pc"], wr)
        for a, s in terms[1:]:
            self.stt(out, a, fix(s), out, ALU.mult, ALU.add, rd + ["pc"] + wr, wr)

    def rmsnorm(self, h, hkey, gname, out, okey, sqR, ssps, rs):
        ones = self.cfv("ones")
        g = self.vv(gname)
        for k in range(8):
            sq, sk = sqR.next()
            self.act(sq[:], h[:, k, :], AF.Square, [hkey], [sk])
            self.mm(ssps[:, :], ones[:, :], sq[:], k == 0, k == 7, [sk, "cf"], ["ssps"])
        self.act(rs[:], ssps[:, :], AF.Ln, ["ssps"], ["rs"], scale=1.0 / D, bias=self.epsb[:, 0:1])
        self.act(rs[:], rs[:], AF.Exp, ["rs"], ["rs"], scale=-0.5)
        if out is not None:
            for k in range(8):
                self.stt(out[:, k, :], h[:, k, :], g[:, k:k + 1], rs[:], ALU.mult, ALU.mult, [hkey, "rs", "vec"], [okey])

    def phase_A(self, l):
        P, T = self.P, self.T
        NG = T // 512
        self.load_vec(l)
        Win = P.sb("Win", [128, 8, INW], BF16)
        P.dma([(lambda e, k=k: e.dma_start(out=Win[:, k, :], in_=self.w_in[l, k * 128:(k + 1) * 128, :])) for k in range(8)],
              (), ["Win"], key="Win", queue="pool")
        Wc = [P.sb("Wck", [64, 32, 64], BF16), P.sb("Wcv", [64, 32, 64], BF16)]
        self.ld(Wc[0][:], self.cmp_w_k[l].rearrange("(l d) o -> d l o", d=64), ["Wc0"], "Wc0", queue="pool")
        self.ld(Wc[1][:], self.cmp_w_v[l].rearrange("(l d) o -> d l o", d=64), ["Wc1"], "Wc1", queue="pool")
        wga = P.sb("wga", [32, 256], BF16)
        self.ld(wga[:], self.wga[l, :, :], ["wga"], "wga", queue="pool")
        hsrc = (self.xT if l == 0 else self.hT).ap().rearrange("(k p) t -> p k t", p=128)
        hR = Ring(P, "hA", 1, [128, 8, 512], F32)
        tabR = Ring(P, "tab", 1, [16, 2, 512], F32)
        sqR = Ring(P, "sq", 2, [128, 512], F32)
        rs = P.sb("rs", [128, 512], F32)
        aT = P.sb("aT", [128, 8, 512], BF16)
        qfR = Ring(P, "qf", 2, [64, 512], F32)
        t1 = P.sb("t1", [16, 512], F32)
        t2 = P.sb("t2", [16, 512], F32)
        obR = Ring(P, "ob", 3, [128, 512], BF16)
        kb = P.sb("kb", [64, 512], BF16)
        pb = P.sb("pb", [64, 2, 32], BF16)
        cbias = P.sb("cbias", [64, 4], F32)
        fgs = P.sb("fgs", [64, 2, 32], F32)
        gnb = P.sb("gnb", [24, 512], F32)
        ubR = Ring(P, "ub", 2, [128, 512], F32)
        rbR = Ring(P, "rb", 2, [128, 512], BF16)
        gmR = Ring(P, "gmb", 2, [128, 2, 512], BF16)
        glaug = P.sb("glaug", [32, 512], BF16)
        vswb = P.sb("vswb", [128, 4, 256], BF16)
        ktb = P.sb("ktb", [128, 4, 256], BF16)
        vgb = P.sb("vgb", [128, 4, 512], BF16)
        ez = P.sb("ez", [128, 256], F32)
        la = P.sb("la", [128, 256], F32)
        ekt = P.sb("ekt", [128, 256], F32)
        e1 = P.sb("e1", [128, 2, 512], F32)
        e2 = P.sb("e2", [128, 2, 512], F32)
        ebt = P.sb("ebt", [128, 2, 8], F32)
        qgf = P.sb("qgf", [128, 4, 512], F32)
        ppR = Ring(P, "pp", 2, [128, 512], F32, psum=True)
        ssps = P.ps("ssps", [128, 512])
        pmisc = P.ps("pmisc", [128, 512])
        tm = P.ps("tm", [128, 1024])
        pz = ssps
        lct = P.ps("lct", [128, 2, 512])
        ident = self.cfv("ident")
        tri = self.cfv("tri")
        m3 = self.cfv("m3")
        rsw = self.cfv("rsw")
        self.memset("dve", glaug[:], 1.0, ["glaug"])
        self.cp("dve", pb[:, 0, :], self.vv("posk"), ["vec"], ["pb"])
        self.cp("dve", pb[:, 1, :], self.vv("posv"), ["vec"], ["pb"])
        for kd in range(2):
            for FG in range(2):
                col = 256 + kd * 2 + FG
                for ll in range(16):
                    self.mm(pmisc[0:64, col:col + 1], Wc[kd][:, FG * 16 + ll, :], pb[:, kd, FG * 16 + ll:FG * 16 + ll + 1],
                            ll == 0, ll == 15, ["pb", "Wc%d" % kd], ["pmisc"])
        self.cp("act", cbias[:], pmisc[0:64, 256:260], ["pmisc"], ["cbias"])

        def proj(c0, M):
            ps, pk = ppR.next()
            for k in range(8):
                self.mm(ps[0:M, :], Win[:, k, c0:c0 + M], aT[:, k, :], k == 0, k == 7, ["aT", "Win"], [pk])
            return ps, pk

        for tg in range(NG):
            t0 = tg * 512
            sl = slice(t0, t0 + 512)
            h, hk = hR.next()
            self.ld(h[:], hsrc[:, :, sl], [hk], hk)
            tab, tk = tabR.next()
            self.ld(tab[:], self.tabs.ap().rearrange("a p t -> p a t")[:, :, sl], [tk], tk)
            self.rmsnorm(h, hk, "gmix", aT, "aT", sqR, ssps, rs)
            heads = [(512 + 64 * hh, "q", 64 * hh) for hh in range(8)]
            for g in range(2):
                heads.append((1024 + 0 * 128 + 64 * g, "kc", g))
                heads.append((1024 + 2 * 128 + 64 * g, "k", 512 + 0 * 128 + 64 * g))
                heads.append((1024 + 4 * 128 + 64 * g, "k", 512 + 1 * 128 + 64 * g))
                heads.append((1024 + 1 * 128 + 64 * g, "vc", g))
            for c0, kind, dst in heads:
                ps, pk = proj(c0, 64)
                qf, qk = qfR.next()
                self.cp("act", qf[:], ps[0:64, :], [pk], [qk])
                if kind != "vc":
                    self.mm(pmisc[0:16, :], rsw[:, :], qf[:], True, True, [qk, "cf"], ["pmisc"])
                    self.tt("dve", t1[:], qf[0:16, :], tab[:, 0, :], ALU.mult, [qk, tk], ["t1"])
                    self.tt("dve", t2[:], pmisc[0:16, :], tab[:, 1, :], ALU.mult, ["pmisc", tk], ["t2"])
                    self.tt("dve", qf[0:16, :], t1[:], t2[:], ALU.add, ["t1", "t2"], [qk])
                if kind == "q" or kind == "k":
                    ob, ok = obR.next()
                    self.ts("dve", ob[0:64, :], qf[:], 0.125 if kind == "q" else 1.0, None, ALU.mult, None, [qk], [ok])
                    self.stq(self.xf(dst, 64)[:, sl], ob[0:64, :], [ok], ok + "s")
                else:
                    kd = 0 if kind == "kc" else 1
                    self.cp("dve", kb[:], qf[:], [qk], ["kb"])
                    for FG in range(2):
                        for ll in range(16):
                            self.mm(pmisc[0:64, 64 + FG * 32:64 + FG * 32 + 32], Wc[kd][:, FG * 16 + ll, :], kb[:, ll:512:16],
                                    ll == 0, ll == 15, ["kb", "Wc%d" % kd], ["pmisc"])
                        self.act(fgs[:, FG, :], pmisc[0:64, 64 + FG * 32:64 + FG * 32 + 32], AF.Identity, ["pmisc", "cbias"], ["fgs"],
                                 bias=cbias[:, kd * 2 + FG:kd * 2 + FG + 1])
                    r0 = (kd * 2 + dst) * 128
                    self.stq(self.X_fg[r0:r0 + 128, tg * 32:(tg + 1) * 32].rearrange("(f d) m -> d f m", d=64), fgs[:], ["fgs"], "fgs_s")
            ps, pk = proj(1792, 24)
            self.act(gnb[:], ps[0:24, :], AF.Sigmoid, [pk], ["gnb"])
            self.stq(self.X_gn[0:24, sl], gnb[:], ["gnb"], "gnb_s")
            for c in range(4):
                ps, pk = proj(c * 128, 128)
                ub, ubk = ubR.next()
                self.cp("act", ub[:], ps[:, :], [pk], [ubk])
                self.stq(self.uT[c * 128:(c + 1) * 128, sl], ub[:], [ubk], ubk + "s")
                if tg == NG - 1:
                    self.stq(self.X_u[c * 128:(c + 1) * 128, :], ub[:, 496:512], [ubk], ubk + "s")
            for c in range(4):
                ps, pk = proj(2856 + c * 128, 128)
                rb, rbk = rbR.next()
                self.act(rb[:], ps[:, :], AF.Silu, [pk], [rbk])
                self.stq(self.rT[c * 128:(c + 1) * 128, sl], rb[:], [rbk], rbk + "s")
            for c2 in range(12):
                gmb, gk = gmR.next()
                for c in range(2):
                    ps, pk = proj(3368 + (c2 * 2 + c) * 128, 128)
                    self.act(gmb[:, c, :], ps[:, :], AF.Sigmoid, [pk], [gk])
                self.stq(self.gmT.ap().rearrange("(g p) t -> p g t", p=128)[:, c2 * 2:(c2 + 1) * 2, sl], gmb[:], [gk], gk + "s")
            ps, pk = proj(2840, 16)
            self.cp("act", glaug[0:16, :], ps[0:16, :], [pk], ["glaug"])
            for c in range(4):
                ps, pk = proj(1816 + c * 128, 128)
                self.cp("act", qgf[:, c, :], ps[:, :], [pk], ["qgf"])
            for tt_ in range(4):
                ts_ = slice(tt_ * 128, (tt_ + 1) * 128)
                for c0, n, o in ((1024 + 384, 128, 0), (1024 + 640, 128, 128), (2072, 256, 256), (2328, 512, 512)):
                    for k in range(8):
                        self.mm(tm[:, o:o + n], aT[:, k, ts_], Win[:, k, c0:c0 + n], k == 0, k == 7, ["aT", "Win"], ["tm"])
                self.cp("act", vswb[:, tt_, :], tm[:, 0:256], ["tm"], ["vswb"])
                self.cp("act", vgb[:, tt_, :], tm[:, 512:1024], ["tm"], ["vgb"])
                self.mm(pz[:, 0:256], glaug[:, ts_], wga[:, :], True, True, ["glaug", "wga"], ["ssps"])
                self.act(ez[:], pz[:, 0:256], AF.Exp, ["ssps"], ["ez"], scale=-1.0)
                self.act(la[:], ez[:], AF.Ln, ["ez"], ["la"], bias=1.0)
                self.mm(pz[:, 256:512], m3[:, :], la[:], True, True, ["la", "cf"], ["ssps"])
                self.act(ekt[:], pz[:, 256:512], AF.Exp, ["ssps"], ["ekt"], scale=1.0 / 16)
                self.tt("dve", ktb[:, tt_, :], tm[:, 256:512], ekt[:], ALU.mult, ["tm", "ekt"], ["ktb"])
                for fc in range(2):
                    self.mm(lct[:, fc, ts_], la[:, fc * 128:(fc + 1) * 128], tri[:, :], True, True, ["la", "cf"], ["lct"])
            xt = self.XTc[tg].ap().rearrange("(tt p) c -> p tt c", p=128)
            self.stq(xt[:, :, 0:256], vswb[:], ["vswb"], "vswb_s")
            self.stq(xt[:, :, 256:512], ktb[:], ["ktb"], "ktb_s")
            self.stq(xt[:, :, 512:1024], vgb[:], ["vgb"], "vgb_s")
            self.act(e1[:], lct[:], AF.Exp, ["lct"], ["e1"], scale=-1.0 / 16)
            self.act(e2[:], lct[:], AF.Exp, ["lct"], ["e2"], scale=1.0 / 16)
            for fc in range(2):
                ob, ok = obR.next()
                self.stt(ob[:, :], qgf[:, fc, :], 0.125, e1[:, fc, :], ALU.mult, ALU.mult, ["qgf", "e1"], [ok])
                self.stq(self.xf(768 + fc * 128, 128)[:, sl], ob[:, :], [ok], ok + "s")
                ob, ok = obR.next()
                self.tt("dve", ob[:, :], qgf[:, 2 + fc, :], e2[:, fc, :], ALU.mult, ["qgf", "e2"], [ok])
                self.stq(self.xf(1024 + fc * 128, 128)[:, sl], ob[:, :], [ok], ok + "s")
            self.cp("dve", ebt[:], e1[:, :, 63:512:64], ["e1"], ["ebt"])
            self.stq(self.X_ge.ap().rearrange("(f p) c -> p f c", p=128)[:, :, tg * 8:(tg + 1) * 8], ebt[:], ["ebt"], "ebt_s")

    def phase_AG1(self, l):
        P = self.P
        grp = [[0, 1, 2, 3], [4, 5, 6, 7]]
        pairs = list(zip(self.XFc, self.GFc)) + list(zip(self.XTc, self.GTc)) + [(self.X_fg, self.G_fg), (self.X_gn, self.G_gn),
                 (self.X_ge, self.G_ge), (self.X_u, self.G_u)]
        import os
        only = os.environ.get("KDBG_AG")
        for i, (a, b) in enumerate(pairs):
            if only is not None and str(i) not in only.split(","):
                continue
            P.dma(lambda e, a=a, b=b: e.collective_compute("AllGather", ALU.bypass, replica_groups=grp,
                                                           ins=[a.ap().opt()], outs=[b.ap().opt()]),
                  (), (), key="cc%d" % i, queue="pool", inc=1)
        if self.debug and l == 0 and os.environ.get("KDBG_COPY", "1") == "1":
            cps = [(self.X_fg.ap(), self.dbgfg.ap()), (self.X_gn.ap(), self.dbggn.ap()), (self.X_ge.ap(), self.dbgge.ap())]
            r = 0
            for c_ in self.XFc:
                n = c_.shape[0]
                cps.append((c_.ap(), self.dbgXF[r:r + n, :]))
                r += n
            for j, c_ in enumerate(self.XTc):
                cps.append((c_.ap(), self.dbgXT[j * 512:(j + 1) * 512, :]))
            for i, (a, b) in enumerate(cps):
                P.dma(lambda e, a=a, b=b: e.dma_start(out=b, in_=a), (), (), key="dbgc%d" % i, queue="sp")

    def phase_AG2(self, l):
        P = self.P
        grp = [[0, 1, 2, 3], [4, 5, 6, 7]]
        for bc in range(2):
            for sg in range(4):
                a, b = self.Yc[bc][sg], self.GYc[bc][sg]
                P.dma(lambda e, a=a, b=b: e.collective_compute("AllGather", ALU.bypass, replica_groups=grp,
                                                               ins=[a.ap().opt()], outs=[b.ap().opt()]),
                      (), ["G_Y%d%d" % (bc, sg)], key="ccY%d%d" % (bc, sg), queue="pool", inc=1)
                if self.debug and l == 0:
                    P.dma(lambda e, b=b, bc=bc, sg=sg: e.dma_start(out=self.dbgY[bc * 4 + sg, :, :], in_=b[:, :]), ["G_Y%d%d" % (bc, sg)], (),
                          key="dbgY%d%d" % (bc, sg), queue="sp")

    def phase_NSA(self, l):
        P, S, T = self.P, self.S, self.T
        NKT = S // 128
        NM = S // 16
        NMT = max(1, NM // 128)
        NSEL = S // 64
        NG4 = (NSEL + 63) // 64
        QPS = T // 128
        KsA = P.sb("KsA", [128, S], BF16)
        self.cb = P.sb("cb", [128, self.cb_off.n], BF16)
        self.ld(self.cb[:], self.cb_in[:, :], ["cb"], "cb", queue="pool")
        self.ld(KsA[64:128, :], self.epat_in[:, :], ["KsA"], "KsAe", queue="pool")
        KwT = P.sb("KwT", [64, S], BF16)
        Vs = P.sb("Vs", [128, NKT, 65], BF16)
        Vw = P.sb("Vw", [128, NKT, 65], BF16)
        Vc = P.sb("Vc", [128, NMT, 65], BF16)
        FG = [P.sb("FGs%d" % i, [64, NMT * 128], F32) for i in range(4)]
        KcA = P.sb("KcA", [64, NMT * 128], BF16)
        VcT = P.sb("VcT", [64, NMT * 128], F32)
        identf = self.cfv("ident")
        onesf = self.cfv("ones")
        at = self.cfv("at")
        identb = self.cbv("identb")
        onesb = self.cbv("onesb")
        causal2 = self.cbv("causal2")
        antic2 = self.cbv("antic2")
        cmpmask = self.cbv("cmpmask")
        ov = self.cbv("ov")
        G_fg, G_gn = self.G_fg, self.G_gn

        self.load_pc()
        selP = self.pcb[:, 0:64]
        kkR = Ring(P, "kk", 2, [128, 2, 512], BF16)
        vaR = Ring(P, "va", 2, [128, 4, 256], BF16)
        fgR = Ring(P, "fga", 2, [64, 8, T // 16], F32)
        pS = P.ps("pS", [128, 2, 512])
        pSs = pS
        nsel = 0
        for s in range(4):
            for kc in range(T // 512):
                c0 = s * T + kc * 512
                kk, kkk = kkR.next()
                P.dma([(lambda e, ty=ty, kk=kk, s=s, kc=kc: e.dma_start(out=kk[:, ty, :], in_=self.gf(s, 4 + ty)[:, kc * 512:(kc + 1) * 512])) for ty in range(2)],
                      (), [kkk], key=kkk)
                for ty, (dst, dk) in enumerate(((KsA, "KsA"), (KwT, "KwT"))):
                    self.mm(pSs[0:64, ty, :], selP, kk[:, ty, :], True, True, [kkk, "pcb"], ["pS%d" % ty])
                    self.cp("act" if (nsel % 2 == 0) else "dve", dst[0:64, c0:c0 + 512], pSs[0:64, ty, :], ["pS%d" % ty], [dk])
                    nsel += 1
            for j in range(T // 512):
                va, vak = vaR.next()
                self.ld(va[:], self.GTc[j][s * 512:(s + 1) * 512, 0:256].rearrange("(kt p) c -> p kt c", p=128), [vak], vak)
                k0 = s * QPS + 4 * j
                ks = slice(k0, k0 + 4)
                self.select(Vs[:, ks, 0:64], [(va[:, :, 0:64], self.oh_g[0]), (va[:, :, 64:128], self.oh_g[1])], [vak], ["Vs"])
                self.select(Vw[:, ks, 0:64], [(va[:, :, 128:192], self.oh_g[0]), (va[:, :, 192:256], self.oh_g[1])], [vak], ["Vw"])
            fga, fgk = fgR.next()
            self.ld(fga[:], G_fg[s * 512:(s + 1) * 512, :].rearrange("(a d) m -> d a m", d=64), [fgk], fgk)
            fgv = fga[:].rearrange("p (kd g f) m -> p kd g f m", kd=2, g=2)
            ms = slice(s * (T // 16), (s + 1) * (T // 16))
            for i in range(4):
                kd, fg = i // 2, i % 2
                self.select(FG[i][:, ms], [(fgv[:, kd, 0, fg, :], self.oh_g[0]), (fgv[:, kd, 1, fg, :], self.oh_g[1])], [fgk], ["FG%d" % i])
        self.memset("dve", Vs[:, :, 64:65], 1.0, ["Vs"])
        self.memset("dve", Vw[:, :, 64:65], 1.0, ["Vw"])
        self.memset("dve", Vc[:, :, 64:65], 1.0, ["Vc"])
        self.memset("dve", KcA[:], 0.0, ["KcA"])
        self.memset("dve", VcT[:], 0.0, ["VcT"])
        self.tt("dve", KcA[:, 0:NM - 1], FG[0][:, 0:NM - 1], FG[1][:, 1:NM], ALU.add, ["FG0", "FG1"], ["KcA"])
        self.tt("dve", VcT[:, 0:NM - 1], FG[2][:, 0:NM - 1], FG[3][:, 1:NM], ALU.add, ["FG2", "FG3"], ["VcT"])
        pO1 = P.ps("pO1", [128, 512])
        pO2 = P.ps("pO2", [128, 512])
        pI = P.ps("pI", [128, 1024])
        pTb = P.ps("pTb", [128, 4, 128], BF16)
        pSf = pS[:].rearrange("p a b -> p (a b)")
        for nt in range(NMT):
            self.tp(pO1[:, 0:64], VcT[:, nt * 128:(nt + 1) * 128], identf[0:64, 0:64], ["VcT", "cf"], ["pO1"])
            self.cp("act", Vc[:, nt, 0:64], pO1[:, 0:64], ["pO1"], ["Vc"])
        q4R = Ring(P, "q4", 2, [64, 4, 128], BF16)
        qMR = Ring(P, "qM", 2, [128, NG4, 2, 128], BF16)
        g6R = Ring(P, "g6", 2, [32, 128], F32)
        qaR = Ring(P, "qa", 2, [64, 8, 128], BF16)
        qg = P.sb("qg", [64, 4, 128], BF16)
        pTR = Ring(P, "pT", 3, [128, 512], BF16)
        for t_ in g6R.t:
            self.memset("dve", t_[:], 0.0, [g6R.k[g6R.t.index(t_)]])
        rsm = P.sb("rsm", [128, 4], F32)
        imp = P.sb("imp", [128, NSEL], F32)
        score = P.sb("score", [128, NSEL], F32)
        sc2 = P.sb("sc2", [128, NSEL], F32)
        m8 = P.sb("m8", [128, 8], F32)
        thr = P.sb("thr", [128, 1], F32)
        mpad = P.sb("mpad", [128, 64 + 64 * NG4], BF16)
        self.memset("dve", mpad[:], 0.0, ["mpad"])
        osb = P.sb("osb", [65, 3, 256], F32)
        rr = P.sb("rr", [65, 3, 256], F32)
        srow = P.sb("srow", [65, 3, 256], F32)
        ya = P.sb("ya", [64, 256], F32)
        yb2 = P.sb("yb2", [64, 256], F32)
        ybR = Ring(P, "yb", 2, [64, 256], BF16)
        ocmp = pO1[0:65, 0:256]
        owin = pO1[0:65, 256:512]
        oslc = pO2[0:65, 0:256]
        rsp = pO2[:, 256:260]
        impU = pI[:].rearrange("p (h j) -> p h j", h=4)

        for qb in range(NKT):
            s = qb // QPS
            tl = (qb % QPS) * 128
            q4, q4k = q4R.next()
            qM, qMk = qMR.next()
            g6, g6k = g6R.next()
            qa, qak = qaR.next()
            P.dma([(lambda e, j=j, qa=qa, s=s, tl=tl: e.dma_start(out=qa[:, 2 * j:2 * j + 2, :],
                                                                   in_=self.gf(s, j)[:, tl:tl + 128].rearrange("(h d) t -> d h t", d=64))) for j in range(4)],
                  (), [qak], key=qak)
            self.ld(g6[0:24, :], G_gn[s * 32:s * 32 + 24, tl:tl + 128], [g6k], g6k)
            self.select(qg[:], [(qa[:, 0:4, :], self.oh_g[0][0:64, :]), (qa[:, 4:8, :], self.oh_g[1][0:64, :])], [qak], ["qg"])
            self.select(q4[:, 0:2, :], [(qg[:, 0:2, :], self.oh_hp[0][0:64, :]), (qg[:, 2:4, :], self.oh_hp[1][0:64, :])], ["qg"], [q4k])
            self.select(q4[:, 2:4, :], [(qg[:, 2:4, :], self.oh_hp[0][0:64, :]), (qg[:, 0:2, :], self.oh_hp[1][0:64, :])], ["qg"], [q4k])
            for v in range(NG4):
                self.cp("pool", qM[0:64, v, :, :], q4[:, 0:2, :], [q4k], [qMk])
            q4f = q4[:].rearrange("p h t -> p (h t)")
            ntmax = min((8 * qb + 6) // 128, NMT - 1)
            for nt in range(ntmax + 1):
                off = 8 * qb - 128 * nt
                partial = off <= 128
                sk = "pS%d" % (nt % 2)
                sps = pS[:, nt % 2, :]
                self.mm(sps, KcA[:, nt * 128:(nt + 1) * 128], q4f, True, not partial, ["KcA", q4k], [sk])
                if partial:
                    idx = off // 8
                    for half in range(2):
                        self.mm(sps[:, half * 256:(half + 1) * 256], identb[:, :], cmpmask[:, idx, :, :].rearrange("p a b -> p (a b)"),
                                False, half == 1, ["cb"], [sk])
                pT, pk = pTR.next()
                self.act(pT[:], sps, AF.Exp, [sk], [pk])
                self.mm(ocmp, Vc[:, nt, :], pT[:, 0:256], nt == 0, nt == ntmax, ["Vc", pk], ["pO1"])
                for h in range(4):
                    self.mm(impU[:, h, 0:NSEL], pT[:, h * 128:(h + 1) * 128], ov[:, nt, :], nt == 0, nt == ntmax, [pk, "cb"], ["pI"])
                    self.mm(rsp[:, h:h + 1], pT[:, h * 128:(h + 1) * 128], onesb[:, 0:1], nt == 0, nt == ntmax, [pk, "cb"], ["pO2"])
            self.ts("dve", rsm[:], rsp, 1e-30, None, ALU.max, None, ["pO2"], ["rsm"])
            self.P.op("dve", lambda e: e.reciprocal(out=rsm[:], in_=rsm[:]), ["rsm"], ["rsm"])
            self.ts("dve", imp[:], impU[:, 0, 0:NSEL], rsm[:, 0:1], None, ALU.mult, None, ["pI", "rsm"], ["imp"])
            for h in range(1, 4):
                self.stt(imp[:], impU[:, h, 0:NSEL], rsm[:, h:h + 1], imp[:], ALU.mult, ALU.add, ["pI", "rsm", "imp"], ["imp"])
            self.tt("dve", score[:], imp[:], at[:, NSEL - 2 * qb:2 * NSEL - 2 * qb], ALU.add, ["imp", "cf"], ["score"])
            self.memset("dve", score[:, 0:1], BIG, ["score"])
            self.P.op("dve", lambda e: e.max(out=m8[:], in_=score[:]), ["score"], ["m8"])
            self.P.op("dve", lambda e: e.match_replace(out=sc2[:], in_to_replace=m8[:], in_values=score[:], imm_value=-2 * BIG), ["score", "m8"], ["sc2"])
            self.P.op("dve", lambda e: e.max(out=m8[:], in_=sc2[:]), ["sc2"], ["m8"])
            self.ts("dve", thr[:], m8[:, 7:8], -0.5 * BIG, None, ALU.max, None, ["m8"], ["thr"])
            self.ts("dve", mpad[:, 64:64 + NSEL], score[:], thr[:, 0:1], NEG, ALU.is_lt, ALU.mult, ["score", "thr"], ["mpad"])
            for v in range(NG4):
                self.tp(pTb[:, v, :], mpad[:, 64 * v:64 * v + 128], identb[:, :], ["mpad", "cb"], ["pTb"])
                for hh in range(2):
                    self.cp("dve" if hh == 0 else "act", qM[64:128, v, hh, :], pTb[64:128, v, :], ["pTb"], [qMk])
            for i0 in range(0, qb + 1, 2):
                pair = list(range(i0, min(i0 + 2, qb + 1)))
                slot = (i0 // 2) % 2
                sk = "pS%d" % slot
                for i, kt in enumerate(pair):
                    v = kt // 32
                    o_ = pS[:, slot, i * 256:(i + 1) * 256]
                    self.mm(o_, KsA[:, kt * 128:(kt + 1) * 128], qM[:, v, :, :].rearrange("p h t -> p (h t)"), True, kt != qb, ["KsA", qMk], [sk])
                    if kt == qb:
                        self.mm(o_, identb[:, :], causal2.rearrange("p a b -> p (a b)"), False, True, ["cb"], [sk])
                pT, pk = pTR.next()
                n_ = 256 * len(pair)
                self.act(pT[:, 0:n_], pS[:, slot, 0:n_], AF.Exp, [sk], [pk])
                for i, kt in enumerate(pair):
                    self.mm(oslc, Vs[:, kt, :], pT[:, i * 256:(i + 1) * 256], kt == 0, kt == qb, ["Vs", pk], ["pO2s"])
            kts = list(range(max(0, qb - 4), qb + 1))
            for kt in kts:
                slot = kt % 2
                sk = "pS%d" % slot
                o_ = pS[:, slot, 0:256]
                nmask = (kt == qb) or (kt == qb - 4)
                self.mm(o_, KwT[:, kt * 128:(kt + 1) * 128], qM[0:64, 0, :, :].rearrange("p h t -> p (h t)"), True, not nmask, ["KwT", qMk], [sk])
                if kt == qb:
                    self.mm(o_, identb[:, :], causal2.rearrange("p a b -> p (a b)"), False, True, ["cb"], [sk])
                elif kt == qb - 4:
                    self.mm(o_, identb[:, :], antic2.rearrange("p a b -> p (a b)"), False, True, ["cb"], [sk])
                pT, pk = pTR.next()
                self.act(pT[:, 0:256], o_, AF.Exp, [sk], [pk])
                self.mm(owin, Vw[:, kt, :], pT[:, 0:256], kt == kts[0], kt == kts[-1], ["Vw", pk], ["pO1w"])
            self.cp("act", osb[:, 0, :], ocmp, ["pO1"], ["osb"])
            self.cp("act", osb[:, 1, :], oslc, ["pO2s"], ["osb"])
            self.cp("act", osb[:, 2, :], owin, ["pO1w"], ["osb"])
            self.ts("dve", rr[64:65, :, :], osb[64:65, :, :], 1e-30, None, ALU.max, None, ["osb"], ["rr"])
            self.P.op("dve", lambda e: e.reciprocal(out=rr[64:65, :, :], in_=rr[64:65, :, :]), ["rr"], ["rr"])
            for br in range(3):
                for hh in range(2):
                    c0 = br * 256 + hh * 128
                    self.mm(pI[0:65, c0:c0 + 128], self.pc[0:32, 2061 + (br * 2 + hh) * 65:2061 + (br * 2 + hh + 1) * 65], g6[:, :], True, True, [g6k, "pc", "imp"], ["pI"])
            self.tt("dve", srow[64:65, :, :].rearrange("p a b -> p (a b)"), rr[64:65, :, :].rearrange("p a b -> p (a b)"), pI[64:65, 0:768],
                    ALU.mult, ["rr", "pI"], ["srow"])
            for br in range(3):
                self.mm(pSf[0:64, br * 256:(br + 1) * 256], onesf[64:65, 0:64], srow[64:65, br, :], True, True, ["srow", "cf"], ["pS0", "pS1"])
            self.tt("dve", ya[:], osb[0:64, 0, :], pSf[0:64, 0:256], ALU.mult, ["osb", "pS0", "pS1"], ["ya"])
            self.tt("dve", yb2[:], osb[0:64, 1, :], pSf[0:64, 256:512], ALU.mult, ["osb", "pS0", "pS1"], ["yb2"])
            self.tt("dve", ya[:], ya[:], yb2[:], ALU.add, ["ya", "yb2"], ["ya"])
            self.tt("dve", yb2[:], osb[0:64, 2, :], pSf[0:64, 512:768], ALU.mult, ["osb", "pS0", "pS1"], ["yb2"])
            yb, ybk = ybR.next()
            self.tt("dve", yb[:], ya[:], yb2[:], ALU.add, ["ya", "yb2"], [ybk])
            self.stq(self.Yc[0][s][:, tl:tl + 128].rearrange("(h d) t -> d h t", d=64), yb[:].rearrange("p (h t) -> p h t", h=2), [ybk], ybk + "s")

    def phase_GLA(self, l):
        P, S, T = self.P, self.S, self.T
        NKT = S // 128
        self.load_vec(l)
        G_ge = self.G_ge
        qsT = P.sb("qsT", [64, S], BF16)
        ksT = P.sb("ksT", [64, S], BF16)
        kt_ = P.sb("ktg", [128, NKT, 64], BF16)
        v_ = P.sb("vg", [128, NKT, 128], BF16)
        eb = P.sb("eb", [64, S // 64], F32)
        self.load_pc()
        selA = self.pcb[:, 64:128]
        selB = self.pcb[:, 128:192]
        gqR = Ring(P, "gq", 2, [128, 2, 2, 512], BF16)
        gtR = Ring(P, "gt", 2, [128, 4, 768], BF16)
        eba = P.sb("eba", [64, 16, T // 64], F32)
        paR = Ring(P, "pa", 2, [128, 512], F32, psum=True)
        nsel = 0
        for s in range(4):
            for kc in range(T // 512):
                c0 = s * T + kc * 512
                gq, gqk = gqR.next()
                P.dma([(lambda e, j=j, gq=gq, s=s, kc=kc: e.dma_start(out=gq[:, j // 2, j % 2, :], in_=self.gf(s, 6 + j)[:, kc * 512:(kc + 1) * 512])) for j in range(4)],
                      (), [gqk], key=gqk)
                for qk_, (dst, dk) in enumerate(((qsT, "qsT"), (ksT, "ksT"))):
                    pss_, pssk = paR.next()
                    self.mm(pss_[0:64, :], selA, gq[:, qk_, 0, :], True, False, [gqk, "pcb"], [pssk])
                    self.mm(pss_[0:64, :], selB, gq[:, qk_, 1, :], False, True, [gqk, "pcb"], [pssk])
                    self.cp("act" if (nsel % 2 == 0) else "dve", dst[0:64, c0:c0 + 512], pss_[0:64, :], [pssk], [dk])
                    nsel += 1
            for j in range(T // 512):
                gt, gtk = gtR.next()
                self.ld(gt[:], self.GTc[j][s * 512:(s + 1) * 512, 256:1024].rearrange("(kt p) c -> p kt c", p=128), [gtk], gtk)
                k0 = (s * T + j * 512) // 128
                ks = slice(k0, k0 + 4)
                self.select(kt_[:, ks, :], [(gt[:, :, h * 64:(h + 1) * 64], self.oh_r[h]) for h in range(4)], [gtk], ["ktg"])
                self.select(v_[:, ks, :], [(gt[:, :, 256 + h * 128:256 + (h + 1) * 128], self.oh_r[h]) for h in range(4)], [gtk], ["vg"])
        self.ld(eba[:], G_ge.ap().rearrange("(a d) c -> d a c", d=64), ["eba"], "eba")
        ebv = eba[:].rearrange("p (s h) c -> p s h c", s=4)
        self.select(eb[:].rearrange("p (s c) -> p s c", s=4), [(ebv[:, :, h, :], self.oh_r[h][0:64, :]) for h in range(4)], ["eba"], ["eb"])
        tri = self.cfv("tri")
        onesf = self.cfv("ones")
        gn = self.vv("gnorm")
        St = P.sb("St", [128, 128], F32)
        SbR = Ring(P, "Sb", 3, [128, 128], BF16)
        AtR = Ring(P, "At", 2, [128, 128], BF16)
        osq = P.sb("osq", [128, 512], F32)
        rs = P.sb("rsg", [128, 512], F32)
        obR = Ring(P, "og", 2, [128, 512], BF16)
        poR = Ring(P, "po", 2, [128, 512], F32, psum=True)
        pkR = Ring(P, "pk", 2, [128, 512], F32, psum=True)
        pn = P.ps("pn", [128, 512])
        self.memset("dve", St[:], 0.0, ["St"])
        Sb, Sbk = SbR.next()
        self.memset("dve", Sb[:], 0.0, [Sbk])
        for tg in range(S // 512):
            po, pok = poR.next()
            for tt_ in range(4):
                ti = tg * 4 + tt_
                c0 = ti * 128
                pa, pak = paR.next()
                self.mm(pa[:, 0:128], ksT[:, c0:c0 + 128], qsT[:, c0:c0 + 128], True, True, ["ksT", "qsT"], [pak])
                At, Atk = AtR.next()
                self.tt("dve", At[:], pa[:, 0:128], tri[:, :], ALU.mult, [pak, "cf"], [Atk])
                for c in range(2):
                    pb = 64 * c
                    cc = slice(c0 + pb, c0 + pb + 64)
                    oc = po[:, tt_ * 128 + pb:tt_ * 128 + pb + 64]
                    self.mm(oc, v_[pb:pb + 64, ti, :], At[pb:pb + 64, pb:pb + 64], True, False, ["vg", Atk], [pok])
                    self.mm(oc, Sb[0:64, :], qsT[:, cc], False, True, [Sbk, "qsT"], [pok])
                    pk_, pkk = pkR.next()
                    self.mm(pk_[0:64, 0:128], kt_[pb:pb + 64, ti, :], v_[pb:pb + 64, ti, :], True, True, ["ktg", "vg"], [pkk])
                    ci = 2 * ti + c
                    self.stt(St[0:64, :], St[0:64, :], eb[:, ci:ci + 1], pk_[0:64, 0:128], ALU.mult, ALU.add, ["St", "eb", pkk], ["St"])
                    Sb, Sbk = SbR.next()
                    self.cp("act", Sb[0:64, :], St[0:64, :], ["St"], [Sbk])
            self.act(osq[:], po[:, :], AF.Square, [pok], ["osq"])
            self.mm(pn[:, :], onesf[:, :], osq[:], True, True, ["osq", "cf"], ["pn"])
            self.act(rs[:], pn[:, :], AF.Ln, ["pn"], ["rsg"], scale=1.0 / 128, bias=self.epsb[:, 0:1])
            self.act(rs[:], rs[:], AF.Exp, ["rsg"], ["rsg"], scale=-0.5)
            ob, obk = obR.next()
            self.stt(ob[:], po[:, :], gn[:, 0:1], rs[:], ALU.mult, ALU.mult, [pok, "vec", "rsg"], [obk])
            t0 = tg * 512
            seg = t0 // T
            lc = t0 % T
            self.stq(self.Yc[1][seg][:, lc:lc + 512], ob[:], [obk], obk + "s")

    def phase_C1(self, l):
        P, S, T = self.P, self.S, self.T
        NG = T // 512
        self.load_vec(l)
        Wb = P.sb("Wb", [128, 3, 4, D], BF16)
        for br in range(3):
            self.ld(Wb[:, br, :, :], self.w_branch[l, br].rearrange("(wc p) d -> p wc d", p=128), ["Wb%d" % br], "Wb%d" % br, queue="pool")
        Wo = P.sb("Wo", [128, 8, D], BF16)
        self.ld(Wo[:], self.w_out[l].rearrange("(k p) d -> p k d", p=128), ["Wo"], "Wo", queue="pool")
        Wp = P.sb("Wpool", [128, 4, 128], BF16)
        self.ld(Wp[:], self.pool_w[l].rearrange("g c d -> c g d"), ["Wpool"], "Wpool", queue="pool")
        self.load_pc()
        pc = self.pc
        ic0 = pc[:, 1:1 + 2048].rearrange("p (g t) -> p g t", g=4)
        uh = P.sb("uh", [128, 4, 4, 16], F32)
        ylR = Ring(P, "yl", 2, [128, 4, 4, 512], BF16)
        hsrc = (self.xT if l == 0 else self.hT).ap().rearrange("(k p) t -> p k t", p=128)
        hdst = self.hT.ap().rearrange("(k p) t -> p k t", p=128)
        uview = self.uT.ap().rearrange("(g p) t -> p g t", p=128)
        hR = Ring(P, "hC", 1, [128, 8, 512], F32)
        uR = Ring(P, "uC", 2, [128, 4, 528], F32)
        ybR = Ring(P, "ybC", 2, [128, 4, 512], BF16)
        ycR = Ring(P, "ycC", 2, [128, 4, 512], BF16)
        rR = Ring(P, "rC", 2, [128, 4, 512], BF16)
        gmR = Ring(P, "gmC", 1, [128, 24, 512], BF16)
        sA = P.sb("sA", [128, 528], F32)
        sB = P.sb("sB", [128, 528], F32)
        pl = P.sb("pl", [128, 4, 512], BF16)
        yaT = P.sb("yaT", [128, 4, 512], BF16)
        ycg = P.sb("ycg", [128, 4, 512], BF16)
        m1 = P.sb("m1", [128, 512], F32)
        m2 = P.sb("m2", [128, 512], F32)
        mb = P.sb("mb", [128, 8, 512], BF16)
        ppR = Ring(P, "ppC", 6, [128, 512], F32, psum=True)
        G_u = self.G_u
        wins = (2, 4, 8, 16)
        for tg in range(NG):
            t0 = tg * 512
            sl = slice(t0, t0 + 512)
            h, hk = hR.next()
            self.ld(h[:], hsrc[:, :, sl], [hk], hk)
            u, uk = uR.next()
            self.ld(u[:, :, 16:528], uview[:, :, sl], [uk], uk + "a")
            if tg == 0:
                self.ld(uh[:], G_u.ap().rearrange("(s g p) t -> p s g t", s=4, g=4), ["uh"], "uh")
                self.select(u[:, :, 0:16], [(uh[:, s_, :, :], self.oh_prev[s_]) for s_ in range(4)], ["uh"], [uk])
            else:
                self.ld(u[:, :, 0:16], uview[:, :, t0 - 16:t0], [uk], uk + "b")
            yb, ybk = ybR.next()
            yc, yck = ycR.next()
            for bc, (yt_, ytk) in enumerate(((yb, ybk), (yc, yck))):
                yl, ylk = ylR.next()
                P.dma([(lambda e, sg=sg, yl=yl, bc=bc, sl=sl: e.dma_start(out=yl[:, sg, :, :], in_=self.GYc[bc][sg][:, sl].rearrange("(r p) t -> p r t", p=128))) for sg in range(4)],
                      (), [ylk], key=ylk)
                for r_ in range(4):
                    self.select(yt_[:, r_, :], [(yl[:, sg, r_, :], self.oh_r[sg]) for sg in range(4)], [ylk], [ytk + "_%d" % r_])
            ybks = [ybk + "_%d" % r_ for r_ in range(4)]
            ycks = [yck + "_%d" % r_ for r_ in range(4)]
            rt, rk = rR.next()
            self.ld(rt[:], self.rT.ap().rearrange("(g p) t -> p g t", p=128)[:, :, sl], [rk], rk)
            gm, gmk = gmR.next()
            self.ld(gm[:], self.gmT.ap().rearrange("(g p) t -> p g t", p=128)[:, :, sl], [gmk], gmk)
            for g in range(4):
                cur, curk = u[:, g, :], uk
                bufs = [(sA, "sA"), (sB, "sB")]
                step = 1
                bi = 0
                while step < wins[g]:
                    nb, nbk = bufs[bi % 2]
                    bi += 1
                    self.tt("dve", nb[:, step:528], cur[:, step:528], cur[:, 0:528 - step], ALU.add, [curk], [nbk])
                    cur, curk = nb[:], nbk
                    step *= 2
                if tg == 0:
                    self.tt("dve", m1[:], cur[:, 16:528], ic0[:, g, :], ALU.mult, [curk, "pc"], ["m1"])
                else:
                    self.ts("dve", m1[:], cur[:, 16:528], 1.0 / wins[g], None, ALU.mult, None, [curk], ["m1"])
                self.tt("dve", pl[:, g, :], m1[:], u[:, g, 16:528], ALU.subtract, ["m1", uk], ["pl"])
            for g in range(4):
                ps, pk = ppR.next()
                self.mm(ps[:, :], Wp[:, g, :], pl[:, g, :], True, True, ["Wpool", "pl"], [pk])
                self.ts("dve", yaT[:, g, :], ps[:, :], self.vv("pscale")[:, g:g + 1], None, ALU.mult, None, [pk, "vec"], ["yaT"])
            self.tt("dve", ycg[:], yc[:], rt[:], ALU.mult, ycks + [rk], ["ycg"])
            ys = [(yaT, ["yaT"]), (yb, ybks), (ycg, ["ycg"])]
            for dc in range(8):
                dsl = slice(dc * 128, (dc + 1) * 128)
                pss = []
                for br in range(3):
                    ps, pk = ppR.next()
                    for wc in range(4):
                        self.mm(ps[:, :], Wb[:, br, wc, dsl], ys[br][0][:, wc, :], wc == 0, wc == 3, ["Wb%d" % br] + ys[br][1], [pk])
                    pss.append((ps, pk))
                self.tt("dve", m1[:], pss[0][0][:, :], gm[:, 0 * 8 + dc, :], ALU.mult, [pss[0][1], gmk], ["m1"])
                self.tt("dve", m2[:], pss[1][0][:, :], gm[:, 1 * 8 + dc, :], ALU.mult, [pss[1][1], gmk], ["m2"])
                self.tt("dve", m1[:], m1[:], m2[:], ALU.add, ["m1", "m2"], ["m1"])
                self.tt("dve", m2[:], pss[2][0][:, :], gm[:, 2 * 8 + dc, :], ALU.mult, [pss[2][1], gmk], ["m2"])
                self.tt("dve", mb[:, dc, :], m1[:], m2[:], ALU.add, ["m1", "m2"], ["mb"])
            for dc in range(8):
                dsl = slice(dc * 128, (dc + 1) * 128)
                ps, pk = ppR.next()
                for k in range(8):
                    self.mm(ps[:, :], Wo[:, k, dsl], mb[:, k, :], k == 0, k == 7, ["Wo", "mb"], [pk])
                self.tt("dve", h[:, dc, :], h[:, dc, :], ps[:, :], ALU.add, [hk, pk], [hk])
            self.stq(hdst[:, :, sl], h[:], [hk], hk + "s")

    def phase_C2(self, l):
        P, T = self.P, self.T
        NG = T // 512
        self.load_vec(l)
        W1 = P.sb("W1", [128, 8, 4 * D], BF16)
        W2 = P.sb("W2", [128, 32, D], BF16)
        P.dma([(lambda e, k=k: e.dma_start(out=W1[:, k, :], in_=self.w_ff1[l, k * 128:(k + 1) * 128, :])) for k in range(8)],
              (), ["W1"], key="W1", queue="pool")
        P.dma([(lambda e, k=k: e.dma_start(out=W2[:, k * 8:(k + 1) * 8, :], in_=self.w_ff2[l, k * 1024:(k + 1) * 1024, :].rearrange("(c p) d -> p c d", p=128))) for k in range(4)],
              (), ["W2"], key="W2", queue="pool")
        hv = self.hT.ap().rearrange("(k p) t -> p k t", p=128)
        hR = Ring(P, "hF", 1, [128, 8, 512], F32)
        sqR = Ring(P, "sqF", 2, [128, 512], F32)
        rs = P.sb("rsF", [128, 512], F32)
        fT = P.sb("fT", [128, 8, 512], BF16)
        uT = P.sb("uF", [128, 32, 512], BF16)
        rl = Ring(P, "rl", 2, [128, 512], F32)
        ssps = P.ps("sspsF", [128, 512])
        ppR = Ring(P, "ppF", 4, [128, 512], F32, psum=True)
        for tg in range(NG):
            sl = slice(tg * 512, (tg + 1) * 512)
            h, hk = hR.next()
            self.ld(h[:], hv[:, :, sl], [hk], hk)
            self.rmsnorm(h, hk, "gffn", fT, "fT", sqR, ssps, rs)
            for fc in range(32):
                ps, pk = ppR.next()
                for k in range(8):
                    self.mm(ps[:, :], W1[:, k, fc * 128:(fc + 1) * 128], fT[:, k, :], k == 0, k == 7, ["W1", "fT"], [pk])
                r_, rk = rl.next()
                self.act(r_[:], ps[:, :], AF.Relu, [pk], [rk])
                self.tt("dve" if fc % 2 == 0 else "pool", uT[:, fc, :], r_[:], r_[:], ALU.mult, [rk], ["uF"])
            for dc in range(8):
                ps, pk = ppR.next()
                for fc in range(32):
                    self.mm(ps[:, :], W2[:, fc, dc * 128:(dc + 1) * 128], uT[:, fc, :], fc == 0, fc == 31, ["W2", "uF"], [pk])
                self.tt("dve", h[:, dc, :], h[:, dc, :], ps[:, :], ALU.add, [hk, pk], [hk])
            self.stq(hv[:, :, sl], h[:], [hk], hk + "s")

    def phase_C3(self, l):
        P, T = self.P, self.T
        NG = T // 512
        last = (l == self.depth - 1)
        self.load_vec(l)
        Wg = P.sb("Wg", [128, 8, D], BF16)
        self.ld(Wg[:], self.w_ple_gate[l].rearrange("(k p) d -> p k d", p=128), ["Wg"], "Wg", queue="pool")
        Wq = P.sb("Wq", [128, 2, D], BF16)
        self.ld(Wq[:], self.w_ple_proj[l].rearrange("(k p) d -> p k d", p=128), ["Wq"], "Wq", queue="pool")
        hv = self.hT.ap().rearrange("(k p) t -> p k t", p=128)
        ov = self.out.ap().rearrange("(k p) t -> p k t", p=128)
        pv = self.pT.ap()
        hR = Ring(P, "hP", 2, [128, 8, 512], F32)
        pR = Ring(P, "pP", 2, [128, 2, 512], BF16)
        sqR = Ring(P, "sqP", 2, [128, 512], F32)
        rs = P.sb("rsP", [128, 512], F32)
        nT = P.sb("nT", [128, 8, 512], BF16)
        gt = P.sb("gt", [128, 512], F32)
        m1 = P.sb("m1P", [128, 512], F32)
        oR = Ring(P, "oP", 2, [128, 8, 512], F32)
        ssps = P.ps("sspsP", [128, 512])
        ppR = Ring(P, "ppP", 4, [128, 512], F32, psum=True)
        for tg in range(NG):
            sl = slice(tg * 512, (tg + 1) * 512)
            h, hk = hR.next()
            self.ld(h[:], hv[:, :, sl], [hk], hk)
            pt, ptk = pR.next()
            self.ld(pt[:], pv[l].rearrange("(k p) t -> p k t", p=128)[:, :, sl], [ptk], ptk, queue="pool")
            self.rmsnorm(h, hk, "gple", nT, "nT", sqR, ssps, rs)
            for dc in range(8):
                dsl = slice(dc * 128, (dc + 1) * 128)
                ps, pk = ppR.next()
                for k in range(8):
                    self.mm(ps[:, :], Wg[:, k, dsl], nT[:, k, :], k == 0, k == 7, ["Wg", "nT"], [pk])
                self.act(gt[:], ps[:, :], AF.Sigmoid, [pk], ["gt"])
                ps2, pk2 = ppR.next()
                for k in range(2):
                    self.mm(ps2[:, :], Wq[:, k, dsl], pt[:, k, :], k == 0, k == 1, ["Wq", ptk], [pk2])
                self.tt("dve", m1[:], gt[:], ps2[:, :], ALU.mult, ["gt", pk2], ["m1P"])
                self.tt("dve", h[:, dc, :], h[:, dc, :], m1[:], ALU.add, [hk, "m1P"], [hk])
            if not last:
                self.stq(hv[:, :, sl], h[:], [hk], hk + "s")
            else:
                o, okk = oR.next()
                self.rmsnorm(h, hk, "gfin", None, None, sqR, ssps, rs)
                g = self.vv("gfin")
                for k in range(8):
                    self.stt(o[:, k, :], h[:, k, :], g[:, k:k + 1], rs[:], ALU.mult, ALU.mult, [hk, "rsP" if False else "rs", "vec"], [okk])
                self.stq(ov[:, :, sl], o[:], [okk], okk + "s")


def vec_pack_offsets():
    dummy = {
        "norm_mix": np.zeros((DEPTH, D), np.float32), "norm_ffn": np.zeros((DEPTH, D), np.float32),
        "norm_ple": np.zeros((DEPTH, D), np.float32), "norm_final": np.zeros((D,), np.float32),
        "pool_scale": np.zeros((DEPTH, 512), np.float32), "gla_norm": np.zeros((DEPTH, 128), np.float32),
        "cmp_pos_k": np.zeros((DEPTH, 32, 64), np.float32), "cmp_pos_v": np.zeros((DEPTH, 32, 64), np.float32),
    }
    return vec_pack(dummy, 0)


def make_in_maps(inp, S, depth=DEPTH):
    T = S // 4
    f = lambda a: np.ascontiguousarray(np.asarray(a, np.float32))
    x = np.asarray(inp["x"], np.float32)
    p = np.asarray(inp["p"], np.float32)
    pos = np.asarray(inp["positions"], np.int32)
    cf = f32_consts(S).array()
    cb = bf_consts(S).array()
    epat = epat_const(S)
    vecs = np.stack([vec_pack(inp, l).array() for l in range(depth)], 0)
    wga = np.zeros((depth, 32, 256), np.float32)
    wga[:, 0:16] = np.asarray(inp["gla_w_gate"], np.float32)[:depth]
    wga[:, 16] = np.asarray(inp["gla_b_gate"], np.float32)[:depth]
    shared = {
        "w_in": f(inp["w_in"][:depth]), "pool_w": f(inp["pool_w"][:depth]),
        "cmp_w_k": f(inp["cmp_w_k"][:depth]), "cmp_w_v": f(inp["cmp_w_v"][:depth]), "wga": wga,
        "w_branch": f(inp["w_branch"][:depth]), "w_out": f(inp["w_out"][:depth]),
        "w_ff1": f(inp["w_ff1"][:depth]), "w_ff2": f(inp["w_ff2"][:depth]),
        "w_ple_gate": f(inp["w_ple_gate"][:depth]), "w_ple_proj": f(inp["w_ple_proj"][:depth]),
        "vecs": f(vecs), "cf": cf, "cb": cb, "epat": epat,
    }
    maps = []
    wins = (2, 4, 8, 16)
    for c in range(8):
        b, seg = c // 4, c % 4
        sl = slice(seg * T, (seg + 1) * T)
        g, hp = seg // 2, seg % 2
        pc = np.zeros((128, NPC), np.float32)
        tglob = seg * T + np.arange(512)
        for gi in range(4):
            pc[:, 1 + gi * 512:1 + (gi + 1) * 512] = (1.0 / np.minimum(tglob + 1, wins[gi]))[None, :]
        pc[:, 2049 + seg] = 1.0
        pc[:, 2053 + g] = 1.0
        pc[:, 2055 + hp] = 1.0
        if seg > 0:
            pc[:, 2057 + seg - 1] = 1.0
        selg = np.zeros((32, 6, 65), np.float32)
        for br in range(3):
            for hh in range(2):
                selg[(4 * g + 2 * hp + hh) * 3 + br, br * 2 + hh, 64] = 1.0
        pc[0:32, 2061:2061 + 390] = selg.reshape(32, 390)
        pcb = np.zeros((128, 192), np.float32)
        dd = np.arange(64)
        pcb[g * 64 + dd, dd] = 1.0
        if seg < 2:
            pcb[seg * 64 + dd, 64 + dd] = 1.0
        else:
            pcb[(seg - 2) * 64 + dd, 128 + dd] = 1.0
        m = dict(shared)
        m["xT"] = np.ascontiguousarray(x[b, sl, :].T)
        m["pT"] = np.ascontiguousarray(np.transpose(p[:depth, b, sl, :], (0, 2, 1)))
        m["pos"] = np.ascontiguousarray(pos[b, sl][None, :])
        m["pc"] = pc
        m["pcb"] = pcb
        maps.append(m)
    return maps


_CACHE = {}


def run(inp, S, depth=DEPTH, debug=False, stop_after=None, trace=False):
    key = (S, depth, debug, stop_after)
    if key not in _CACHE:
        _CACHE[key] = Builder(S, depth, debug, stop_after)
    bld = _CACHE[key]
    maps = make_in_maps(inp, S, depth)
    res = run_bass_kernel_spmd(bld.nc, maps, core_ids=list(range(8)), trace=trace) if trace else \
        run_bass_kernel_spmd(bld.nc, maps, core_ids=list(range(8)))
    return res


def kernel(**inputs):
    S = int(np.asarray(inputs["x"]).shape[1])
    T = S // 4
    res = run(inputs, S)
    out = np.zeros((2, S, D), np.float32)
    for c in range(8):
        b, seg = c // 4, c % 4
        out[b, seg * T:(seg + 1) * T, :] = np.asarray(res.results[c]["outT"]).T
    return out
```

```python
import numpy as np
import concourse.bass as bass
import concourse.mybir as mybir
from concourse.bass_utils import run_bass_kernel_spmd
from contextlib import ExitStack

F32 = mybir.dt.float32
BF16 = mybir.dt.bfloat16
I32 = mybir.dt.int32
AF = mybir.ActivationFunctionType
ALU = mybir.AluOpType

D = 1024
DEPTH = 2
NEG = -30000.0
BIG = 1.0e4
EPS = 1e-6
INW = 6440
PI = float(np.pi)
NPC = 2061 + 6 * 65
ENGS = ("pe", "act", "dve", "pool", "sp")


class Op:
    __slots__ = ("eng", "fn", "deps", "stream", "inc", "count", "marked", "idx", "ninst")


class Prog:
    def __init__(self, nc):
        self.nc = nc
        self.g = ExitStack()
        self.sems = []
        self.semcnt = []
        self.ccsems = []
        self.nphase = 0
        self.begin()

    def begin(self):
        self.ops = []
        self.last_w = {}
        self.readers = {}
        self.last_dma = {}
        self.st = ExitStack()
        self.uses_pid = False
        self.pid = None
        self.npool = 0

    def gsb(self, name, shape, dt):
        return self.g.enter_context(self.nc.sbuf_tensor(name, list(shape), dt))

    def sb(self, name, shape, dt):
        return self.st.enter_context(self.nc.sbuf_tensor("%s_p%d" % (name, self.nphase), list(shape), dt))

    def ps(self, name, shape, dt=F32):
        return self.st.enter_context(self.nc.psum_tensor("%s_p%d" % (name, self.nphase), list(shape), dt))

    def _add(self, eng, fn, reads, writes, stream, inc, ninst=1):
        op = Op()
        op.eng, op.fn, op.stream, op.inc = eng, fn, stream, inc
        op.marked = False
        op.count = 0
        op.idx = len(self.ops)
        op.ninst = ninst
        deps = set()
        for r in reads:
            w = self.last_w.get(r)
            if w is not None:
                deps.add(w)
        for w_ in writes:
            w = self.last_w.get(w_)
            if w is not None:
                deps.add(w)
            for ri in self.readers.get(w_, {}).values():
                deps.add(ri)
        op.deps = deps
        for r in reads:
            self.readers.setdefault(r, {})[stream] = op.idx
        for w_ in writes:
            self.last_w[w_] = op.idx
            self.readers[w_] = {}
        self.ops.append(op)
        return op

    def op(self, eng, fn, reads=(), writes=()):
        return self._add(eng, fn, reads, writes, ("eng", eng), 1)

    def dma(self, fns, reads=(), writes=(), key=None, queue="sp", inc=16):
        if not isinstance(fns, (list, tuple)):
            fns = [fns]
        op = self._add(queue, fns, reads, writes, ("dma", key), inc, ninst=len(fns))
        prev = self.last_dma.get(key)
        if prev is not None:
            op.deps.add(prev)
        self.last_dma[key] = op.idx
        op.marked = True
        return op

    def end(self):
        nc = self.nc
        ops = self.ops
        last_eng = {}
        for op in ops:
            for d in op.deps:
                ops[d].marked = True
            if op.stream[0] == "eng":
                last_eng[op.eng] = op
        for op in last_eng.values():
            op.marked = True
        sidx = {}
        counts = {}
        for op in ops:
            if op.marked:
                if op.stream not in sidx:
                    if op.stream[0] == "dma" and str(op.stream[1]).startswith("cc"):
                        self.ccsems.append(self.g.enter_context(nc.semaphore("ccsem%d" % len(self.ccsems))))
                        sidx[op.stream] = -len(self.ccsems)
                        counts[op.stream] = 0
                    else:
                        i = self.npool
                        self.npool += 1
                        sidx[op.stream] = i
                        while len(self.sems) <= i:
                            self.sems.append(self.g.enter_context(nc.semaphore("sem%d" % len(self.sems))))
                            self.semcnt.append(0)
                        counts[op.stream] = self.semcnt[i]
                counts[op.stream] += op.inc * op.ninst
                op.count = counts[op.stream]
        per_eng = {e: [] for e in ENGS}
        for op in ops:
            per_eng[op.eng].append(op)
        sems = {s: (self.sems[i] if i >= 0 else self.ccsems[-i - 1]) for s, i in sidx.items()}
        final = dict(counts)

        def run(eng_name, eng):
            waited = {}
            if eng_name == "sp" and self.uses_pid:
                self.pid = eng.partition_id()
            for op in per_eng[eng_name]:
                need = {}
                for d in op.deps:
                    p = ops[d]
                    if p.stream == ("eng", "pe") and eng_name == "pe" and op.stream[0] == "eng":
                        continue
                    if need.get(p.stream, 0) < p.count:
                        need[p.stream] = p.count
                for s, c in need.items():
                    if waited.get(s, 0) < c:
                        eng.wait_ge(sems[s], c)
                        waited[s] = c
                if op.stream[0] == "dma":
                    for f in op.fn:
                        f(eng).then_inc(sems[op.stream], op.inc)
                else:
                    ins = op.fn(eng)
                    if op.marked:
                        ins.then_inc(sems[op.stream], 1)
            for s, c in final.items():
                if waited.get(s, 0) < c:
                    eng.wait_ge(sems[s], c)

        with nc.Block() as block:
            @block.tensor
            def _(e):
                run("pe", e)

            @block.scalar
            def _(e):
                run("act", e)

            @block.vector
            def _(e):
                run("dve", e)

            @block.gpsimd
            def _(e):
                run("pool", e)

            @block.sync
            def _(e):
                run("sp", e)
        for s, i in sidx.items():
            if i >= 0:
                self.semcnt[i] = final[s]
        self.st.close()
        self.nphase += 1
        self.begin()

    def finish(self):
        self.g.close()


def _o(c, expr):
    return (expr + c) if c else expr


class Ring:
    def __init__(self, P, name, n, shape, dt, psum=False):
        self.n = n
        self.i = 0
        self.t = [(P.ps if psum else P.sb)("%s%d" % (name, j), shape, dt) for j in range(n)]
        self.k = ["%s%d" % (name, j) for j in range(n)]

    def next(self):
        j = self.i % self.n
        self.i += 1
        return self.t[j], self.k[j]


class Pack:
    def __init__(self):
        self.items = []
        self.off = {}
        self.n = 0

    def add(self, name, a):
        a = np.asarray(a, np.float32)
        rows = a.shape[0]
        flat = a.reshape(rows, -1)
        buf = np.zeros((128, flat.shape[1]), np.float32)
        buf[:rows] = flat
        self.off[name] = (self.n, rows, a.shape[1:])
        self.n += flat.shape[1]
        self.items.append(buf)

    def array(self):
        return np.ascontiguousarray(np.concatenate(self.items, axis=1))


def f32_consts(S):
    NSEL = S // 64
    p = Pack()
    j = np.arange(128)[:, None]
    i = np.arange(128)[None, :]
    same = (j // 64) == (i // 64)
    p.add("ident", np.eye(128))
    p.add("ones", np.ones((128, 128)))
    p.add("tri", (same & (j <= i)).astype(np.float32))
    p.add("m3", -(same & (j > i)).astype(np.float32))
    hi = (np.arange(128) >= 64).astype(np.float32)
    x = np.arange(2 * NSEL)[None, :]
    d = x - NSEL - hi[:, None]
    at = np.where(d > 0, -BIG, np.where(d >= -1, BIG, 0.0))
    p.add("at", at)
    half = 8
    invf = np.power(np.float32(500000.0), -np.arange(half, dtype=np.float32) * np.float32(2.0 / 16)).astype(np.float32)
    p.add("invf", np.concatenate([invf, invf])[:, None])
    p.add("sgn", np.concatenate([-np.ones(8), np.ones(8)])[:, None])
    rsw = np.zeros((64, 16), np.float32)
    for ii in range(16):
        rsw[(ii + 8) % 16, ii] = 1.0
    p.add("rsw", rsw)
    selg = np.zeros((32, 6, 65), np.float32)
    for br in range(3):
        for hh in range(2):
            selg[hh * 3 + br, br * 2 + hh, 64] = 1.0
    p.add("selg", selg)
    return p


def bf_consts(S):
    NM = S // 16
    NMT = max(1, NM // 128)
    NSEL = S // 64
    p = Pack()
    j = np.arange(128)[:, None]
    t = np.arange(128)[None, :]
    p.add("identb", np.eye(128))
    p.add("onesb", np.ones((128, 8)))
    c = np.where(j <= t, 0.0, NEG)
    p.add("causal2", np.stack([c, c], 1))
    a = np.where(j > t, 0.0, NEG)
    p.add("antic2", np.stack([a, a], 1))
    fl = np.floor((np.arange(128) - 31) / 16.0)[None, :]
    cm = np.zeros((128, 17, 2, 128), np.float32)
    for o in range(17):
        m = np.where(j - fl <= 8 * o, 0.0, NEG)
        cm[:, o, 0] = m
        cm[:, o, 1] = m
    p.add("cmpmask", cm)
    n = np.arange(NMT * 128)
    ncmp = NM - 1
    bs = n * 16
    ss = np.arange(NSEL) * 64
    ov = ((bs[:, None] < ss[None, :] + 64) & (bs[:, None] + 32 > ss[None, :]) & (n[:, None] < ncmp)).astype(np.float32)
    p.add("ov", ov.reshape(NMT, 128, NSEL).transpose(1, 0, 2))
    return p


def epat_const(S):
    key = np.arange(S)
    r = np.arange(64)[:, None]
    return (((key // 64) % 64)[None, :] == r).astype(np.float32)


VEC_ITEMS = ["gmix", "gffn", "gple", "gfin", "pscale", "gnorm", "posFk", "posGk", "posFv", "posGv"]


def vec_pack(inp, l):
    p = Pack()
    fm = lambda v: np.asarray(v, np.float32).reshape(-1, 128).T
    p.add("gmix", fm(inp["norm_mix"][l]))
    p.add("gffn", fm(inp["norm_ffn"][l]))
    p.add("gple", fm(inp["norm_ple"][l]))
    p.add("gfin", fm(inp["norm_final"]))
    p.add("pscale", fm(inp["pool_scale"][l]))
    p.add("gnorm", np.asarray(inp["gla_norm"][l], np.float32)[:, None])
    p.add("posk", np.asarray(inp["cmp_pos_k"][l], np.float32).T)
    p.add("posv", np.asarray(inp["cmp_pos_v"][l], np.float32).T)
    return p


class Builder:
    def __init__(self, S, depth=DEPTH, debug=False, stop_after=None):
        self.S = S
        self.T = S // 4
        self.depth = depth
        self.debug = debug
        self.stop_after = stop_after
        self.nc = bass.Bass("TRN2", target_bir_lowering=False)
        self.P = Prog(self.nc)
        self.cf_off = f32_consts(S)
        self.cb_off = bf_consts(S)
        self.build()

    def din(self, name, shape, dt=F32):
        return self.nc.dram_tensor(name, list(shape), dt, kind="ExternalInput")

    def scratch(self, name, shape, dt, collective=False):
        if self.debug and not collective:
            return self.nc.dram_tensor(name, list(shape), dt, kind="ExternalOutput")
        return self.nc.dram_tensor(name, list(shape), dt)

    def xf(self, r0, n):
        i = r0 // self.CR
        assert (r0 + n - 1) // self.CR == i
        lo = r0 - i * self.CR
        return self.XFc[i][lo:lo + n, :]

    def gf(self, s, blk):
        r0 = blk * 128
        i = r0 // self.CR
        rows = min(self.CR, 1280 - i * self.CR)
        lo = r0 - i * self.CR
        return self.GFc[i][s * rows + lo:s * rows + lo + 128, :]

    def mm(self, out, lhsT, rhs, start, stop, rd, wr):
        self.P.op("pe", lambda e: e.matmul(out, lhsT=lhsT, rhs=rhs, start=start, stop=stop), rd, wr)

    def tp(self, out, in_, ident, rd, wr):
        self.P.op("pe", lambda e: e.transpose(out=out, in_=in_, identity=ident), rd, wr)

    def act(self, out, in_, func, rd, wr, scale=None, bias=None):
        kw = {}
        if scale is not None:
            kw["scale"] = scale
        if bias is not None:
            kw["bias"] = bias
        self.P.op("act", lambda e: e.activation(out=out, in_=in_, func=func, **kw), rd, wr)

    def tt(self, eng, out, in0, in1, op, rd, wr):
        self.P.op(eng, lambda e: e.tensor_tensor(out=out, in0=in0, in1=in1, op=op), rd, wr)

    def ts(self, eng, out, in0, s1, s2, op0, op1, rd, wr):
        if op1 is None:
            self.P.op(eng, lambda e: e.tensor_scalar(out=out, in0=in0, scalar1=s1, scalar2=None, op0=op0), rd, wr)
        else:
            self.P.op(eng, lambda e: e.tensor_scalar(out=out, in0=in0, scalar1=s1, scalar2=s2, op0=op0, op1=op1), rd, wr)

    def stt(self, out, in0, scalar, in1, op0, op1, rd, wr):
        self.P.op("dve", lambda e: e.scalar_tensor_tensor(out=out, in0=in0, scalar=scalar, in1=in1, op0=op0, op1=op1), rd, wr)

    def cp(self, eng, out, in_, rd, wr):
        if eng == "act":
            self.P.op("act", lambda e: e.copy(out=out, in_=in_), rd, wr)
        else:
            self.P.op(eng, lambda e: e.tensor_copy(out=out, in_=in_), rd, wr)

    def memset(self, eng, ap, val, wr):
        self.P.op(eng, lambda e: e.memset(ap, val), (), wr)

    def ld(self, out, in_, wr, key, queue="sp", rd=()):
        self.P.dma(lambda e: e.dma_start(out=out, in_=in_), rd, wr, key=key, queue=queue)

    def stq(self, out, in_, rd, key, wr=()):
        self.P.dma(lambda e: e.dma_start(out=out, in_=in_), rd, wr, key=key, queue="pool")

    def dyn(self, mk, wr, key, rd=()):
        self.P.uses_pid = True
        self.P.dma(lambda e: mk(e, self.P.pid), rd, wr, key=key, queue="sp")

    def cfv(self, name):
        off, rows, shp = self.cf_off.off[name]
        n = int(np.prod(shp))
        ap = self.cf[0:rows, off:off + n]
        if len(shp) == 2:
            ap = ap.rearrange("p (a b) -> p a b", b=shp[1])
        return ap

    def cbv(self, name):
        off, rows, shp = self.cb_off.off[name]
        n = int(np.prod(shp))
        ap = self.cb[0:rows, off:off + n]
        if len(shp) == 2:
            ap = ap.rearrange("p (a b) -> p a b", b=shp[1])
        elif len(shp) == 3:
            ap = ap.rearrange("p (a b c) -> p a b c", b=shp[1], c=shp[2])
        return ap

    def vv(self, name):
        off, rows, shp = self.vec_off.off[name]
        n = int(np.prod(shp))
        return self.vec[0:rows, off:off + n]

    def build(self):
        nc, P, S, T = self.nc, self.P, self.S, self.T
        depth = self.depth
        self.xT = self.din("xT", [D, T])
        self.pT = self.din("pT", [depth, 256, T])
        self.pos = self.din("pos", [1, T], I32)
        self.w_in = self.din("w_in", [depth, D, INW])
        self.pool_w = self.din("pool_w", [depth, 4, 128, 128])
        self.cmp_w_k = self.din("cmp_w_k", [depth, 2048, 64])
        self.cmp_w_v = self.din("cmp_w_v", [depth, 2048, 64])
        self.wga = self.din("wga", [depth, 32, 256])
        self.w_branch = self.din("w_branch", [depth, 3, 512, D])
        self.w_out = self.din("w_out", [depth, D, D])
        self.w_ff1 = self.din("w_ff1", [depth, D, 4 * D])
        self.w_ff2 = self.din("w_ff2", [depth, 4 * D, D])
        self.w_ple_gate = self.din("w_ple_gate", [depth, D, D])
        self.w_ple_proj = self.din("w_ple_proj", [depth, 256, D])
        self.vec_off = vec_pack_offsets()
        self.vecs = self.din("vecs", [depth, 128, self.vec_off.n])
        self.cf_in = self.din("cf", [128, self.cf_off.n])
        self.cb_in = self.din("cb", [128, self.cb_off.n])
        self.epat_in = self.din("epat", [64, S])
        self.pc_in = self.din("pc", [128, NPC])
        self.pcb_in = self.din("pcb", [128, 192])
        self.out = nc.dram_tensor("outT", [D, T], F32, kind="ExternalOutput")
        sc = self.scratch
        self.hT = sc("hT", [D, T], F32)
        self.uT = sc("uT", [512, T], F32)
        self.rT = sc("rT", [512, T], BF16)
        self.gmT = sc("gmT", [3 * D, T], BF16)
        self.tabs = sc("tabs", [2, 16, T], F32)
        self.CR = max(128, min(1024, (524288 // T) // 128 * 128))
        self.XFc, self.GFc = [], []
        r = 0
        while r < 1280:
            n = min(self.CR, 1280 - r)
            self.XFc.append(sc("X_F%d" % len(self.XFc), [n, T], BF16, True))
            self.GFc.append(sc("G_F%d" % len(self.GFc), [4 * n, T], BF16, True))
            r += n
        self.XTc = [sc("X_T%d" % j, [512, 1024], BF16, True) for j in range(T // 512)]
        self.GTc = [sc("G_T%d" % j, [4 * 512, 1024], BF16, True) for j in range(T // 512)]
        self.X_fg = sc("X_fg", [512, T // 16], F32, True)
        self.X_gn = sc("X_gn", [32, T], F32, True)
        self.X_ge = sc("X_ge", [256, T // 64], F32, True)
        self.X_u = sc("X_u", [512, 16], F32, True)
        self.G_fg = sc("G_fg", [4 * 512, T // 16], F32, True)
        self.G_gn = sc("G_gn", [4 * 32, T], F32, True)
        self.G_ge = sc("G_ge", [4 * 256, T // 64], F32, True)
        self.G_u = sc("G_u", [4 * 512, 16], F32, True)
        self.Yc = [[sc("Y%d_%d" % (bc, sg), [128, T], BF16, True) for sg in range(4)] for bc in range(2)]
        self.GYc = [[sc("G_Y%d_%d" % (bc, sg), [4 * 128, T], BF16, True) for sg in range(4)] for bc in range(2)]
        if self.debug:
            self.dbgY = nc.dram_tensor("dbgY", [8, 4 * 128, T], BF16, kind="ExternalOutput")
            self.dbgXF = nc.dram_tensor("dbgXF", [1280, T], BF16, kind="ExternalOutput")
            self.dbgXT = nc.dram_tensor("dbgXT", [T, 1024], BF16, kind="ExternalOutput")
            self.dbgfg = nc.dram_tensor("dbgfg", [512, T // 16], F32, kind="ExternalOutput")
            self.dbggn = nc.dram_tensor("dbggn", [32, T], F32, kind="ExternalOutput")
            self.dbgge = nc.dram_tensor("dbgge", [256, T // 64], F32, kind="ExternalOutput")
        self.cf = P.gsb("cf_sb", [128, self.cf_off.n], F32)
        self.epsb = P.gsb("epsb", [128, 1], F32)

        self.phase_setup()
        stages = []
        for l in range(depth):
            stages += [("A", l), ("AG1", l), ("NSA", l), ("GLA", l), ("AG2", l), ("C1", l), ("C2", l), ("C3", l)]
        for nm, l in stages:
            getattr(self, "phase_" + nm)(l)
            P.end()
            if self.stop_after == (nm, l):
                break
        P.finish()

    def phase_setup(self):
        P, T = self.P, self.T
        self.ld(self.cf[:], self.cf_in[:, :], ["cf"], "cf")
        self.memset("dve", self.epsb[:], EPS, ["epsb"])
        posi = P.sb("posi", [16, 512], I32)
        ang = P.sb("ang", [16, 512], F32)
        red = P.sb("red", [16, 512], F32)
        xs = P.sb("xs", [16, 512], F32)
        ki = P.sb("ki", [16, 512], I32)
        tb = P.sb("tb", [16, 2, 512], F32)
        invf = self.cfv("invf")
        sgn = self.cfv("sgn")
        for c in range(T // 512):
            sl = slice(c * 512, (c + 1) * 512)
            self.ld(posi[:], self.pos[0:1, sl].partition_broadcast(16), ["posi"], "posi")
            self.cp("dve", ang[:], posi[:], ["posi"], ["ang"])
            self.ts("dve", ang[:], ang[:], invf[:, 0:1], None, ALU.mult, None, ["ang", "cf"], ["ang"])
            C1, C2 = 6.28125, 2 * PI - 6.28125
            for a_, shift in ((0, 0.5 * PI), (1, 0.0)):
                self.ts("dve", xs[:], ang[:], shift, None, ALU.add, None, ["ang"], ["xs"])
                self.ts("dve", red[:], xs[:], 1.0 / (2 * PI), None, ALU.mult, None, ["xs"], ["red"])
                self.cp("dve", ki[:], red[:], ["red"], ["ki"])
                self.cp("dve", red[:], ki[:], ["ki"], ["red"])
                self.stt(xs[:], red[:], -C1, xs[:], ALU.mult, ALU.add, ["red", "xs"], ["xs"])
                self.stt(xs[:], red[:], -C2, xs[:], ALU.mult, ALU.add, ["red", "xs"], ["xs"])
                self.ts("dve", red[:], xs[:], PI, -2 * PI, ALU.is_gt, ALU.mult, ["xs"], ["red"])
                self.tt("dve", xs[:], xs[:], red[:], ALU.add, ["xs", "red"], ["xs"])
                self.ts("dve", red[:], xs[:], -PI, 2 * PI, ALU.is_lt, ALU.mult, ["xs"], ["red"])
                self.tt("dve", xs[:], xs[:], red[:], ALU.add, ["xs", "red"], ["xs"])
                self.act(tb[:, a_, :], xs[:], AF.Sin, ["xs"], ["tb"])
            self.ts("dve", tb[:, 1, :], tb[:, 1, :], sgn[:, 0:1], None, ALU.mult, None, ["tb", "cf"], ["tb"])
            self.stq(self.tabs.ap().rearrange("a p t -> p a t")[:, 0:2, sl], tb[:], ["tb"], "tb_st")
        P.end()

    def load_vec(self, l):
        P = self.P
        self.vec = P.sb("vec", [128, self.vec_off.n], F32)
        self.ld(self.vec[:], self.vecs[l, :, :], ["vec"], "vec")

    def load_pc(self):
        P = self.P
        self.pc = P.sb("pc", [128, NPC], F32)
        self.ld(self.pc[:], self.pc_in[:, :], ["pc"], "pc")
        self.pcb = P.sb("pcb", [128, 192], BF16)
        self.ld(self.pcb[:], self.pcb_in[:, :], ["pcb"], "pcb", queue="pool")
        self.oh_r = [self.pc[:, 2049 + i:2050 + i] for i in range(4)]
        self.oh_g = [self.pc[:, 2053 + i:2054 + i] for i in range(2)]
        self.oh_hp = [self.pc[:, 2055 + i:2056 + i] for i in range(2)]
        self.oh_prev = [self.pc[:, 2057 + i:2058 + i] for i in range(4)]

    def select(self, out, terms, rd, wr):
        (a0, s0) = terms[0]
        n = a0.shape[0]
        fix = lambda s: s if s.shape[0] == n else s[0:n, :]
        self.ts("dve", out, a0, fix(s0), None, ALU.mult, None, rd + ["pc"], wr)
        for a, s in terms[1:]:
            self.stt(out, a, fix(s), out, ALU.mult, ALU.add, rd + ["pc"] + wr, wr)

    def rmsnorm(self, h, hkey, gname, out, okey, sqR, ssps, rs):
        ones = self.cfv("ones")
        g = self.vv(gname)
        for k in range(8):
            sq, sk = sqR.next()
            self.act(sq[:], h[:, k, :], AF.Square, [hkey], [sk])
            self.mm(ssps[:, :], ones[:, :], sq[:], k == 0, k == 7, [sk, "cf"], ["ssps"])
        self.act(rs[:], ssps[:, :], AF.Ln, ["ssps"], ["rs"], scale=1.0 / D, bias=self.epsb[:, 0:1])
        self.act(rs[:], rs[:], AF.Exp, ["rs"], ["rs"], scale=-0.5)
        if out is not None:
            for k in range(8):
                self.stt(out[:, k, :], h[:, k, :], g[:, k:k + 1], rs[:], ALU.mult, ALU.mult, [hkey, "rs", "vec"], [okey])

    def phase_A(self, l):
        P, T = self.P, self.T
        NG = T // 512
        self.load_vec(l)
        Win = P.sb("Win", [128, 8, INW], BF16)
        P.dma([(lambda e, k=k: e.dma_start(out=Win[:, k, :], in_=self.w_in[l, k * 128:(k + 1) * 128, :])) for k in range(8)],
              (), ["Win"], key="Win", queue="pool")
        Wc = [P.sb("Wck", [64, 32, 64], BF16), P.sb("Wcv", [64, 32, 64], BF16)]
        self.ld(Wc[0][:], self.cmp_w_k[l].rearrange("(l d) o -> d l o", d=64), ["Wc0"], "Wc0", queue="pool")
        self.ld(Wc[1][:], self.cmp_w_v[l].rearrange("(l d) o -> d l o", d=64), ["Wc1"], "Wc1", queue="pool")
        wga = P.sb("wga", [32, 256], BF16)
        self.ld(wga[:], self.wga[l, :, :], ["wga"], "wga", queue="pool")
        hsrc = (self.xT if l == 0 else self.hT).ap().rearrange("(k p) t -> p k t", p=128)
        hR = Ring(P, "hA", 1, [128, 8, 512], F32)
        tabR = Ring(P, "tab", 1, [16, 2, 512], F32)
        sqR = Ring(P, "sq", 2, [128, 512], F32)
        rs = P.sb("rs", [128, 512], F32)
        aT = P.sb("aT", [128, 8, 512], BF16)
        qfR = Ring(P, "qf", 2, [64, 512], F32)
        t1 = P.sb("t1", [16, 512], F32)
        t2 = P.sb("t2", [16, 512], F32)
        obR = Ring(P, "ob", 3, [128, 512], BF16)
        kb = P.sb("kb", [64, 512], BF16)
        pb = P.sb("pb", [64, 2, 32], BF16)
        cbias = P.sb("cbias", [64, 4], F32)
        fgs = P.sb("fgs", [64, 2, 32], F32)
        gnb = P.sb("gnb", [24, 512], F32)
        ubR = Ring(P, "ub", 2, [128, 512], F32)
        rbR = Ring(P, "rb", 2, [128, 512], BF16)
        gmR = Ring(P, "gmb", 2, [128, 2, 512], BF16)
        glaug = P.sb("glaug", [32, 512], BF16)
        vswb = P.sb("vswb", [128, 4, 256], BF16)
        ktb = P.sb("ktb", [128, 4, 256], BF16)
        vgb = P.sb("vgb", [128, 4, 512], BF16)
        ez = P.sb("ez", [128, 256], F32)
        la = P.sb("la", [128, 256], F32)
        ekt = P.sb("ekt", [128, 256], F32)
        e1 = P.sb("e1", [128, 2, 512], F32)
        e2 = P.sb("e2", [128, 2, 512], F32)
        ebt = P.sb("ebt", [128, 2, 8], F32)
        qgf = P.sb("qgf", [128, 4, 512], F32)
        ppR = Ring(P, "pp", 2, [128, 512], F32, psum=True)
        ssps = P.ps("ssps", [128, 512])
        pmisc = P.ps("pmisc", [128, 512])
        tm = P.ps("tm", [128, 1024])
        pz = ssps
        lct = P.ps("lct", [128, 2, 512])
        ident = self.cfv("ident")
        tri = self.cfv("tri")
        m3 = self.cfv("m3")
        rsw = self.cfv("rsw")
        self.memset("dve", glaug[:], 1.0, ["glaug"])
        self.cp("dve", pb[:, 0, :], self.vv("posk"), ["vec"], ["pb"])
        self.cp("dve", pb[:, 1, :], self.vv("posv"), ["vec"], ["pb"])
        for kd in range(2):
            for FG in range(2):
                col = 256 + kd * 2 + FG
                for ll in range(16):
                    self.mm(pmisc[0:64, col:col + 1], Wc[kd][:, FG * 16 + ll, :], pb[:, kd, FG * 16 + ll:FG * 16 + ll + 1],
                            ll == 0, ll == 15, ["pb", "Wc%d" % kd], ["pmisc"])
        self.cp("act", cbias[:], pmisc[0:64, 256:260], ["pmisc"], ["cbias"])

        def proj(c0, M):
            ps, pk = ppR.next()
            for k in range(8):
                self.mm(ps[0:M, :], Win[:, k, c0:c0 + M], aT[:, k, :], k == 0, k == 7, ["aT", "Win"], [pk])
            return ps, pk

        for tg in range(NG):
            t0 = tg * 512
            sl = slice(t0, t0 + 512)
            h, hk = hR.next()
            self.ld(h[:], hsrc[:, :, sl], [hk], hk)
            tab, tk = tabR.next()
            self.ld(tab[:], self.tabs.ap().rearrange("a p t -> p a t")[:, :, sl], [tk], tk)
            self.rmsnorm(h, hk, "gmix", aT, "aT", sqR, ssps, rs)
            heads = [(512 + 64 * hh, "q", 64 * hh) for hh in range(8)]
            for g in range(2):
                heads.append((1024 + 0 * 128 + 64 * g, "kc", g))
                heads.append((1024 + 2 * 128 + 64 * g, "k", 512 + 0 * 128 + 64 * g))
                heads.append((1024 + 4 * 128 + 64 * g, "k", 512 + 1 * 128 + 64 * g))
                heads.append((1024 + 1 * 128 + 64 * g, "vc", g))
            for c0, kind, dst in heads:
                ps, pk = proj(c0, 64)
                qf, qk = qfR.next()
                self.cp("act", qf[:], ps[0:64, :], [pk], [qk])
                if kind != "vc":
                    self.mm(pmisc[0:16, :], rsw[:, :], qf[:], True, True, [qk, "cf"], ["pmisc"])
                    self.tt("dve", t1[:], qf[0:16, :], tab[:, 0, :], ALU.mult, [qk, tk], ["t1"])
                    self.tt("dve", t2[:], pmisc[0:16, :], tab[:, 1, :], ALU.mult, ["pmisc", tk], ["t2"])
                    self.tt("dve", qf[0:16, :], t1[:], t2[:], ALU.add, ["t1", "t2"], [qk])
                if kind == "q" or kind == "k":
                    ob, ok = obR.next()
                    self.ts("dve", ob[0:64, :], qf[:], 0.125 if kind == "q" else 1.0, None, ALU.mult, None, [qk], [ok])
                    self.stq(self.xf(dst, 64)[:, sl], ob[0:64, :], [ok], ok + "s")
                else:
                    kd = 0 if kind == "kc" else 1
                    self.cp("dve", kb[:], qf[:], [qk], ["kb"])
                    for FG in range(2):
                        for ll in range(16):
                            self.mm(pmisc[0:64, 64 + FG * 32:64 + FG * 32 + 32], Wc[kd][:, FG * 16 + ll, :], kb[:, ll:512:16],
                                    ll == 0, ll == 15, ["kb", "Wc%d" % kd], ["pmisc"])
                        self.act(fgs[:, FG, :], pmisc[0:64, 64 + FG * 32:64 + FG * 32 + 32], AF.Identity, ["pmisc", "cbias"], ["fgs"],
                                 bias=cbias[:, kd * 2 + FG:kd * 2 + FG + 1])
                    r0 = (kd * 2 + dst) * 128
                    self.stq(self.X_fg[r0:r0 + 128, tg * 32:(tg + 1) * 32].rearrange("(f d) m -> d f m", d=64), fgs[:], ["fgs"], "fgs_s")
            ps, pk = proj(1792, 24)
            self.act(gnb[:], ps[0:24, :], AF.Sigmoid, [pk], ["gnb"])
            self.stq(self.X_gn[0:24, sl], gnb[:], ["gnb"], "gnb_s")
            for c in range(4):
                ps, pk = proj(c * 128, 128)
                ub, ubk = ubR.next()
                self.cp("act", ub[:], ps[:, :], [pk], [ubk])
                self.stq(self.uT[c * 128:(c + 1) * 128, sl], ub[:], [ubk], ubk + "s")
                if tg == NG - 1:
                    self.stq(self.X_u[c * 128:(c + 1) * 128, :], ub[:, 496:512], [ubk], ubk + "s")
            for c in range(4):
                ps, pk = proj(2856 + c * 128, 128)
                rb, rbk = rbR.next()
                self.act(rb[:], ps[:, :], AF.Silu, [pk], [rbk])
                self.stq(self.rT[c * 128:(c + 1) * 128, sl], rb[:], [rbk], rbk + "s")
            for c2 in range(12):
                gmb, gk = gmR.next()
                for c in range(2):
                    ps, pk = proj(3368 + (c2 * 2 + c) * 128, 128)
                    self.act(gmb[:, c, :], ps[:, :], AF.Sigmoid, [pk], [gk])
                self.stq(self.gmT.ap().rearrange("(g p) t -> p g t", p=128)[:, c2 * 2:(c2 + 1) * 2, sl], gmb[:], [gk], gk + "s")
            ps, pk = proj(2840, 16)
            self.cp("act", glaug[0:16, :], ps[0:16, :], [pk], ["glaug"])
            for c in range(4):
                ps, pk = proj(1816 + c * 128, 128)
                self.cp("act", qgf[:, c, :], ps[:, :], [pk], ["qgf"])
            for tt_ in range(4):
                ts_ = slice(tt_ * 128, (tt_ + 1) * 128)
                for c0, n, o in ((1024 + 384, 128, 0), (1024 + 640, 128, 128), (2072, 256, 256), (2328, 512, 512)):
                    for k in range(8):
                        self.mm(tm[:, o:o + n], aT[:, k, ts_], Win[:, k, c0:c0 + n], k == 0, k == 7, ["aT", "Win"], ["tm"])
                self.cp("act", vswb[:, tt_, :], tm[:, 0:256], ["tm"], ["vswb"])
                self.cp("act", vgb[:, tt_, :], tm[:, 512:1024], ["tm"], ["vgb"])
                self.mm(pz[:, 0:256], glaug[:, ts_], wga[:, :], True, True, ["glaug", "wga"], ["ssps"])
                self.act(ez[:], pz[:, 0:256], AF.Exp, ["ssps"], ["ez"], scale=-1.0)
                self.act(la[:], ez[:], AF.Ln, ["ez"], ["la"], bias=1.0)
                self.mm(pz[:, 256:512], m3[:, :], la[:], True, True, ["la", "cf"], ["ssps"])
                self.act(ekt[:], pz[:, 256:512], AF.Exp, ["ssps"], ["ekt"], scale=1.0 / 16)
                self.tt("dve", ktb[:, tt_, :], tm[:, 256:512], ekt[:], ALU.mult, ["tm", "ekt"], ["ktb"])
                for fc in range(2):
                    self.mm(lct[:, fc, ts_], la[:, fc * 128:(fc + 1) * 128], tri[:, :], True, True, ["la", "cf"], ["lct"])
            xt = self.XTc[tg].ap().rearrange("(tt p) c -> p tt c", p=128)
            self.stq(xt[:, :, 0:256], vswb[:], ["vswb"], "vswb_s")
            self.stq(xt[:, :, 256:512], ktb[:], ["ktb"], "ktb_s")
            self.stq(xt[:, :, 512:1024], vgb[:], ["vgb"], "vgb_s")
            self.act(e1[:], lct[:], AF.Exp, ["lct"], ["e1"], scale=-1.0 / 16)
            self.act(e2[:], lct[:], AF.Exp, ["lct"], ["e2"], scale=1.0 / 16)
            for fc in range(2):
                ob, ok = obR.next()
                self.stt(ob[:, :], qgf[:, fc, :], 0.125, e1[:, fc, :], ALU.mult, ALU.mult, ["qgf", "e1"], [ok])
                self.stq(self.xf(768 + fc * 128, 128)[:, sl], ob[:, :], [ok], ok + "s")
                ob, ok = obR.next()
                self.tt("dve", ob[:, :], qgf[:, 2 + fc, :], e2[:, fc, :], ALU.mult, ["qgf", "e2"], [ok])
                self.stq(self.xf(1024 + fc * 128, 128)[:, sl], ob[:, :], [ok], ok + "s")
            self.cp("dve", ebt[:], e1[:, :, 63:512:64], ["e1"], ["ebt"])
            self.stq(self.X_ge.ap().rearrange("(f p) c -> p f c", p=128)[:, :, tg * 8:(tg + 1) * 8], ebt[:], ["ebt"], "ebt_s")

    def phase_AG1(self, l):
        P = self.P
        grp = [[0, 1, 2, 3], [4, 5, 6, 7]]
        pairs = list(zip(self.XFc, self.GFc)) + list(zip(self.XTc, self.GTc)) + [(self.X_fg, self.G_fg), (self.X_gn, self.G_gn),
                 (self.X_ge, self.G_ge), (self.X_u, self.G_u)]
        import os
        only = os.environ.get("KDBG_AG")
        for i, (a, b) in enumerate(pairs):
            if only is not None and str(i) not in only.split(","):
                continue
            P.dma(lambda e, a=a, b=b: e.collective_compute("AllGather", ALU.bypass, replica_groups=grp,
                                                           ins=[a.ap().opt()], outs=[b.ap().opt()]),
                  (), (), key="cc%d" % i, queue="pool", inc=1)
        if self.debug and l == 0 and os.environ.get("KDBG_COPY", "1") == "1":
            cps = [(self.X_fg.ap(), self.dbgfg.ap()), (self.X_gn.ap(), self.dbggn.ap()), (self.X_ge.ap(), self.dbgge.ap())]
            r = 0
            for c_ in self.XFc:
                n = c_.shape[0]
                cps.append((c_.ap(), self.dbgXF[r:r + n, :]))
                r += n
            for j, c_ in enumerate(self.XTc):
                cps.append((c_.ap(), self.dbgXT[j * 512:(j + 1) * 512, :]))
            for i, (a, b) in enumerate(cps):
                P.dma(lambda e, a=a, b=b: e.dma_start(out=b, in_=a), (), (), key="dbgc%d" % i, queue="sp")

    def phase_AG2(self, l):
        P = self.P
        grp = [[0, 1, 2, 3], [4, 5, 6, 7]]
        for bc in range(2):
            for sg in range(4):
                a, b = self.Yc[bc][sg], self.GYc[bc][sg]
                P.dma(lambda e, a=a, b=b: e.collective_compute("AllGather", ALU.bypass, replica_groups=grp,
                                                               ins=[a.ap().opt()], outs=[b.ap().opt()]),
                      (), ["G_Y%d%d" % (bc, sg)], key="ccY%d%d" % (bc, sg), queue="pool", inc=1)
                if self.debug and l == 0:
                    P.dma(lambda e, b=b, bc=bc, sg=sg: e.dma_start(out=self.dbgY[bc * 4 + sg, :, :], in_=b[:, :]), ["G_Y%d%d" % (bc, sg)], (),
                          key="dbgY%d%d" % (bc, sg), queue="sp")

    def phase_NSA(self, l):
        P, S, T = self.P, self.S, self.T
        NKT = S // 128
        NM = S // 16
        NMT = max(1, NM // 128)
        NSEL = S // 64
        NG4 = (NSEL + 63) // 64
        QPS = T // 128
        KsA = P.sb("KsA", [128, S], BF16)
        self.cb = P.sb("cb", [128, self.cb_off.n], BF16)
        self.ld(self.cb[:], self.cb_in[:, :], ["cb"], "cb", queue="pool")
        self.ld(KsA[64:128, :], self.epat_in[:, :], ["KsA"], "KsAe", queue="pool")
        KwT = P.sb("KwT", [64, S], BF16)
        Vs = P.sb("Vs", [128, NKT, 65], BF16)
        Vw = P.sb("Vw", [128, NKT, 65], BF16)
        Vc = P.sb("Vc", [128, NMT, 65], BF16)
        FG = [P.sb("FGs%d" % i, [64, NMT * 128], F32) for i in range(4)]
        KcA = P.sb("KcA", [64, NMT * 128], BF16)
        VcT = P.sb("VcT", [64, NMT * 128], F32)
        identf = self.cfv("ident")
        onesf = self.cfv("ones")
        at = self.cfv("at")
        identb = self.cbv("identb")
        onesb = self.cbv("onesb")
        causal2 = self.cbv("causal2")
        antic2 = self.cbv("antic2")
        cmpmask = self.cbv("cmpmask")
        ov = self.cbv("ov")
        G_fg, G_gn = self.G_fg, self.G_gn

        self.load_pc()
        selP = self.pcb[:, 0:64]
        kkR = Ring(P, "kk", 2, [128, 2, 512], BF16)
        vaR = Ring(P, "va", 2, [128, 4, 256], BF16)
        fgR = Ring(P, "fga", 2, [64, 8, T // 16], F32)
        pS = P.ps("pS", [128, 3, 512])
        pSs = pS
        nsel = 0
        for s in range(4):
            for kc in range(T // 512):
                c0 = s * T + kc * 512
                kk, kkk = kkR.next()
                P.dma([(lambda e, ty=ty, kk=kk, s=s, kc=kc: e.dma_start(out=kk[:, ty, :], in_=self.gf(s, 4 + ty)[:, kc * 512:(kc + 1) * 512])) for ty in range(2)],
                      (), [kkk], key=kkk)
                for ty, (dst, dk) in enumerate(((KsA, "KsA"), (KwT, "KwT"))):
                    self.mm(pSs[0:64, ty, :], selP, kk[:, ty, :], True, True, [kkk, "pcb"], ["pS%d" % ty])
                    self.cp("act" if (nsel % 2 == 0) else "dve", dst[0:64, c0:c0 + 512], pSs[0:64, ty, :], ["pS%d" % ty], [dk])
                    nsel += 1
            for j in range(T // 512):
                va, vak = vaR.next()
                self.ld(va[:], self.GTc[j][s * 512:(s + 1) * 512, 0:256].rearrange("(kt p) c -> p kt c", p=128), [vak], vak)
                k0 = s * QPS + 4 * j
                ks = slice(k0, k0 + 4)
                self.select(Vs[:, ks, 0:64], [(va[:, :, 0:64], self.oh_g[0]), (va[:, :, 64:128], self.oh_g[1])], [vak], ["Vs"])
                self.select(Vw[:, ks, 0:64], [(va[:, :, 128:192], self.oh_g[0]), (va[:, :, 192:256], self.oh_g[1])], [vak], ["Vw"])
            fga, fgk = fgR.next()
            self.ld(fga[:], G_fg[s * 512:(s + 1) * 512, :].rearrange("(a d) m -> d a m", d=64), [fgk], fgk)
            fgv = fga[:].rearrange("p (kd g f) m -> p kd g f m", kd=2, g=2)
            ms = slice(s * (T // 16), (s + 1) * (T // 16))
            for i in range(4):
                kd, fg = i // 2, i % 2
                self.select(FG[i][:, ms], [(fgv[:, kd, 0, fg, :], self.oh_g[0]), (fgv[:, kd, 1, fg, :], self.oh_g[1])], [fgk], ["FG%d" % i])
        self.memset("dve", Vs[:, :, 64:65], 1.0, ["Vs"])
        self.memset("dve", Vw[:, :, 64:65], 1.0, ["Vw"])
        self.memset("dve", Vc[:, :, 64:65], 1.0, ["Vc"])
        self.memset("dve", KcA[:], 0.0, ["KcA"])
        self.memset("dve", VcT[:], 0.0, ["VcT"])
        self.tt("dve", KcA[:, 0:NM - 1], FG[0][:, 0:NM - 1], FG[1][:, 1:NM], ALU.add, ["FG0", "FG1"], ["KcA"])
        self.tt("dve", VcT[:, 0:NM - 1], FG[2][:, 0:NM - 1], FG[3][:, 1:NM], ALU.add, ["FG2", "FG3"], ["VcT"])
        pO1 = P.ps("pO1", [128, 512])
        pO2 = P.ps("pO2", [128, 512])
        pI = P.ps("pI", [128, 1024])
        pTb = P.ps("pTb", [128, 4, 128], BF16)
        pSf = pS[:].rearrange("p a b -> p (a b)")
        for nt in range(NMT):
            self.tp(pO1[:, 0:64], VcT[:, nt * 128:(nt + 1) * 128], identf[0:64, 0:64], ["VcT", "cf"], ["pO1"])
            self.cp("act", Vc[:, nt, 0:64], pO1[:, 0:64], ["pO1"], ["Vc"])
        q4R = Ring(P, "q4", 2, [64, 4, 128], BF16)
        qMR = Ring(P, "qM", 2, [128, NG4, 2, 128], BF16)
        g6R = Ring(P, "g6", 2, [32, 128], F32)
        qaR = Ring(P, "qa", 2, [64, 8, 128], BF16)
        qg = P.sb("qg", [64, 4, 128], BF16)
        pTR = Ring(P, "pT", 4, [128, 512], BF16)
        for t_ in g6R.t:
            self.memset("dve", t_[:], 0.0, [g6R.k[g6R.t.index(t_)]])
        rsm = P.sb("rsm", [128, 4], F32)
        imp = P.sb("imp", [128, NSEL], F32)
        score = P.sb("score", [128, NSEL], F32)
        sc2 = P.sb("sc2", [128, NSEL], F32)
        m8 = P.sb("m8", [128, 8], F32)
        thr = P.sb("thr", [128, 1], F32)
        mpad = P.sb("mpad", [128, 64 + 64 * NG4], BF16)
        self.memset("dve", mpad[:], 0.0, ["mpad"])
        osb = P.sb("osb", [65, 3, 256], F32)
        rr = P.sb("rr", [65, 3, 256], F32)
        srow = P.sb("srow", [65, 3, 256], F32)
        ya = P.sb("ya", [64, 256], F32)
        yb2 = P.sb("yb2", [64, 256], F32)
        ybR = Ring(P, "yb", 2, [64, 256], BF16)
        ocmp = pO1[0:65, 0:256]
        owin = pO1[0:65, 256:512]
        oslc = pO2[0:65, 0:256]
        rsp = pO2[:, 256:260]
        impU = pI[:].rearrange("p (h j) -> p h j", h=4)

        nslot = 0
        for qb in range(NKT):
            s = qb // QPS
            tl = (qb % QPS) * 128
            q4, q4k = q4R.next()
            qM, qMk = qMR.next()
            g6, g6k = g6R.next()
            qa, qak = qaR.next()
            P.dma([(lambda e, j=j, qa=qa, s=s, tl=tl: e.dma_start(out=qa[:, 2 * j:2 * j + 2, :],
                                                                   in_=self.gf(s, j)[:, tl:tl + 128].rearrange("(h d) t -> d h t", d=64))) for j in range(4)],
                  (), [qak], key=qak)
            self.ld(g6[0:24, :], G_gn[s * 32:s * 32 + 24, tl:tl + 128], [g6k], g6k)
            self.select(qg[:], [(qa[:, 0:4, :], self.oh_g[0][0:64, :]), (qa[:, 4:8, :], self.oh_g[1][0:64, :])], [qak], ["qg"])
            self.select(q4[:, 0:2, :], [(qg[:, 0:2, :], self.oh_hp[0][0:64, :]), (qg[:, 2:4, :], self.oh_hp[1][0:64, :])], ["qg"], [q4k])
            self.select(q4[:, 2:4, :], [(qg[:, 2:4, :], self.oh_hp[0][0:64, :]), (qg[:, 0:2, :], self.oh_hp[1][0:64, :])], ["qg"], [q4k])
            for v in range(NG4):
                self.cp("pool", qM[0:64, v, :, :], q4[:, 0:2, :], [q4k], [qMk])
            q4f = q4[:].rearrange("p h t -> p (h t)")
            jobs = []
            ntmax = min((8 * qb + 6) // 128, NMT - 1)

            def mk_cmp(nt):
                off = 8 * qb - 128 * nt
                partial = off <= 128

                def qk(sps, sk):
                    self.mm(sps[:, 0:512], KcA[:, nt * 128:(nt + 1) * 128], q4f, True, not partial, ["KcA", q4k], [sk])
                    if partial:
                        idx = off // 8
                        for half in range(2):
                            self.mm(sps[:, half * 256:(half + 1) * 256], identb[:, :], cmpmask[:, idx, :, :].rearrange("p a b -> p (a b)"),
                                    False, half == 1, ["cb"], [sk])

                def pv(pT, pk):
                    self.mm(ocmp, Vc[:, nt, :], pT[:, 0:256], nt == 0, nt == ntmax, ["Vc", pk], ["pO1"])
                    for h in range(4):
                        self.mm(impU[:, h, 0:NSEL], pT[:, h * 128:(h + 1) * 128], ov[:, nt, :], nt == 0, nt == ntmax, [pk, "cb"], ["pI"])
                        self.mm(rsp[:, h:h + 1], pT[:, h * 128:(h + 1) * 128], onesb[:, 0:1], nt == 0, nt == ntmax, [pk, "cb"], ["pO2"])
                return dict(n=512, qk=qk, pv=pv)

            def selection():
                self.ts("dve", rsm[:], rsp, 1e-30, None, ALU.max, None, ["pO2"], ["rsm"])
                self.P.op("dve", lambda e: e.reciprocal(out=rsm[:], in_=rsm[:]), ["rsm"], ["rsm"])
                self.ts("dve", imp[:], impU[:, 0, 0:NSEL], rsm[:, 0:1], None, ALU.mult, None, ["pI", "rsm"], ["imp"])
                for h in range(1, 4):
                    self.stt(imp[:], impU[:, h, 0:NSEL], rsm[:, h:h + 1], imp[:], ALU.mult, ALU.add, ["pI", "rsm", "imp"], ["imp"])
                self.tt("dve", score[:], imp[:], at[:, NSEL - 2 * qb:2 * NSEL - 2 * qb], ALU.add, ["imp", "cf"], ["score"])
                self.memset("dve", score[:, 0:1], BIG, ["score"])
                self.P.op("dve", lambda e: e.max(out=m8[:], in_=score[:]), ["score"], ["m8"])
                self.P.op("dve", lambda e: e.match_replace(out=sc2[:], in_to_replace=m8[:], in_values=score[:], imm_value=-2 * BIG), ["score", "m8"], ["sc2"])
                self.P.op("dve", lambda e: e.max(out=m8[:], in_=sc2[:]), ["sc2"], ["m8"])
                self.ts("dve", thr[:], m8[:, 7:8], -0.5 * BIG, None, ALU.max, None, ["m8"], ["thr"])
                self.ts("dve", mpad[:, 64:64 + NSEL], score[:], thr[:, 0:1], NEG, ALU.is_lt, ALU.mult, ["score", "thr"], ["mpad"])

            def augrows():
                for v in range(NG4):
                    self.tp(pTb[:, v, :], mpad[:, 64 * v:64 * v + 128], identb[:, :], ["mpad", "cb"], ["pTb"])
                    for hh in range(2):
                        self.cp("dve", qM[64:128, v, hh, :], pTb[64:128, v, :], ["pTb"], [qMk + "a"])

            for nt in range(ntmax + 1):
                jobs.append(mk_cmp(nt))
            jobs[-1]["after"] = selection
            kts = list(range(max(0, qb - 4), qb + 1))

            def mk_win(kt):
                def qk(sps, sk):
                    nmask = (kt == qb) or (kt == qb - 4)
                    self.mm(sps[:, 0:256], KwT[:, kt * 128:(kt + 1) * 128], qM[0:64, 0, :, :].rearrange("p h t -> p (h t)"), True, not nmask, ["KwT", qMk], [sk])
                    if kt == qb:
                        self.mm(sps[:, 0:256], identb[:, :], causal2.rearrange("p a b -> p (a b)"), False, True, ["cb"], [sk])
                    elif kt == qb - 4:
                        self.mm(sps[:, 0:256], identb[:, :], antic2.rearrange("p a b -> p (a b)"), False, True, ["cb"], [sk])

                def pv(pT, pk):
                    self.mm(owin, Vw[:, kt, :], pT[:, 0:256], kt == kts[0], kt == kts[-1], ["Vw", pk], ["pO1w"])
                return dict(n=256, qk=qk, pv=pv)

            for kt in kts:
                jobs.append(mk_win(kt))

            def mk_slc(pair):
                def qk(sps, sk):
                    for i, kt in enumerate(pair):
                        v = kt // 32
                        o_ = sps[:, i * 256:(i + 1) * 256]
                        self.mm(o_, KsA[:, kt * 128:(kt + 1) * 128], qM[:, v, :, :].rearrange("p h t -> p (h t)"), True, kt != qb, ["KsA", qMk, qMk + "a"], [sk])
                        if kt == qb:
                            self.mm(o_, identb[:, :], causal2.rearrange("p a b -> p (a b)"), False, True, ["cb"], [sk])

                def pv(pT, pk):
                    for i, kt in enumerate(pair):
                        self.mm(oslc, Vs[:, kt, :], pT[:, i * 256:(i + 1) * 256], kt == 0, kt == qb, ["Vs", pk], ["pO2s"])
                return dict(n=256 * len(pair), qk=qk, pv=pv)

            first_slc = len(jobs)
            for i0 in range(0, qb + 1, 2):
                jobs.append(mk_slc(list(range(i0, min(i0 + 2, qb + 1)))))
            jobs[first_slc]["before"] = augrows
            LA = 2
            nj = len(jobs)
            slots = []
            for j in range(nj):
                slots.append(nslot % 3)
                nslot += 1

            def emit_qk(j):
                if "before" in jobs[j]:
                    jobs[j]["before"]()
                jobs[j]["qk"](pS[:, slots[j], :], "pS%d" % slots[j])

            n_cmp_jobs = ntmax + 1
            for j in range(min(LA, nj, first_slc)):
                emit_qk(j)
            pre = min(LA, nj, first_slc)
            for j in range(nj):
                pT, pk = pTR.next()
                n_ = jobs[j]["n"]
                self.act(pT[:, 0:n_], pS[:, slots[j], 0:n_], AF.Exp, ["pS%d" % slots[j]], [pk])
                jobs[j]["pv"](pT, pk)
                if "after" in jobs[j]:
                    jobs[j]["after"]()
                nxt = j + LA
                if nxt < nj and nxt >= pre:
                    emit_qk(nxt)
                elif nxt >= nj and False:
                    pass
            self.cp("act", osb[:, 0, :], ocmp, ["pO1"], ["osb"])
            self.cp("act", osb[:, 1, :], oslc, ["pO2s"], ["osb"])
            self.cp("act", osb[:, 2, :], owin, ["pO1w"], ["osb"])
            self.ts("dve", rr[64:65, :, :], osb[64:65, :, :], 1e-30, None, ALU.max, None, ["osb"], ["rr"])
            self.P.op("dve", lambda e: e.reciprocal(out=rr[64:65, :, :], in_=rr[64:65, :, :]), ["rr"], ["rr"])
            for br in range(3):
                for hh in range(2):
                    c0 = br * 256 + hh * 128
                    self.mm(pI[0:65, c0:c0 + 128], self.pc[0:32, 2061 + (br * 2 + hh) * 65:2061 + (br * 2 + hh + 1) * 65], g6[:, :], True, True, [g6k, "pc", "imp"], ["pI"])
            self.tt("dve", srow[64:65, :, :].rearrange("p a b -> p (a b)"), rr[64:65, :, :].rearrange("p a b -> p (a b)"), pI[64:65, 0:768],
                    ALU.mult, ["rr", "pI"], ["srow"])
            for br in range(3):
                self.mm(pSf[0:64, br * 256:(br + 1) * 256], onesf[64:65, 0:64], srow[64:65, br, :], True, True, ["srow", "cf"], ["pS0", "pS1"])
            self.tt("dve", ya[:], osb[0:64, 0, :], pSf[0:64, 0:256], ALU.mult, ["osb", "pS0", "pS1"], ["ya"])
            self.tt("dve", yb2[:], osb[0:64, 1, :], pSf[0:64, 256:512], ALU.mult, ["osb", "pS0", "pS1"], ["yb2"])
            self.tt("dve", ya[:], ya[:], yb2[:], ALU.add, ["ya", "yb2"], ["ya"])
            self.tt("dve", yb2[:], osb[0:64, 2, :], pSf[0:64, 512:768], ALU.mult, ["osb", "pS0", "pS1"], ["yb2"])
            yb, ybk = ybR.next()
            self.tt("dve", yb[:], ya[:], yb2[:], ALU.add, ["ya", "yb2"], [ybk])
            self.stq(self.Yc[0][s][:, tl:tl + 128].rearrange("(h d) t -> d h t", d=64), yb[:].rearrange("p (h t) -> p h t", h=2), [ybk], ybk + "s")

    def phase_GLA(self, l):
        P, S, T = self.P, self.S, self.T
        NKT = S // 128
        self.load_vec(l)
        G_ge = self.G_ge
        qsT = P.sb("qsT", [64, S], BF16)
        ksT = P.sb("ksT", [64, S], BF16)
        kt_ = P.sb("ktg", [128, NKT, 64], BF16)
        v_ = P.sb("vg", [128, NKT, 128], BF16)
        eb = P.sb("eb", [64, S // 64], F32)
        self.load_pc()
        selA = self.pcb[:, 64:128]
        selB = self.pcb[:, 128:192]
        gqR = Ring(P, "gq", 2, [128, 2, 2, 512], BF16)
        gtR = Ring(P, "gt", 2, [128, 4, 768], BF16)
        eba = P.sb("eba", [64, 16, T // 64], F32)
        paR = Ring(P, "pa", 2, [128, 512], F32, psum=True)
        nsel = 0
        for s in range(4):
            for kc in range(T // 512):
                c0 = s * T + kc * 512
                gq, gqk = gqR.next()
                P.dma([(lambda e, j=j, gq=gq, s=s, kc=kc: e.dma_start(out=gq[:, j // 2, j % 2, :], in_=self.gf(s, 6 + j)[:, kc * 512:(kc + 1) * 512])) for j in range(4)],
                      (), [gqk], key=gqk)
                for qk_, (dst, dk) in enumerate(((qsT, "qsT"), (ksT, "ksT"))):
                    pss_, pssk = paR.next()
                    self.mm(pss_[0:64, :], selA, gq[:, qk_, 0, :], True, False, [gqk, "pcb"], [pssk])
                    self.mm(pss_[0:64, :], selB, gq[:, qk_, 1, :], False, True, [gqk, "pcb"], [pssk])
                    self.cp("act" if (nsel % 2 == 0) else "dve", dst[0:64, c0:c0 + 512], pss_[0:64, :], [pssk], [dk])
                    nsel += 1
            for j in range(T // 512):
                gt, gtk = gtR.next()
                self.ld(gt[:], self.GTc[j][s * 512:(s + 1) * 512, 256:1024].rearrange("(kt p) c -> p kt c", p=128), [gtk], gtk)
                k0 = (s * T + j * 512) // 128
                ks = slice(k0, k0 + 4)
                self.select(kt_[:, ks, :], [(gt[:, :, h * 64:(h + 1) * 64], self.oh_r[h]) for h in range(4)], [gtk], ["ktg"])
                self.select(v_[:, ks, :], [(gt[:, :, 256 + h * 128:256 + (h + 1) * 128], self.oh_r[h]) for h in range(4)], [gtk], ["vg"])
        self.ld(eba[:], G_ge.ap().rearrange("(a d) c -> d a c", d=64), ["eba"], "eba")
        ebv = eba[:].rearrange("p (s h) c -> p s h c", s=4)
        self.select(eb[:].rearrange("p (s c) -> p s c", s=4), [(ebv[:, :, h, :], self.oh_r[h][0:64, :]) for h in range(4)], ["eba"], ["eb"])
        tri = self.cfv("tri")
        onesf = self.cfv("ones")
        gn = self.vv("gnorm")
        St = P.sb("St", [128, 128], F32)
        SbR = Ring(P, "Sb", 3, [128, 128], BF16)
        AtR = Ring(P, "At", 2, [128, 128], BF16)
        osq = P.sb("osq", [128, 512], F32)
        rs = P.sb("rsg", [128, 512], F32)
        obR = Ring(P, "og", 2, [128, 512], BF16)
        poR = Ring(P, "po", 2, [128, 512], F32, psum=True)
        pkR = Ring(P, "pk", 2, [128, 512], F32, psum=True)
        pn = P.ps("pn", [128, 512])
        self.memset("dve", St[:], 0.0, ["St"])
        Sb, Sbk = SbR.next()
        self.memset("dve", Sb[:], 0.0, [Sbk])
        for tg in range(S // 512):
            po, pok = poR.next()
            for tt_ in range(4):
                ti = tg * 4 + tt_
                c0 = ti * 128
                pa, pak = paR.next()
                self.mm(pa[:, 0:128], ksT[:, c0:c0 + 128], qsT[:, c0:c0 + 128], True, True, ["ksT", "qsT"], [pak])
                At, Atk = AtR.next()
                self.tt("dve", At[:], pa[:, 0:128], tri[:, :], ALU.mult, [pak, "cf"], [Atk])
                for c in range(2):
                    pb = 64 * c
                    cc = slice(c0 + pb, c0 + pb + 64)
                    oc = po[:, tt_ * 128 + pb:tt_ * 128 + pb + 64]
                    self.mm(oc, v_[pb:pb + 64, ti, :], At[pb:pb + 64, pb:pb + 64], True, False, ["vg", Atk], [pok])
                    self.mm(oc, Sb[0:64, :], qsT[:, cc], False, True, [Sbk, "qsT"], [pok])
                    pk_, pkk = pkR.next()
                    self.mm(pk_[0:64, 0:128], kt_[pb:pb + 64, ti, :], v_[pb:pb + 64, ti, :], True, True, ["ktg", "vg"], [pkk])
                    ci = 2 * ti + c
                    self.stt(St[0:64, :], St[0:64, :], eb[:, ci:ci + 1], pk_[0:64, 0:128], ALU.mult, ALU.add, ["St", "eb", pkk], ["St"])
                    Sb, Sbk = SbR.next()
                    self.cp("act", Sb[0:64, :], St[0:64, :], ["St"], [Sbk])
            self.act(osq[:], po[:, :], AF.Square, [pok], ["osq"])
            self.mm(pn[:, :], onesf[:, :], osq[:], True, True, ["osq", "cf"], ["pn"])
            self.act(rs[:], pn[:, :], AF.Ln, ["pn"], ["rsg"], scale=1.0 / 128, bias=self.epsb[:, 0:1])
            self.act(rs[:], rs[:], AF.Exp, ["rsg"], ["rsg"], scale=-0.5)
            ob, obk = obR.next()
            self.stt(ob[:], po[:, :], gn[:, 0:1], rs[:], ALU.mult, ALU.mult, [pok, "vec", "rsg"], [obk])
            t0 = tg * 512
            seg = t0 // T
            lc = t0 % T
            self.stq(self.Yc[1][seg][:, lc:lc + 512], ob[:], [obk], obk + "s")

    def phase_C1(self, l):
        P, S, T = self.P, self.S, self.T
        NG = T // 512
        self.load_vec(l)
        Wb = P.sb("Wb", [128, 3, 4, D], BF16)
        for br in range(3):
            self.ld(Wb[:, br, :, :], self.w_branch[l, br].rearrange("(wc p) d -> p wc d", p=128), ["Wb%d" % br], "Wb%d" % br, queue="pool")
        Wo = P.sb("Wo", [128, 8, D], BF16)
        self.ld(Wo[:], self.w_out[l].rearrange("(k p) d -> p k d", p=128), ["Wo"], "Wo", queue="pool")
        Wp = P.sb("Wpool", [128, 4, 128], BF16)
        self.ld(Wp[:], self.pool_w[l].rearrange("g c d -> c g d"), ["Wpool"], "Wpool", queue="pool")
        self.load_pc()
        pc = self.pc
        ic0 = pc[:, 1:1 + 2048].rearrange("p (g t) -> p g t", g=4)
        uh = P.sb("uh", [128, 4, 4, 16], F32)
        ylR = Ring(P, "yl", 2, [128, 4, 4, 512], BF16)
        hsrc = (self.xT if l == 0 else self.hT).ap().rearrange("(k p) t -> p k t", p=128)
        hdst = self.hT.ap().rearrange("(k p) t -> p k t", p=128)
        uview = self.uT.ap().rearrange("(g p) t -> p g t", p=128)
        hR = Ring(P, "hC", 1, [128, 8, 512], F32)
        uR = Ring(P, "uC", 2, [128, 4, 528], F32)
        ybR = Ring(P, "ybC", 2, [128, 4, 512], BF16)
        ycR = Ring(P, "ycC", 2, [128, 4, 512], BF16)
        rR = Ring(P, "rC", 2, [128, 4, 512], BF16)
        gmR = Ring(P, "gmC", 1, [128, 24, 512], BF16)
        sA = P.sb("sA", [128, 528], F32)
        sB = P.sb("sB", [128, 528], F32)
        pl = P.sb("pl", [128, 4, 512], BF16)
        yaT = P.sb("yaT", [128, 4, 512], BF16)
        ycg = P.sb("ycg", [128, 4, 512], BF16)
        m1 = P.sb("m1", [128, 512], F32)
        m2 = P.sb("m2", [128, 512], F32)
        mb = P.sb("mb", [128, 8, 512], BF16)
        ppR = Ring(P, "ppC", 6, [128, 512], F32, psum=True)
        G_u = self.G_u
        wins = (2, 4, 8, 16)
        for tg in range(NG):
            t0 = tg * 512
            sl = slice(t0, t0 + 512)
            h, hk = hR.next()
            self.ld(h[:], hsrc[:, :, sl], [hk], hk)
            u, uk = uR.next()
            self.ld(u[:, :, 16:528], uview[:, :, sl], [uk], uk + "a")
            if tg == 0:
                self.ld(uh[:], G_u.ap().rearrange("(s g p) t -> p s g t", s=4, g=4), ["uh"], "uh")
                self.select(u[:, :, 0:16], [(uh[:, s_, :, :], self.oh_prev[s_]) for s_ in range(4)], ["uh"], [uk])
            else:
                self.ld(u[:, :, 0:16], uview[:, :, t0 - 16:t0], [uk], uk + "b")
            yb, ybk = ybR.next()
            yc, yck = ycR.next()
            for bc, (yt_, ytk) in enumerate(((yb, ybk), (yc, yck))):
                yl, ylk = ylR.next()
                P.dma([(lambda e, sg=sg, yl=yl, bc=bc, sl=sl: e.dma_start(out=yl[:, sg, :, :], in_=self.GYc[bc][sg][:, sl].rearrange("(r p) t -> p r t", p=128))) for sg in range(4)],
                      (), [ylk], key=ylk)
                for r_ in range(4):
                    self.select(yt_[:, r_, :], [(yl[:, sg, r_, :], self.oh_r[sg]) for sg in range(4)], [ylk], [ytk + "_%d" % r_])
            ybks = [ybk + "_%d" % r_ for r_ in range(4)]
            ycks = [yck + "_%d" % r_ for r_ in range(4)]
            rt, rk = rR.next()
            self.ld(rt[:], self.rT.ap().rearrange("(g p) t -> p g t", p=128)[:, :, sl], [rk], rk)
            gm, gmk = gmR.next()
            self.ld(gm[:], self.gmT.ap().rearrange("(g p) t -> p g t", p=128)[:, :, sl], [gmk], gmk)
            for g in range(4):
                cur, curk = u[:, g, :], uk
                bufs = [(sA, "sA"), (sB, "sB")]
                step = 1
                bi = 0
                while step < wins[g]:
                    nb, nbk = bufs[bi % 2]
                    bi += 1
                    self.tt("dve", nb[:, step:528], cur[:, step:528], cur[:, 0:528 - step], ALU.add, [curk], [nbk])
                    cur, curk = nb[:], nbk
                    step *= 2
                if tg == 0:
                    self.tt("dve", m1[:], cur[:, 16:528], ic0[:, g, :], ALU.mult, [curk, "pc"], ["m1"])
                else:
                    self.ts("dve", m1[:], cur[:, 16:528], 1.0 / wins[g], None, ALU.mult, None, [curk], ["m1"])
                self.tt("dve", pl[:, g, :], m1[:], u[:, g, 16:528], ALU.subtract, ["m1", uk], ["pl"])
            for g in range(4):
                ps, pk = ppR.next()
                self.mm(ps[:, :], Wp[:, g, :], pl[:, g, :], True, True, ["Wpool", "pl"], [pk])
                self.ts("dve", yaT[:, g, :], ps[:, :], self.vv("pscale")[:, g:g + 1], None, ALU.mult, None, [pk, "vec"], ["yaT"])
            self.tt("dve", ycg[:], yc[:], rt[:], ALU.mult, ycks + [rk], ["ycg"])
            ys = [(yaT, ["yaT"]), (yb, ybks), (ycg, ["ycg"])]
            for dc in range(8):
                dsl = slice(dc * 128, (dc + 1) * 128)
                pss = []
                for br in range(3):
                    ps, pk = ppR.next()
                    for wc in range(4):
                        self.mm(ps[:, :], Wb[:, br, wc, dsl], ys[br][0][:, wc, :], wc == 0, wc == 3, ["Wb%d" % br] + ys[br][1], [pk])
                    pss.append((ps, pk))
                self.tt("dve", m1[:], pss[0][0][:, :], gm[:, 0 * 8 + dc, :], ALU.mult, [pss[0][1], gmk], ["m1"])
                self.tt("dve", m2[:], pss[1][0][:, :], gm[:, 1 * 8 + dc, :], ALU.mult, [pss[1][1], gmk], ["m2"])
                self.tt("dve", m1[:], m1[:], m2[:], ALU.add, ["m1", "m2"], ["m1"])
                self.tt("dve", m2[:], pss[2][0][:, :], gm[:, 2 * 8 + dc, :], ALU.mult, [pss[2][1], gmk], ["m2"])
                self.tt("dve", mb[:, dc, :], m1[:], m2[:], ALU.add, ["m1", "m2"], ["mb"])
            for dc in range(8):
                dsl = slice(dc * 128, (dc + 1) * 128)
                ps, pk = ppR.next()
                for k in range(8):
                    self.mm(ps[:, :], Wo[:, k, dsl], mb[:, k, :], k == 0, k == 7, ["Wo", "mb"], [pk])
                self.tt("dve", h[:, dc, :], h[:, dc, :], ps[:, :], ALU.add, [hk, pk], [hk])
            self.stq(hdst[:, :, sl], h[:], [hk], hk + "s")

    def phase_C2(self, l):
        P, T = self.P, self.T
        NG = T // 512
        self.load_vec(l)
        W1 = P.sb("W1", [128, 8, 4 * D], BF16)
        W2 = P.sb("W2", [128, 32, D], BF16)
        P.dma([(lambda e, k=k: e.dma_start(out=W1[:, k, :], in_=self.w_ff1[l, k * 128:(k + 1) * 128, :])) for k in range(8)],
              (), ["W1"], key="W1", queue="pool")
        P.dma([(lambda e, k=k: e.dma_start(out=W2[:, k * 8:(k + 1) * 8, :], in_=self.w_ff2[l, k * 1024:(k + 1) * 1024, :].rearrange("(c p) d -> p c d", p=128))) for k in range(4)],
              (), ["W2"], key="W2", queue="pool")
        hv = self.hT.ap().rearrange("(k p) t -> p k t", p=128)
        hR = Ring(P, "hF", 1, [128, 8, 512], F32)
        sqR = Ring(P, "sqF", 2, [128, 512], F32)
        rs = P.sb("rsF", [128, 512], F32)
        fT = P.sb("fT", [128, 8, 512], BF16)
        uT = P.sb("uF", [128, 32, 512], BF16)
        rl = Ring(P, "rl", 2, [128, 512], F32)
        ssps = P.ps("sspsF", [128, 512])
        ppR = Ring(P, "ppF", 4, [128, 512], F32, psum=True)
        for tg in range(NG):
            sl = slice(tg * 512, (tg + 1) * 512)
            h, hk = hR.next()
            self.ld(h[:], hv[:, :, sl], [hk], hk)
            self.rmsnorm(h, hk, "gffn", fT, "fT", sqR, ssps, rs)
            for fc in range(32):
                ps, pk = ppR.next()
                for k in range(8):
                    self.mm(ps[:, :], W1[:, k, fc * 128:(fc + 1) * 128], fT[:, k, :], k == 0, k == 7, ["W1", "fT"], [pk])
                r_, rk = rl.next()
                self.act(r_[:], ps[:, :], AF.Relu, [pk], [rk])
                self.tt("dve" if fc % 2 == 0 else "pool", uT[:, fc, :], r_[:], r_[:], ALU.mult, [rk], ["uF"])
            for dc in range(8):
                ps, pk = ppR.next()
                for fc in range(32):
                    self.mm(ps[:, :], W2[:, fc, dc * 128:(dc + 1) * 128], uT[:, fc, :], fc == 0, fc == 31, ["W2", "uF"], [pk])
                self.tt("dve", h[:, dc, :], h[:, dc, :], ps[:, :], ALU.add, [hk, pk], [hk])
            self.stq(hv[:, :, sl], h[:], [hk], hk + "s")

    def phase_C3(self, l):
        P, T = self.P, self.T
        NG = T // 512
        last = (l == self.depth - 1)
        self.load_vec(l)
        Wg = P.sb("Wg", [128, 8, D], BF16)
        self.ld(Wg[:], self.w_ple_gate[l].rearrange("(k p) d -> p k d", p=128), ["Wg"], "Wg", queue="pool")
        Wq = P.sb("Wq", [128, 2, D], BF16)
        self.ld(Wq[:], self.w_ple_proj[l].rearrange("(k p) d -> p k d", p=128), ["Wq"], "Wq", queue="pool")
        hv = self.hT.ap().rearrange("(k p) t -> p k t", p=128)
        ov = self.out.ap().rearrange("(k p) t -> p k t", p=128)
        pv = self.pT.ap()
        hR = Ring(P, "hP", 2, [128, 8, 512], F32)
        pR = Ring(P, "pP", 2, [128, 2, 512], BF16)
        sqR = Ring(P, "sqP", 2, [128, 512], F32)
        rs = P.sb("rsP", [128, 512], F32)
        nT = P.sb("nT", [128, 8, 512], BF16)
        gt = P.sb("gt", [128, 512], F32)
        m1 = P.sb("m1P", [128, 512], F32)
        oR = Ring(P, "oP", 2, [128, 8, 512], F32)
        ssps = P.ps("sspsP", [128, 512])
        ppR = Ring(P, "ppP", 4, [128, 512], F32, psum=True)
        for tg in range(NG):
            sl = slice(tg * 512, (tg + 1) * 512)
            h, hk = hR.next()
            self.ld(h[:], hv[:, :, sl], [hk], hk)
            pt, ptk = pR.next()
            self.ld(pt[:], pv[l].rearrange("(k p) t -> p k t", p=128)[:, :, sl], [ptk], ptk, queue="pool")
            self.rmsnorm(h, hk, "gple", nT, "nT", sqR, ssps, rs)
            for dc in range(8):
                dsl = slice(dc * 128, (dc + 1) * 128)
                ps, pk = ppR.next()
                for k in range(8):
                    self.mm(ps[:, :], Wg[:, k, dsl], nT[:, k, :], k == 0, k == 7, ["Wg", "nT"], [pk])
                self.act(gt[:], ps[:, :], AF.Sigmoid, [pk], ["gt"])
                ps2, pk2 = ppR.next()
                for k in range(2):
                    self.mm(ps2[:, :], Wq[:, k, dsl], pt[:, k, :], k == 0, k == 1, ["Wq", ptk], [pk2])
                self.tt("dve", m1[:], gt[:], ps2[:, :], ALU.mult, ["gt", pk2], ["m1P"])
                self.tt("dve", h[:, dc, :], h[:, dc, :], m1[:], ALU.add, [hk, "m1P"], [hk])
            if not last:
                self.stq(hv[:, :, sl], h[:], [hk], hk + "s")
            else:
                o, okk = oR.next()
                self.rmsnorm(h, hk, "gfin", None, None, sqR, ssps, rs)
                g = self.vv("gfin")
                for k in range(8):
                    self.stt(o[:, k, :], h[:, k, :], g[:, k:k + 1], rs[:], ALU.mult, ALU.mult, [hk, "rsP" if False else "rs", "vec"], [okk])
                self.stq(ov[:, :, sl], o[:], [okk], okk + "s")


def vec_pack_offsets():
    dummy = {
        "norm_mix": np.zeros((DEPTH, D), np.float32), "norm_ffn": np.zeros((DEPTH, D), np.float32),
        "norm_ple": np.zeros((DEPTH, D), np.float32), "norm_final": np.zeros((D,), np.float32),
        "pool_scale": np.zeros((DEPTH, 512), np.float32), "gla_norm": np.zeros((DEPTH, 128), np.float32),
        "cmp_pos_k": np.zeros((DEPTH, 32, 64), np.float32), "cmp_pos_v": np.zeros((DEPTH, 32, 64), np.float32),
    }
    return vec_pack(dummy, 0)


def make_in_maps(inp, S, depth=DEPTH):
    T = S // 4
    f = lambda a: np.ascontiguousarray(np.asarray(a, np.float32))
    x = np.asarray(inp["x"], np.float32)
    p = np.asarray(inp["p"], np.float32)
    pos = np.asarray(inp["positions"], np.int32)
    cf = f32_consts(S).array()
    cb = bf_consts(S).array()
    epat = epat_const(S)
    vecs = np.stack([vec_pack(inp, l).array() for l in range(depth)], 0)
    wga = np.zeros((depth, 32, 256), np.float32)
    wga[:, 0:16] = np.asarray(inp["gla_w_gate"], np.float32)[:depth]
    wga[:, 16] = np.asarray(inp["gla_b_gate"], np.float32)[:depth]
    shared = {
        "w_in": f(inp["w_in"][:depth]), "pool_w": f(inp["pool_w"][:depth]),
        "cmp_w_k": f(inp["cmp_w_k"][:depth]), "cmp_w_v": f(inp["cmp_w_v"][:depth]), "wga": wga,
        "w_branch": f(inp["w_branch"][:depth]), "w_out": f(inp["w_out"][:depth]),
        "w_ff1": f(inp["w_ff1"][:depth]), "w_ff2": f(inp["w_ff2"][:depth]),
        "w_ple_gate": f(inp["w_ple_gate"][:depth]), "w_ple_proj": f(inp["w_ple_proj"][:depth]),
        "vecs": f(vecs), "cf": cf, "cb": cb, "epat": epat,
    }
    maps = []
    wins = (2, 4, 8, 16)
    for c in range(8):
        b, seg = c // 4, c % 4
        sl = slice(seg * T, (seg + 1) * T)
        g, hp = seg // 2, seg % 2
        pc = np.zeros((128, NPC), np.float32)
        tglob = seg * T + np.arange(512)
        for gi in range(4):
            pc[:, 1 + gi * 512:1 + (gi + 1) * 512] = (1.0 / np.minimum(tglob + 1, wins[gi]))[None, :]
        pc[:, 2049 + seg] = 1.0
        pc[:, 2053 + g] = 1.0
        pc[:, 2055 + hp] = 1.0
        if seg > 0:
            pc[:, 2057 + seg - 1] = 1.0
        selg = np.zeros((32, 6, 65), np.float32)
        for br in range(3):
            for hh in range(2):
                selg[(4 * g + 2 * hp + hh) * 3 + br, br * 2 + hh, 64] = 1.0
        pc[0:32, 2061:2061 + 390] = selg.reshape(32, 390)
        pcb = np.zeros((128, 192), np.float32)
        dd = np.arange(64)
        pcb[g * 64 + dd, dd] = 1.0
        if seg < 2:
            pcb[seg * 64 + dd, 64 + dd] = 1.0
        else:
            pcb[(seg - 2) * 64 + dd, 128 + dd] = 1.0
        m = dict(shared)
        m["xT"] = np.ascontiguousarray(x[b, sl, :].T)
        m["pT"] = np.ascontiguousarray(np.transpose(p[:depth, b, sl, :], (0, 2, 1)))
        m["pos"] = np.ascontiguousarray(pos[b, sl][None, :])
        m["pc"] = pc
        m["pcb"] = pcb
        maps.append(m)
    return maps


_CACHE = {}


def run(inp, S, depth=DEPTH, debug=False, stop_after=None, trace=False):
    key = (S, depth, debug, stop_after)
    if key not in _CACHE:
        _CACHE[key] = Builder(S, depth, debug, stop_after)
    bld = _CACHE[key]
    maps = make_in_maps(inp, S, depth)
    res = run_bass_kernel_spmd(bld.nc, maps, core_ids=list(range(8)), trace=trace) if trace else \
        run_bass_kernel_spmd(bld.nc, maps, core_ids=list(range(8)))
    return res


def kernel(**inputs):
    S = int(np.asarray(inputs["x"]).shape[1])
    T = S // 4
    res = run(inputs, S)
    out = np.zeros((2, S, D), np.float32)
    for c in range(8):
        b, seg = c // 4, c % 4
        out[b, seg * T:(seg + 1) * T, :] = np.asarray(res.results[c]["outT"]).T
    return out
```

```python
import numpy as np
import concourse.bass as bass
import concourse.mybir as mybir
from concourse.bass_utils import run_bass_kernel_spmd
from contextlib import ExitStack

F32 = mybir.dt.float32
BF16 = mybir.dt.bfloat16
I32 = mybir.dt.int32
AF = mybir.ActivationFunctionType
ALU = mybir.AluOpType

D = 1024
DEPTH = 2
NEG = -30000.0
BIG = 1.0e4
EPS = 1e-6
INW = 6440
PI = float(np.pi)
NPC = 2061 + 6 * 65
ENGS = ("pe", "act", "dve", "pool", "sp")


class Op:
    __slots__ = ("eng", "fn", "deps", "stream", "inc", "count", "marked", "idx", "ninst")


class Prog:
    def __init__(self, nc):
        self.nc = nc
        self.g = ExitStack()
        self.sems = []
        self.semcnt = []
        self.ccsems = []
        self.nphase = 0
        self.begin()

    def begin(self):
        self.ops = []
        self.last_w = {}
        self.readers = {}
        self.last_dma = {}
        self.st = ExitStack()
        self.uses_pid = False
        self.pid = None
        self.npool = 0

    def gsb(self, name, shape, dt):
        return self.g.enter_context(self.nc.sbuf_tensor(name, list(shape), dt))

    def sb(self, name, shape, dt):
        return self.st.enter_context(self.nc.sbuf_tensor("%s_p%d" % (name, self.nphase), list(shape), dt))

    def ps(self, name, shape, dt=F32):
        return self.st.enter_context(self.nc.psum_tensor("%s_p%d" % (name, self.nphase), list(shape), dt))

    def _add(self, eng, fn, reads, writes, stream, inc, ninst=1):
        op = Op()
        op.eng, op.fn, op.stream, op.inc = eng, fn, stream, inc
        op.marked = False
        op.count = 0
        op.idx = len(self.ops)
        op.ninst = ninst
        deps = set()
        for r in reads:
            w = self.last_w.get(r)
            if w is not None:
                deps.add(w)
        for w_ in writes:
            w = self.last_w.get(w_)
            if w is not None:
                deps.add(w)
            for ri in self.readers.get(w_, {}).values():
                deps.add(ri)
        op.deps = deps
        for r in reads:
            self.readers.setdefault(r, {})[stream] = op.idx
        for w_ in writes:
            self.last_w[w_] = op.idx
            self.readers[w_] = {}
        self.ops.append(op)
        return op

    def op(self, eng, fn, reads=(), writes=()):
        return self._add(eng, fn, reads, writes, ("eng", eng), 1)

    def dma(self, fns, reads=(), writes=(), key=None, queue="sp", inc=16):
        if not isinstance(fns, (list, tuple)):
            fns = [fns]
        op = self._add(queue, fns, reads, writes, ("dma", key), inc, ninst=len(fns))
        prev = self.last_dma.get(key)
        if prev is not None:
            op.deps.add(prev)
        self.last_dma[key] = op.idx
        op.marked = True
        return op

    def end(self):
        nc = self.nc
        ops = self.ops
        last_eng = {}
        for op in ops:
            for d in op.deps:
                ops[d].marked = True
            if op.stream[0] == "eng":
                last_eng[op.eng] = op
        for op in last_eng.values():
            op.marked = True
        sidx = {}
        counts = {}
        for op in ops:
            if op.marked:
                if op.stream not in sidx:
                    if op.stream[0] == "dma" and str(op.stream[1]).startswith("cc"):
                        self.ccsems.append(self.g.enter_context(nc.semaphore("ccsem%d" % len(self.ccsems))))
                        sidx[op.stream] = -len(self.ccsems)
                        counts[op.stream] = 0
                    else:
                        i = self.npool
                        self.npool += 1
                        sidx[op.stream] = i
                        while len(self.sems) <= i:
                            self.sems.append(self.g.enter_context(nc.semaphore("sem%d" % len(self.sems))))
                            self.semcnt.append(0)
                        counts[op.stream] = self.semcnt[i]
                counts[op.stream] += op.inc * op.ninst
                op.count = counts[op.stream]
        per_eng = {e: [] for e in ENGS}
        for op in ops:
            per_eng[op.eng].append(op)
        sems = {s: (self.sems[i] if i >= 0 else self.ccsems[-i - 1]) for s, i in sidx.items()}
        final = dict(counts)

        def run(eng_name, eng):
            waited = {}
            if eng_name == "sp" and self.uses_pid:
                self.pid = eng.partition_id()
            for op in per_eng[eng_name]:
                need = {}
                for d in op.deps:
                    p = ops[d]
                    if p.stream == ("eng", "pe") and eng_name == "pe" and op.stream[0] == "eng":
                        continue
                    if need.get(p.stream, 0) < p.count:
                        need[p.stream] = p.count
                for s, c in need.items():
                    if waited.get(s, 0) < c:
                        eng.wait_ge(sems[s], c)
                        waited[s] = c
                if op.stream[0] == "dma":
                    for f in op.fn:
                        f(eng).then_inc(sems[op.stream], op.inc)
                else:
                    ins = op.fn(eng)
                    if op.marked:
                        ins.then_inc(sems[op.stream], 1)
            for s, c in final.items():
                if waited.get(s, 0) < c:
                    eng.wait_ge(sems[s], c)

        with nc.Block() as block:
            @block.tensor
            def _(e):
                run("pe", e)

            @block.scalar
            def _(e):
                run("act", e)

            @block.vector
            def _(e):
                run("dve", e)

            @block.gpsimd
            def _(e):
                run("pool", e)

            @block.sync
            def _(e):
                run("sp", e)
        for s, i in sidx.items():
            if i >= 0:
                self.semcnt[i] = final[s]
        self.st.close()
        self.nphase += 1
        self.begin()

    def finish(self):
        self.g.close()


def _o(c, expr):
    return (expr + c) if c else expr


class Ring:
    def __init__(self, P, name, n, shape, dt, psum=False):
        self.n = n
        self.i = 0
        self.t = [(P.ps if psum else P.sb)("%s%d" % (name, j), shape, dt) for j in range(n)]
        self.k = ["%s%d" % (name, j) for j in range(n)]

    def next(self):
        j = self.i % self.n
        self.i += 1
        return self.t[j], self.k[j]


class Pack:
    def __init__(self):
        self.items = []
        self.off = {}
        self.n = 0

    def add(self, name, a):
        a = np.asarray(a, np.float32)
        rows = a.shape[0]
        flat = a.reshape(rows, -1)
        buf = np.zeros((128, flat.shape[1]), np.float32)
        buf[:rows] = flat
        self.off[name] = (self.n, rows, a.shape[1:])
        self.n += flat.shape[1]
        self.items.append(buf)

    def array(self):
        return np.ascontiguousarray(np.concatenate(self.items, axis=1))


def f32_consts(S):
    NSEL = S // 64
    p = Pack()
    j = np.arange(128)[:, None]
    i = np.arange(128)[None, :]
    same = (j // 64) == (i // 64)
    p.add("ident", np.eye(128))
    p.add("ones", np.ones((128, 128)))
    p.add("tri", (same & (j <= i)).astype(np.float32))
    p.add("m3", -(same & (j > i)).astype(np.float32))
    hi = (np.arange(128) >= 64).astype(np.float32)
    x = np.arange(2 * NSEL)[None, :]
    d = x - NSEL - hi[:, None]
    at = np.where(d > 0, -BIG, np.where(d >= -1, BIG, 0.0))
    p.add("at", at)
    half = 8
    invf = np.power(np.float32(500000.0), -np.arange(half, dtype=np.float32) * np.float32(2.0 / 16)).astype(np.float32)
    p.add("invf", np.concatenate([invf, invf])[:, None])
    p.add("sgn", np.concatenate([-np.ones(8), np.ones(8)])[:, None])
    rsw = np.zeros((64, 16), np.float32)
    for ii in range(16):
        rsw[(ii + 8) % 16, ii] = 1.0
    p.add("rsw", rsw)
    selg = np.zeros((32, 6, 65), np.float32)
    for br in range(3):
        for hh in range(2):
            selg[hh * 3 + br, br * 2 + hh, 64] = 1.0
    p.add("selg", selg)
    return p


def bf_consts(S):
    NM = S // 16
    NMT = max(1, NM // 128)
    NSEL = S // 64
    p = Pack()
    j = np.arange(128)[:, None]
    t = np.arange(128)[None, :]
    p.add("identb", np.eye(128))
    p.add("onesb", np.ones((128, 8)))
    c = np.where(j <= t, 0.0, NEG)
    p.add("causal2", np.stack([c, c], 1))
    a = np.where(j > t, 0.0, NEG)
    p.add("antic2", np.stack([a, a], 1))
    fl = np.floor((np.arange(128) - 31) / 16.0)[None, :]
    cm = np.zeros((128, 17, 2, 128), np.float32)
    for o in range(17):
        m = np.where(j - fl <= 8 * o, 0.0, NEG)
        cm[:, o, 0] = m
        cm[:, o, 1] = m
    p.add("cmpmask", cm)
    n = np.arange(NMT * 128)
    ncmp = NM - 1
    bs = n * 16
    ss = np.arange(NSEL) * 64
    ov = ((bs[:, None] < ss[None, :] + 64) & (bs[:, None] + 32 > ss[None, :]) & (n[:, None] < ncmp)).astype(np.float32)
    p.add("ov", ov.reshape(NMT, 128, NSEL).transpose(1, 0, 2))
    return p


def epat_const(S):
    key = np.arange(S)
    r = np.arange(64)[:, None]
    return (((key // 64) % 64)[None, :] == r).astype(np.float32)


VEC_ITEMS = ["gmix", "gffn", "gple", "gfin", "pscale", "gnorm", "posFk", "posGk", "posFv", "posGv"]


def vec_pack(inp, l):
    p = Pack()
    fm = lambda v: np.asarray(v, np.float32).reshape(-1, 128).T
    p.add("gmix", fm(inp["norm_mix"][l]))
    p.add("gffn", fm(inp["norm_ffn"][l]))
    p.add("gple", fm(inp["norm_ple"][l]))
    p.add("gfin", fm(inp["norm_final"]))
    p.add("pscale", fm(inp["pool_scale"][l]))
    p.add("gnorm", np.asarray(inp["gla_norm"][l], np.float32)[:, None])
    p.add("posk", np.asarray(inp["cmp_pos_k"][l], np.float32).T)
    p.add("posv", np.asarray(inp["cmp_pos_v"][l], np.float32).T)
    return p


class Builder:
    def __init__(self, S, depth=DEPTH, debug=False, stop_after=None):
        self.S = S
        self.T = S // 4
        self.depth = depth
        self.debug = debug
        self.stop_after = stop_after
        self.nc = bass.Bass("TRN2", target_bir_lowering=False)
        self.P = Prog(self.nc)
        self.cf_off = f32_consts(S)
        self.cb_off = bf_consts(S)
        self.build()

    def din(self, name, shape, dt=F32):
        return self.nc.dram_tensor(name, list(shape), dt, kind="ExternalInput")

    def scratch(self, name, shape, dt, collective=False):
        if self.debug and not collective:
            return self.nc.dram_tensor(name, list(shape), dt, kind="ExternalOutput")
        return self.nc.dram_tensor(name, list(shape), dt)

    def xf(self, r0, n):
        i = r0 // self.CR
        assert (r0 + n - 1) // self.CR == i
        lo = r0 - i * self.CR
        return self.XFc[i][lo:lo + n, :]

    def gf(self, s, blk):
        r0 = blk * 128
        i = r0 // self.CR
        rows = min(self.CR, 1280 - i * self.CR)
        lo = r0 - i * self.CR
        return self.GFc[i][s * rows + lo:s * rows + lo + 128, :]

    def mm(self, out, lhsT, rhs, start, stop, rd, wr):
        self.P.op("pe", lambda e: e.matmul(out, lhsT=lhsT, rhs=rhs, start=start, stop=stop), rd, wr)

    def tp(self, out, in_, ident, rd, wr):
        self.P.op("pe", lambda e: e.transpose(out=out, in_=in_, identity=ident), rd, wr)

    def act(self, out, in_, func, rd, wr, scale=None, bias=None):
        kw = {}
        if scale is not None:
            kw["scale"] = scale
        if bias is not None:
            kw["bias"] = bias
        self.P.op("act", lambda e: e.activation(out=out, in_=in_, func=func, **kw), rd, wr)

    def tt(self, eng, out, in0, in1, op, rd, wr):
        self.P.op(eng, lambda e: e.tensor_tensor(out=out, in0=in0, in1=in1, op=op), rd, wr)

    def ts(self, eng, out, in0, s1, s2, op0, op1, rd, wr):
        if op1 is None:
            self.P.op(eng, lambda e: e.tensor_scalar(out=out, in0=in0, scalar1=s1, scalar2=None, op0=op0), rd, wr)
        else:
            self.P.op(eng, lambda e: e.tensor_scalar(out=out, in0=in0, scalar1=s1, scalar2=s2, op0=op0, op1=op1), rd, wr)

    def stt(self, out, in0, scalar, in1, op0, op1, rd, wr):
        self.P.op("dve", lambda e: e.scalar_tensor_tensor(out=out, in0=in0, scalar=scalar, in1=in1, op0=op0, op1=op1), rd, wr)

    def cp(self, eng, out, in_, rd, wr):
        if eng == "act":
            self.P.op("act", lambda e: e.copy(out=out, in_=in_), rd, wr)
        else:
            self.P.op(eng, lambda e: e.tensor_copy(out=out, in_=in_), rd, wr)

    def memset(self, eng, ap, val, wr):
        self.P.op(eng, lambda e: e.memset(ap, val), (), wr)

    def ld(self, out, in_, wr, key, queue="sp", rd=()):
        self.P.dma(lambda e: e.dma_start(out=out, in_=in_), rd, wr, key=key, queue=queue)

    def stq(self, out, in_, rd, key, wr=()):
        self.P.dma(lambda e: e.dma_start(out=out, in_=in_), rd, wr, key=key, queue="pool")

    def dyn(self, mk, wr, key, rd=()):
        self.P.uses_pid = True
        self.P.dma(lambda e: mk(e, self.P.pid), rd, wr, key=key, queue="sp")

    def cfv(self, name):
        off, rows, shp = self.cf_off.off[name]
        n = int(np.prod(shp))
        ap = self.cf[0:rows, off:off + n]
        if len(shp) == 2:
            ap = ap.rearrange("p (a b) -> p a b", b=shp[1])
        return ap

    def cbv(self, name):
        off, rows, shp = self.cb_off.off[name]
        n = int(np.prod(shp))
        ap = self.cb[0:rows, off:off + n]
        if len(shp) == 2:
            ap = ap.rearrange("p (a b) -> p a b", b=shp[1])
        elif len(shp) == 3:
            ap = ap.rearrange("p (a b c) -> p a b c", b=shp[1], c=shp[2])
        return ap

    def vv(self, name):
        off, rows, shp = self.vec_off.off[name]
        n = int(np.prod(shp))
        return self.vec[0:rows, off:off + n]

    def build(self):
        nc, P, S, T = self.nc, self.P, self.S, self.T
        depth = self.depth
        self.xT = self.din("xT", [D, T])
        self.pT = self.din("pT", [depth, 256, T])
        self.pos = self.din("pos", [1, T], I32)
        self.w_in = self.din("w_in", [depth, D, INW])
        self.pool_w = self.din("pool_w", [depth, 4, 128, 128])
        self.cmp_w_k = self.din("cmp_w_k", [depth, 2048, 64])
        self.cmp_w_v = self.din("cmp_w_v", [depth, 2048, 64])
        self.wga = self.din("wga", [depth, 32, 256])
        self.w_branch = self.din("w_branch", [depth, 3, 512, D])
        self.w_out = self.din("w_out", [depth, D, D])
        self.w_ff1 = self.din("w_ff1", [depth, D, 4 * D])
        self.w_ff2 = self.din("w_ff2", [depth, 4 * D, D])
        self.w_ple_gate = self.din("w_ple_gate", [depth, D, D])
        self.w_ple_proj = self.din("w_ple_proj", [depth, 256, D])
        self.vec_off = vec_pack_offsets()
        self.vecs = self.din("vecs", [depth, 128, self.vec_off.n])
        self.cf_in = self.din("cf", [128, self.cf_off.n])
        self.cb_in = self.din("cb", [128, self.cb_off.n])
        self.epat_in = self.din("epat", [64, S])
        self.pc_in = self.din("pc", [128, NPC])
        self.pcb_in = self.din("pcb", [128, 192])
        self.out = nc.dram_tensor("outT", [D, T], F32, kind="ExternalOutput")
        sc = self.scratch
        self.hT = sc("hT", [D, T], F32)
        self.uT = sc("uT", [512, T], F32)
        self.rT = sc("rT", [512, T], BF16)
        self.gmT = sc("gmT", [3 * D, T], BF16)
        self.tabs = sc("tabs", [2, 16, T], F32)
        self.CR = max(128, min(1024, (524288 // T) // 128 * 128))
        self.XFc, self.GFc = [], []
        r = 0
        while r < 1280:
            n = min(self.CR, 1280 - r)
            self.XFc.append(sc("X_F%d" % len(self.XFc), [n, T], BF16, True))
            self.GFc.append(sc("G_F%d" % len(self.GFc), [4 * n, T], BF16, True))
            r += n
        self.XTc = [sc("X_T%d" % j, [512, 1024], BF16, True) for j in range(T // 512)]
        self.GTc = [sc("G_T%d" % j, [4 * 512, 1024], BF16, True) for j in range(T // 512)]
        self.X_fg = sc("X_fg", [512, T // 16], F32, True)
        self.X_gn = sc("X_gn", [32, T], F32, True)
        self.X_ge = sc("X_ge", [256, T // 64], F32, True)
        self.X_u = sc("X_u", [512, 16], F32, True)
        self.G_fg = sc("G_fg", [4 * 512, T // 16], F32, True)
        self.G_gn = sc("G_gn", [4 * 32, T], F32, True)
        self.G_ge = sc("G_ge", [4 * 256, T // 64], F32, True)
        self.G_u = sc("G_u", [4 * 512, 16], F32, True)
        self.Yc = [[sc("Y%d_%d" % (bc, sg), [128, T], BF16, True) for sg in range(4)] for bc in range(2)]
        self.GYc = [[sc("G_Y%d_%d" % (bc, sg), [4 * 128, T], BF16, True) for sg in range(4)] for bc in range(2)]
        if self.debug:
            self.dbgY = nc.dram_tensor("dbgY", [8, 4 * 128, T], BF16, kind="ExternalOutput")
            self.dbgXF = nc.dram_tensor("dbgXF", [1280, T], BF16, kind="ExternalOutput")
            self.dbgXT = nc.dram_tensor("dbgXT", [T, 1024], BF16, kind="ExternalOutput")
            self.dbgfg = nc.dram_tensor("dbgfg", [512, T // 16], F32, kind="ExternalOutput")
            self.dbggn = nc.dram_tensor("dbggn", [32, T], F32, kind="ExternalOutput")
            self.dbgge = nc.dram_tensor("dbgge", [256, T // 64], F32, kind="ExternalOutput")
        self.cf = P.gsb("cf_sb", [128, self.cf_off.n], F32)
        self.epsb = P.gsb("epsb", [128, 1], F32)

        self.phase_setup()
        stages = []
        for l in range(depth):
            stages += [("A", l), ("AG1", l), ("NSA", l), ("GLA", l), ("AG2", l), ("C1", l), ("C2", l), ("C3", l)]
        for nm, l in stages:
            getattr(self, "phase_" + nm)(l)
            P.end()
            if self.stop_after == (nm, l):
                break
        P.finish()

    def phase_setup(self):
        P, T = self.P, self.T
        self.ld(self.cf[:], self.cf_in[:, :], ["cf"], "cf")
        self.memset("dve", self.epsb[:], EPS, ["epsb"])
        posi = P.sb("posi", [16, 512], I32)
        ang = P.sb("ang", [16, 512], F32)
        red = P.sb("red", [16, 512], F32)
        xs = P.sb("xs", [16, 512], F32)
        ki = P.sb("ki", [16, 512], I32)
        tb = P.sb("tb", [16, 2, 512], F32)
        invf = self.cfv("invf")
        sgn = self.cfv("sgn")
        for c in range(T // 512):
            sl = slice(c * 512, (c + 1) * 512)
            self.ld(posi[:], self.pos[0:1, sl].partition_broadcast(16), ["posi"], "posi")
            self.cp("dve", ang[:], posi[:], ["posi"], ["ang"])
            self.ts("dve", ang[:], ang[:], invf[:, 0:1], None, ALU.mult, None, ["ang", "cf"], ["ang"])
            C1, C2 = 6.28125, 2 * PI - 6.28125
            for a_, shift in ((0, 0.5 * PI), (1, 0.0)):
                self.ts("dve", xs[:], ang[:], shift, None, ALU.add, None, ["ang"], ["xs"])
                self.ts("dve", red[:], xs[:], 1.0 / (2 * PI), None, ALU.mult, None, ["xs"], ["red"])
                self.cp("dve", ki[:], red[:], ["red"], ["ki"])
                self.cp("dve", red[:], ki[:], ["ki"], ["red"])
                self.stt(xs[:], red[:], -C1, xs[:], ALU.mult, ALU.add, ["red", "xs"], ["xs"])
                self.stt(xs[:], red[:], -C2, xs[:], ALU.mult, ALU.add, ["red", "xs"], ["xs"])
                self.ts("dve", red[:], xs[:], PI, -2 * PI, ALU.is_gt, ALU.mult, ["xs"], ["red"])
                self.tt("dve", xs[:], xs[:], red[:], ALU.add, ["xs", "red"], ["xs"])
                self.ts("dve", red[:], xs[:], -PI, 2 * PI, ALU.is_lt, ALU.mult, ["xs"], ["red"])
                self.tt("dve", xs[:], xs[:], red[:], ALU.add, ["xs", "red"], ["xs"])
                self.act(tb[:, a_, :], xs[:], AF.Sin, ["xs"], ["tb"])
            self.ts("dve", tb[:, 1, :], tb[:, 1, :], sgn[:, 0:1], None, ALU.mult, None, ["tb", "cf"], ["tb"])
            self.stq(self.tabs.ap().rearrange("a p t -> p a t")[:, 0:2, sl], tb[:], ["tb"], "tb_st")
        P.end()

    def load_vec(self, l):
        P = self.P
        self.vec = P.sb("vec", [128, self.vec_off.n], F32)
        self.ld(self.vec[:], self.vecs[l, :, :], ["vec"], "vec")

    def load_pc(self):
        P = self.P
        self.pc = P.sb("pc", [128, NPC], F32)
        self.ld(self.pc[:], self.pc_in[:, :], ["pc"], "pc")
        self.pcb = P.sb("pcb", [128, 192], BF16)
        self.ld(self.pcb[:], self.pcb_in[:, :], ["pcb"], "pcb", queue="pool")
        self.oh_r = [self.pc[:, 2049 + i:2050 + i] for i in range(4)]
        self.oh_g = [self.pc[:, 2053 + i:2054 + i] for i in range(2)]
        self.oh_hp = [self.pc[:, 2055 + i:2056 + i] for i in range(2)]
        self.oh_prev = [self.pc[:, 2057 + i:2058 + i] for i in range(4)]

    def select(self, out, terms, rd, wr):
        (a0, s0) = terms[0]
        n = a0.shape[0]
        fix = lambda s: s if s.shape[0] == n else s[0:n, :]
        self.ts("dve", out, a0, fix(s0), None, ALU.mult, None, rd + ["pc"], wr)
        for a, s in terms[1:]:
            self.stt(out, a, fix(s), out, ALU.mult, ALU.add, rd + ["pc"] + wr, wr)

    def rmsnorm(self, h, hkey, gname, out, okey, sqR, ssps, rs):
        ones = self.cfv("ones")
        g = self.vv(gname)
        for k in range(8):
            sq, sk = sqR.next()
            self.act(sq[:], h[:, k, :], AF.Square, [hkey], [sk])
            self.mm(ssps[:, :], ones[:, :], sq[:], k == 0, k == 7, [sk, "cf"], ["ssps"])
        self.act(rs[:], ssps[:, :], AF.Ln, ["ssps"], ["rs"], scale=1.0 / D, bias=self.epsb[:, 0:1])
        self.act(rs[:], rs[:], AF.Exp, ["rs"], ["rs"], scale=-0.5)
        if out is not None:
            for k in range(8):
                self.stt(out[:, k, :], h[:, k, :], g[:, k:k + 1], rs[:], ALU.mult, ALU.mult, [hkey, "rs", "vec"], [okey])

    def phase_A(self, l):
        P, T = self.P, self.T
        NG = T // 512
        self.load_vec(l)
        Win = P.sb("Win", [128, 8, INW], BF16)
        P.dma([(lambda e, k=k: e.dma_start(out=Win[:, k, :], in_=self.w_in[l, k * 128:(k + 1) * 128, :])) for k in range(8)],
              (), ["Win"], key="Win", queue="pool")
        Wc = [P.sb("Wck", [64, 32, 64], BF16), P.sb("Wcv", [64, 32, 64], BF16)]
        self.ld(Wc[0][:], self.cmp_w_k[l].rearrange("(l d) o -> d l o", d=64), ["Wc0"], "Wc0", queue="pool")
        self.ld(Wc[1][:], self.cmp_w_v[l].rearrange("(l d) o -> d l o", d=64), ["Wc1"], "Wc1", queue="pool")
        wga = P.sb("wga", [32, 256], BF16)
        self.ld(wga[:], self.wga[l, :, :], ["wga"], "wga", queue="pool")
        hsrc = (self.xT if l == 0 else self.hT).ap().rearrange("(k p) t -> p k t", p=128)
        hR = Ring(P, "hA", 1, [128, 8, 512], F32)
        tabR = Ring(P, "tab", 1, [16, 2, 512], F32)
        sqR = Ring(P, "sq", 2, [128, 512], F32)
        rs = P.sb("rs", [128, 512], F32)
        aT = P.sb("aT", [128, 8, 512], BF16)
        qfR = Ring(P, "qf", 2, [64, 512], F32)
        t1 = P.sb("t1", [16, 512], F32)
        t2 = P.sb("t2", [16, 512], F32)
        obR = Ring(P, "ob", 3, [128, 512], BF16)
        kb = P.sb("kb", [64, 512], BF16)
        pb = P.sb("pb", [64, 2, 32], BF16)
        cbias = P.sb("cbias", [64, 4], F32)
        fgs = P.sb("fgs", [64, 2, 32], F32)
        gnb = P.sb("gnb", [24, 512], F32)
        ubR = Ring(P, "ub", 2, [128, 512], F32)
        rbR = Ring(P, "rb", 2, [128, 512], BF16)
        gmR = Ring(P, "gmb", 2, [128, 2, 512], BF16)
        glaug = P.sb("glaug", [32, 512], BF16)
        vswb = P.sb("vswb", [128, 4, 256], BF16)
        ktb = P.sb("ktb", [128, 4, 256], BF16)
        vgb = P.sb("vgb", [128, 4, 512], BF16)
        ez = P.sb("ez", [128, 256], F32)
        la = P.sb("la", [128, 256], F32)
        ekt = P.sb("ekt", [128, 256], F32)
        e1 = P.sb("e1", [128, 2, 512], F32)
        e2 = P.sb("e2", [128, 2, 512], F32)
        ebt = P.sb("ebt", [128, 2, 8], F32)
        qgf = P.sb("qgf", [128, 4, 512], F32)
        ppR = Ring(P, "pp", 2, [128, 512], F32, psum=True)
        ssps = P.ps("ssps", [128, 512])
        pmisc = P.ps("pmisc", [128, 512])
        tm = P.ps("tm", [128, 1024])
        pz = ssps
        lct = P.ps("lct", [128, 2, 512])
        ident = self.cfv("ident")
        tri = self.cfv("tri")
        m3 = self.cfv("m3")
        rsw = self.cfv("rsw")
        self.memset("dve", glaug[:], 1.0, ["glaug"])
        self.cp("dve", pb[:, 0, :], self.vv("posk"), ["vec"], ["pb"])
        self.cp("dve", pb[:, 1, :], self.vv("posv"), ["vec"], ["pb"])
        for kd in range(2):
            for FG in range(2):
                col = 256 + kd * 2 + FG
                for ll in range(16):
                    self.mm(pmisc[0:64, col:col + 1], Wc[kd][:, FG * 16 + ll, :], pb[:, kd, FG * 16 + ll:FG * 16 + ll + 1],
                            ll == 0, ll == 15, ["pb", "Wc%d" % kd], ["pmisc"])
        self.cp("act", cbias[:], pmisc[0:64, 256:260], ["pmisc"], ["cbias"])

        def proj(c0, M):
            ps, pk = ppR.next()
            for k in range(8):
                self.mm(ps[0:M, :], Win[:, k, c0:c0 + M], aT[:, k, :], k == 0, k == 7, ["aT", "Win"], [pk])
            return ps, pk

        for tg in range(NG):
            t0 = tg * 512
            sl = slice(t0, t0 + 512)
            h, hk = hR.next()
            self.ld(h[:], hsrc[:, :, sl], [hk], hk)
            tab, tk = tabR.next()
            self.ld(tab[:], self.tabs.ap().rearrange("a p t -> p a t")[:, :, sl], [tk], tk)
            self.rmsnorm(h, hk, "gmix", aT, "aT", sqR, ssps, rs)
            heads = [(512 + 64 * hh, "q", 64 * hh) for hh in range(8)]
            for g in range(2):
                heads.append((1024 + 0 * 128 + 64 * g, "kc", g))
                heads.append((1024 + 2 * 128 + 64 * g, "k", 512 + 0 * 128 + 64 * g))
                heads.append((1024 + 4 * 128 + 64 * g, "k", 512 + 1 * 128 + 64 * g))
                heads.append((1024 + 1 * 128 + 64 * g, "vc", g))
            for c0, kind, dst in heads:
                ps, pk = proj(c0, 64)
                qf, qk = qfR.next()
                self.cp("act", qf[:], ps[0:64, :], [pk], [qk])
                if kind != "vc":
                    self.mm(pmisc[0:16, :], rsw[:, :], qf[:], True, True, [qk, "cf"], ["pmisc"])
                    self.tt("dve", t1[:], qf[0:16, :], tab[:, 0, :], ALU.mult, [qk, tk], ["t1"])
                    self.tt("dve", t2[:], pmisc[0:16, :], tab[:, 1, :], ALU.mult, ["pmisc", tk], ["t2"])
                    self.tt("dve", qf[0:16, :], t1[:], t2[:], ALU.add, ["t1", "t2"], [qk])
                if kind == "q" or kind == "k":
                    ob, ok = obR.next()
                    self.ts("dve", ob[0:64, :], qf[:], 0.125 if kind == "q" else 1.0, None, ALU.mult, None, [qk], [ok])
                    self.stq(self.xf(dst, 64)[:, sl], ob[0:64, :], [ok], ok + "s")
                else:
                    kd = 0 if kind == "kc" else 1
                    self.cp("dve", kb[:], qf[:], [qk], ["kb"])
                    for FG in range(2):
                        for ll in range(16):
                            self.mm(pmisc[0:64, 64 + FG * 32:64 + FG * 32 + 32], Wc[kd][:, FG * 16 + ll, :], kb[:, ll:512:16],
                                    ll == 0, ll == 15, ["kb", "Wc%d" % kd], ["pmisc"])
                        self.act(fgs[:, FG, :], pmisc[0:64, 64 + FG * 32:64 + FG * 32 + 32], AF.Identity, ["pmisc", "cbias"], ["fgs"],
                                 bias=cbias[:, kd * 2 + FG:kd * 2 + FG + 1])
                    r0 = (kd * 2 + dst) * 128
                    self.stq(self.X_fg[r0:r0 + 128, tg * 32:(tg + 1) * 32].rearrange("(f d) m -> d f m", d=64), fgs[:], ["fgs"], "fgs_s")
            ps, pk = proj(1792, 24)
            self.act(gnb[:], ps[0:24, :], AF.Sigmoid, [pk], ["gnb"])
            self.stq(self.X_gn[0:24, sl], gnb[:], ["gnb"], "gnb_s")
            for c in range(4):
                ps, pk = proj(c * 128, 128)
                ub, ubk = ubR.next()
                self.cp("act", ub[:], ps[:, :], [pk], [ubk])
                self.stq(self.uT[c * 128:(c + 1) * 128, sl], ub[:], [ubk], ubk + "s")
                if tg == NG - 1:
                    self.stq(self.X_u[c * 128:(c + 1) * 128, :], ub[:, 496:512], [ubk], ubk + "s")
            for c in range(4):
                ps, pk = proj(2856 + c * 128, 128)
                rb, rbk = rbR.next()
                self.act(rb[:], ps[:, :], AF.Silu, [pk], [rbk])
                self.stq(self.rT[c * 128:(c + 1) * 128, sl], rb[:], [rbk], rbk + "s")
            for c2 in range(12):
                gmb, gk = gmR.next()
                for c in range(2):
                    ps, pk = proj(3368 + (c2 * 2 + c) * 128, 128)
                    self.act(gmb[:, c, :], ps[:, :], AF.Sigmoid, [pk], [gk])
                self.stq(self.gmT.ap().rearrange("(g p) t -> p g t", p=128)[:, c2 * 2:(c2 + 1) * 2, sl], gmb[:], [gk], gk + "s")
            ps, pk = proj(2840, 16)
            self.cp("act", glaug[0:16, :], ps[0:16, :], [pk], ["glaug"])
            for c in range(4):
                ps, pk = proj(1816 + c * 128, 128)
                self.cp("act", qgf[:, c, :], ps[:, :], [pk], ["qgf"])
            for tt_ in range(4):
                ts_ = slice(tt_ * 128, (tt_ + 1) * 128)
                for c0, n, o in ((1024 + 384, 128, 0), (1024 + 640, 128, 128), (2072, 256, 256), (2328, 512, 512)):
                    for k in range(8):
                        self.mm(tm[:, o:o + n], aT[:, k, ts_], Win[:, k, c0:c0 + n], k == 0, k == 7, ["aT", "Win"], ["tm"])
                self.cp("act", vswb[:, tt_, :], tm[:, 0:256], ["tm"], ["vswb"])
                self.cp("act", vgb[:, tt_, :], tm[:, 512:1024], ["tm"], ["vgb"])
                self.mm(pz[:, 0:256], glaug[:, ts_], wga[:, :], True, True, ["glaug", "wga"], ["ssps"])
                self.act(ez[:], pz[:, 0:256], AF.Exp, ["ssps"], ["ez"], scale=-1.0)
                self.act(la[:], ez[:], AF.Ln, ["ez"], ["la"], bias=1.0)
                self.mm(pz[:, 256:512], m3[:, :], la[:], True, True, ["la", "cf"], ["ssps"])
                self.act(ekt[:], pz[:, 256:512], AF.Exp, ["ssps"], ["ekt"], scale=1.0 / 16)
                self.tt("dve", ktb[:, tt_, :], tm[:, 256:512], ekt[:], ALU.mult, ["tm", "ekt"], ["ktb"])
                for fc in range(2):
                    self.mm(lct[:, fc, ts_], la[:, fc * 128:(fc + 1) * 128], tri[:, :], True, True, ["la", "cf"], ["lct"])
            xt = self.XTc[tg].ap().rearrange("(tt p) c -> p tt c", p=128)
            self.stq(xt[:, :, 0:256], vswb[:], ["vswb"], "vswb_s")
            self.stq(xt[:, :, 256:512], ktb[:], ["ktb"], "ktb_s")
            self.stq(xt[:, :, 512:1024], vgb[:], ["vgb"], "vgb_s")
            self.act(e1[:], lct[:], AF.Exp, ["lct"], ["e1"], scale=-1.0 / 16)
            self.act(e2[:], lct[:], AF.Exp, ["lct"], ["e2"], scale=1.0 / 16)
            for fc in range(2):
                ob, ok = obR.next()
                self.stt(ob[:, :], qgf[:, fc, :], 0.125, e1[:, fc, :], ALU.mult, ALU.mult, ["qgf", "e1"], [ok])
                self.stq(self.xf(768 + fc * 128, 128)[:, sl], ob[:, :], [ok], ok + "s")
                ob, ok = obR.next()
                self.tt("dve", ob[:, :], qgf[:, 2 + fc, :], e2[:, fc, :], ALU.mult, ["qgf", "e2"], [ok])
                self.stq(self.xf(1024 + fc * 128, 128)[:, sl], ob[:, :], [ok], ok + "s")
            self.cp("dve", ebt[:], e1[:, :, 63:512:64], ["e1"], ["ebt"])
            self.stq(self.X_ge.ap().rearrange("(f p) c -> p f c", p=128)[:, :, tg * 8:(tg + 1) * 8], ebt[:], ["ebt"], "ebt_s")

    def phase_AG1(self, l):
        P = self.P
        grp = [[0, 1, 2, 3], [4, 5, 6, 7]]
        pairs = list(zip(self.XFc, self.GFc)) + list(zip(self.XTc, self.GTc)) + [(self.X_fg, self.G_fg), (self.X_gn, self.G_gn),
                 (self.X_ge, self.G_ge), (self.X_u, self.G_u)]
        import os
        only = os.environ.get("KDBG_AG")
        for i, (a, b) in enumerate(pairs):
            if only is not None and str(i) not in only.split(","):
                continue
            P.dma(lambda e, a=a, b=b: e.collective_compute("AllGather", ALU.bypass, replica_groups=grp,
                                                           ins=[a.ap().opt()], outs=[b.ap().opt()]),
                  (), (), key="cc%d" % i, queue="pool", inc=1)
        if self.debug and l == 0 and os.environ.get("KDBG_COPY", "1") == "1":
            cps = [(self.X_fg.ap(), self.dbgfg.ap()), (self.X_gn.ap(), self.dbggn.ap()), (self.X_ge.ap(), self.dbgge.ap())]
            r = 0
            for c_ in self.XFc:
                n = c_.shape[0]
                cps.append((c_.ap(), self.dbgXF[r:r + n, :]))
                r += n
            for j, c_ in enumerate(self.XTc):
                cps.append((c_.ap(), self.dbgXT[j * 512:(j + 1) * 512, :]))
            for i, (a, b) in enumerate(cps):
                P.dma(lambda e, a=a, b=b: e.dma_start(out=b, in_=a), (), (), key="dbgc%d" % i, queue="sp")

    def phase_AG2(self, l):
        P = self.P
        grp = [[0, 1, 2, 3], [4, 5, 6, 7]]
        for bc in range(2):
            for sg in range(4):
                a, b = self.Yc[bc][sg], self.GYc[bc][sg]
                P.dma(lambda e, a=a, b=b: e.collective_compute("AllGather", ALU.bypass, replica_groups=grp,
                                                               ins=[a.ap().opt()], outs=[b.ap().opt()]),
                      (), ["G_Y%d%d" % (bc, sg)], key="ccY%d%d" % (bc, sg), queue="pool", inc=1)
                if self.debug and l == 0:
                    P.dma(lambda e, b=b, bc=bc, sg=sg: e.dma_start(out=self.dbgY[bc * 4 + sg, :, :], in_=b[:, :]), ["G_Y%d%d" % (bc, sg)], (),
                          key="dbgY%d%d" % (bc, sg), queue="sp")

    def phase_NSA(self, l):
        P, S, T = self.P, self.S, self.T
        NKT = S // 128
        NM = S // 16
        NMT = max(1, NM // 128)
        NSEL = S // 64
        NG4 = (NSEL + 63) // 64
        QPS = T // 128
        KsA = P.sb("KsA", [128, S], BF16)
        self.cb = P.sb("cb", [128, self.cb_off.n], BF16)
        self.ld(self.cb[:], self.cb_in[:, :], ["cb"], "cb", queue="pool")
        self.ld(KsA[64:128, :], self.epat_in[:, :], ["KsA"], "KsAe", queue="pool")
        KwT = P.sb("KwT", [64, S], BF16)
        Vs = P.sb("Vs", [128, NKT, 65], BF16)
        Vw = P.sb("Vw", [128, NKT, 65], BF16)
        Vc = P.sb("Vc", [128, NMT, 65], BF16)
        FG = [P.sb("FGs%d" % i, [64, NMT * 128], F32) for i in range(4)]
        KcA = P.sb("KcA", [64, NMT * 128], BF16)
        VcT = P.sb("VcT", [64, NMT * 128], F32)
        identf = self.cfv("ident")
        onesf = self.cfv("ones")
        at = self.cfv("at")
        identb = self.cbv("identb")
        onesb = self.cbv("onesb")
        causal2 = self.cbv("causal2")
        antic2 = self.cbv("antic2")
        cmpmask = self.cbv("cmpmask")
        ov = self.cbv("ov")
        G_fg, G_gn = self.G_fg, self.G_gn

        self.load_pc()
        selP = self.pcb[:, 0:64]
        kkR = Ring(P, "kk", 2, [128, 2, 512], BF16)
        vaR = Ring(P, "va", 2, [128, 4, 256], BF16)
        fgR = Ring(P, "fga", 1, [64, 8, T // 16], F32)
        pS = P.ps("pS", [128, 3, 512])
        pSs = pS
        nsel = 0
        for s in range(4):
            for kc in range(T // 512):
                c0 = s * T + kc * 512
                kk, kkk = kkR.next()
                P.dma([(lambda e, ty=ty, kk=kk, s=s, kc=kc: e.dma_start(out=kk[:, ty, :], in_=self.gf(s, 4 + ty)[:, kc * 512:(kc + 1) * 512])) for ty in range(2)],
                      (), [kkk], key=kkk)
                for ty, (dst, dk) in enumerate(((KsA, "KsA"), (KwT, "KwT"))):
                    self.mm(pSs[0:64, ty, :], selP, kk[:, ty, :], True, True, [kkk, "pcb"], ["pS%d" % ty])
                    self.cp("act" if (nsel % 2 == 0) else "dve", dst[0:64, c0:c0 + 512], pSs[0:64, ty, :], ["pS%d" % ty], [dk])
                    nsel += 1
            for j in range(T // 512):
                va, vak = vaR.next()
                self.ld(va[:], self.GTc[j][s * 512:(s + 1) * 512, 0:256].rearrange("(kt p) c -> p kt c", p=128), [vak], vak)
                k0 = s * QPS + 4 * j
                ks = slice(k0, k0 + 4)
                self.select(Vs[:, ks, 0:64], [(va[:, :, 0:64], self.oh_g[0]), (va[:, :, 64:128], self.oh_g[1])], [vak], ["Vs"])
                self.select(Vw[:, ks, 0:64], [(va[:, :, 128:192], self.oh_g[0]), (va[:, :, 192:256], self.oh_g[1])], [vak], ["Vw"])
            fga, fgk = fgR.next()
            self.ld(fga[:], G_fg[s * 512:(s + 1) * 512, :].rearrange("(a d) m -> d a m", d=64), [fgk], fgk)
            fgv = fga[:].rearrange("p (kd g f) m -> p kd g f m", kd=2, g=2)
            ms = slice(s * (T // 16), (s + 1) * (T // 16))
            for i in range(4):
                kd, fg = i // 2, i % 2
                self.select(FG[i][:, ms], [(fgv[:, kd, 0, fg, :], self.oh_g[0]), (fgv[:, kd, 1, fg, :], self.oh_g[1])], [fgk], ["FG%d" % i])
        self.memset("dve", Vs[:, :, 64:65], 1.0, ["Vs"])
        self.memset("dve", Vw[:, :, 64:65], 1.0, ["Vw"])
        self.memset("dve", Vc[:, :, 64:65], 1.0, ["Vc"])
        self.memset("dve", KcA[:], 0.0, ["KcA"])
        self.memset("dve", VcT[:], 0.0, ["VcT"])
        self.tt("dve", KcA[:, 0:NM - 1], FG[0][:, 0:NM - 1], FG[1][:, 1:NM], ALU.add, ["FG0", "FG1"], ["KcA"])
        self.tt("dve", VcT[:, 0:NM - 1], FG[2][:, 0:NM - 1], FG[3][:, 1:NM], ALU.add, ["FG2", "FG3"], ["VcT"])
        pO1 = P.ps("pO1", [128, 512])
        pO2 = P.ps("pO2", [128, 512])
        pI = P.ps("pI", [128, 1024])
        pTb = P.ps("pTb", [128, 4, 128], BF16)
        pSf = pS[:].rearrange("p a b -> p (a b)")
        for nt in range(NMT):
            self.tp(pO1[:, 0:64], VcT[:, nt * 128:(nt + 1) * 128], identf[0:64, 0:64], ["VcT", "cf"], ["pO1"])
            self.cp("act", Vc[:, nt, 0:64], pO1[:, 0:64], ["pO1"], ["Vc"])
        q4R = Ring(P, "q4", 2, [64, 4, 128], BF16)
        qMR = Ring(P, "qM", 2, [128, NG4, 2, 128], BF16)
        g6R = Ring(P, "g6", 3, [32, 128], F32)
        qaR = Ring(P, "qa", 2, [64, 8, 128], BF16)
        qg = P.sb("qg", [64, 4, 128], BF16)
        pTR = Ring(P, "pT", 4, [128, 512], BF16)
        for t_ in g6R.t:
            self.memset("dve", t_[:], 0.0, [g6R.k[g6R.t.index(t_)]])
        rsm = P.sb("rsm", [128, 4], F32)
        imp = P.sb("imp", [128, NSEL], F32)
        score = P.sb("score", [128, NSEL], F32)
        sc2 = P.sb("sc2", [128, NSEL], F32)
        m8 = P.sb("m8", [128, 8], F32)
        thr = P.sb("thr", [128, 1], F32)
        mpad = P.sb("mpad", [128, 64 + 64 * NG4], BF16)
        self.memset("dve", mpad[:], 0.0, ["mpad"])
        osbR = Ring(P, "osb", 2, [65, 3, 256], F32)
        rr = P.sb("rr", [65, 3, 256], F32)
        srow = P.sb("srow", [65, 3, 256], F32)
        ya = P.sb("ya", [64, 256], F32)
        yb2 = P.sb("yb2", [64, 256], F32)
        ybR = Ring(P, "yb", 2, [64, 256], BF16)
        ocmp = pO1[0:65, 0:256]
        owin = pO1[0:65, 256:512]
        oslc = pO2[0:65, 0:256]
        rsp = pO2[:, 256:260]
        impU = pI[:].rearrange("p (h j) -> p h j", h=4)

        nslot = 0
        pending = []
        pro = {}

        def prologue(qb_):
            s_ = qb_ // QPS
            tl_ = (qb_ % QPS) * 128
            q4_, q4k_ = q4R.next()
            qM_, qMk_ = qMR.next()
            g6_, g6k_ = g6R.next()
            qa, qak = qaR.next()
            P.dma([(lambda e, j=j, qa=qa: e.dma_start(out=qa[:, 2 * j:2 * j + 2, :],
                                                      in_=self.gf(s_, j)[:, tl_:tl_ + 128].rearrange("(h d) t -> d h t", d=64))) for j in range(4)],
                  (), [qak], key=qak)
            self.ld(g6_[0:24, :], G_gn[s_ * 32:s_ * 32 + 24, tl_:tl_ + 128], [g6k_], g6k_)
            self.select(qg[:], [(qa[:, 0:4, :], self.oh_g[0][0:64, :]), (qa[:, 4:8, :], self.oh_g[1][0:64, :])], [qak], ["qg"])
            self.select(q4_[:, 0:2, :], [(qg[:, 0:2, :], self.oh_hp[0][0:64, :]), (qg[:, 2:4, :], self.oh_hp[1][0:64, :])], ["qg"], [q4k_])
            self.select(q4_[:, 2:4, :], [(qg[:, 2:4, :], self.oh_hp[0][0:64, :]), (qg[:, 0:2, :], self.oh_hp[1][0:64, :])], ["qg"], [q4k_])
            for v in range(NG4):
                self.cp("pool", qM_[0:64, v, :, :], q4_[:, 0:2, :], [q4k_], [qMk_])
            pro[qb_] = (s_, tl_, q4_, q4k_, qM_, qMk_, g6_, g6k_)

        prologue(0)
        for qb in range(NKT):
            s, tl, q4, q4k, qM, qMk, g6, g6k = pro.pop(qb)
            q4f = q4[:].rearrange("p h t -> p (h t)")
            jobs = []
            ntmax = min((8 * qb + 6) // 128, NMT - 1)

            def mk_cmp(nt):
                off = 8 * qb - 128 * nt
                partial = off <= 128

                def qk(sps, sk):
                    self.mm(sps[:, 0:512], KcA[:, nt * 128:(nt + 1) * 128], q4f, True, not partial, ["KcA", q4k], [sk])
                    if partial:
                        idx = off // 8
                        for half in range(2):
                            self.mm(sps[:, half * 256:(half + 1) * 256], identb[:, :], cmpmask[:, idx, :, :].rearrange("p a b -> p (a b)"),
                                    False, half == 1, ["cb"], [sk])

                def pv(pT, pk):
                    self.mm(ocmp, Vc[:, nt, :], pT[:, 0:256], nt == 0, nt == ntmax, ["Vc", pk], ["pO1"])
                    for h in range(4):
                        self.mm(impU[:, h, 0:NSEL], pT[:, h * 128:(h + 1) * 128], ov[:, nt, :], nt == 0, nt == ntmax, [pk, "cb"], ["pI"])
                        self.mm(rsp[:, h:h + 1], pT[:, h * 128:(h + 1) * 128], onesb[:, 0:1], nt == 0, nt == ntmax, [pk, "cb"], ["pO2"])
                return dict(n=512, qk=qk, pv=pv)

            def selection():
                self.ts("dve", rsm[:], rsp, 1e-30, None, ALU.max, None, ["pO2"], ["rsm"])
                self.P.op("dve", lambda e: e.reciprocal(out=rsm[:], in_=rsm[:]), ["rsm"], ["rsm"])
                self.ts("dve", imp[:], impU[:, 0, 0:NSEL], rsm[:, 0:1], None, ALU.mult, None, ["pI", "rsm"], ["imp"])
                for h in range(1, 4):
                    self.stt(imp[:], impU[:, h, 0:NSEL], rsm[:, h:h + 1], imp[:], ALU.mult, ALU.add, ["pI", "rsm", "imp"], ["imp"])
                self.tt("dve", score[:], imp[:], at[:, NSEL - 2 * qb:2 * NSEL - 2 * qb], ALU.add, ["imp", "cf"], ["score"])
                self.memset("dve", score[:, 0:1], BIG, ["score"])
                self.P.op("dve", lambda e: e.max(out=m8[:], in_=score[:]), ["score"], ["m8"])
                self.P.op("dve", lambda e: e.match_replace(out=sc2[:], in_to_replace=m8[:], in_values=score[:], imm_value=-2 * BIG), ["score", "m8"], ["sc2"])
                self.P.op("dve", lambda e: e.max(out=m8[:], in_=sc2[:]), ["sc2"], ["m8"])
                self.ts("dve", thr[:], m8[:, 7:8], -0.5 * BIG, None, ALU.max, None, ["m8"], ["thr"])
                self.ts("dve", mpad[:, 64:64 + NSEL], score[:], thr[:, 0:1], NEG, ALU.is_lt, ALU.mult, ["score", "thr"], ["mpad"])

            def augrows():
                for v in range(NG4):
                    self.tp(pTb[:, v, :], mpad[:, 64 * v:64 * v + 128], identb[:, :], ["mpad", "cb"], ["pTb"])
                    for hh in range(2):
                        self.cp("dve", qM[64:128, v, hh, :], pTb[64:128, v, :], ["pTb"], [qMk + "a"])

            for nt in range(ntmax + 1):
                jobs.append(mk_cmp(nt))
            jobs[-1]["after"] = selection
            kts = list(range(max(0, qb - 4), qb + 1))

            def mk_win(kt):
                def qk(sps, sk):
                    nmask = (kt == qb) or (kt == qb - 4)
                    self.mm(sps[:, 0:256], KwT[:, kt * 128:(kt + 1) * 128], qM[0:64, 0, :, :].rearrange("p h t -> p (h t)"), True, not nmask, ["KwT", qMk], [sk])
                    if kt == qb:
                        self.mm(sps[:, 0:256], identb[:, :], causal2.rearrange("p a b -> p (a b)"), False, True, ["cb"], [sk])
                    elif kt == qb - 4:
                        self.mm(sps[:, 0:256], identb[:, :], antic2.rearrange("p a b -> p (a b)"), False, True, ["cb"], [sk])

                def pv(pT, pk):
                    self.mm(owin, Vw[:, kt, :], pT[:, 0:256], kt == kts[0], kt == kts[-1], ["Vw", pk], ["pO1w"])
                return dict(n=256, qk=qk, pv=pv)

            for kt in kts:
                jobs.append(mk_win(kt))

            def mk_slc(pair):
                def qk(sps, sk):
                    for i, kt in enumerate(pair):
                        v = kt // 32
                        o_ = sps[:, i * 256:(i + 1) * 256]
                        self.mm(o_, KsA[:, kt * 128:(kt + 1) * 128], qM[:, v, :, :].rearrange("p h t -> p (h t)"), True, kt != qb, ["KsA", qMk, qMk + "a"], [sk])
                        if kt == qb:
                            self.mm(o_, identb[:, :], causal2.rearrange("p a b -> p (a b)"), False, True, ["cb"], [sk])

                def pv(pT, pk):
                    for i, kt in enumerate(pair):
                        self.mm(oslc, Vs[:, kt, :], pT[:, i * 256:(i + 1) * 256], kt == 0, kt == qb, ["Vs", pk], ["pO2s"])
                return dict(n=256 * len(pair), qk=qk, pv=pv)

            first_slc = len(jobs)
            for i0 in range(0, qb + 1, 2):
                jobs.append(mk_slc(list(range(i0, min(i0 + 2, qb + 1)))))
            jobs[first_slc]["before"] = augrows
            if qb + 1 < NKT:
                jobs[first_slc].setdefault("afters", []).append(lambda nq=qb + 1: prologue(nq))
            if pending:
                for si, st_ in enumerate(pending):
                    ji = first_slc + 1 + 2 * si
                    if ji < len(jobs):
                        jobs[ji].setdefault("afters", []).append(st_)
                    else:
                        jobs[-1].setdefault("afters", []).append(st_)
                pending = []
            LA = 2
            nj = len(jobs)
            slots = []
            for j in range(nj):
                slots.append(nslot % 3)
                nslot += 1

            def emit_qk(j):
                if "before" in jobs[j]:
                    jobs[j]["before"]()
                jobs[j]["qk"](pS[:, slots[j], :], "pS%d" % slots[j])

            n_cmp_jobs = ntmax + 1
            for j in range(min(LA, nj, first_slc)):
                emit_qk(j)
            pre = min(LA, nj, first_slc)
            for j in range(nj):
                pT, pk = pTR.next()
                n_ = jobs[j]["n"]
                self.act(pT[:, 0:n_], pS[:, slots[j], 0:n_], AF.Exp, ["pS%d" % slots[j]], [pk])
                jobs[j]["pv"](pT, pk)
                if "after" in jobs[j]:
                    jobs[j]["after"]()
                for st_ in jobs[j].get("afters", []):
                    st_()
                nxt = j + LA
                if nxt < nj and nxt >= pre:
                    emit_qk(nxt)
                elif nxt >= nj and False:
                    pass
            osb, osk = osbR.next()
            self.cp("act", osb[:, 0, :], ocmp, ["pO1"], [osk])
            self.cp("act", osb[:, 1, :], oslc, ["pO2s"], [osk])
            self.cp("act", osb[:, 2, :], owin, ["pO1w"], [osk])
            pending = self.nsa_epilogue_stages(osb, osk, g6, g6k, s, tl, rr, srow, ya, yb2, ybR, pI, onesf)
        for st_ in pending:
            st_()


    def nsa_epilogue_stages(self, osb, osk, g6, g6k, s, tl, rr, srow, ya, yb2, ybR, pI, onesf):
        def st1():
            self.ts("dve", rr[64:65, :, :], osb[64:65, :, :], 1e-30, None, ALU.max, None, [osk], ["rr"])
            self.P.op("dve", lambda e: e.reciprocal(out=rr[64:65, :, :], in_=rr[64:65, :, :]), ["rr"], ["rr"])

        def st2():
            for br in range(3):
                for hh in range(2):
                    c0 = br * 256 + hh * 128
                    self.mm(pI[0:65, c0:c0 + 128], self.pc[0:32, 2061 + (br * 2 + hh) * 65:2061 + (br * 2 + hh + 1) * 65], g6[:, :], True, True, [g6k, "pc"], ["pI"])

        def st3():
            self.tt("dve", srow[64:65, :, :].rearrange("p a b -> p (a b)"), rr[64:65, :, :].rearrange("p a b -> p (a b)"), pI[64:65, 0:768],
                    ALU.mult, ["rr", "pI"], ["srow"])

        def st4():
            for br in range(3):
                self.mm(pI[0:64, br * 256:(br + 1) * 256], onesf[64:65, 0:64], srow[64:65, br, :], True, True, ["srow", "cf"], ["pI"])

        def st5():
            self.tt("dve", ya[:], osb[0:64, 0, :], pI[0:64, 0:256], ALU.mult, [osk, "pI"], ["ya"])
            self.tt("dve", yb2[:], osb[0:64, 1, :], pI[0:64, 256:512], ALU.mult, [osk, "pI"], ["yb2"])
            self.tt("dve", ya[:], ya[:], yb2[:], ALU.add, ["ya", "yb2"], ["ya"])
            self.tt("dve", yb2[:], osb[0:64, 2, :], pI[0:64, 512:768], ALU.mult, [osk, "pI"], ["yb2"])
            yb, ybk = ybR.next()
            self.tt("dve", yb[:], ya[:], yb2[:], ALU.add, ["ya", "yb2"], [ybk])
            self.stq(self.Yc[0][s][:, tl:tl + 128].rearrange("(h d) t -> d h t", d=64), yb[:].rearrange("p (h t) -> p h t", h=2), [ybk], ybk + "s")
        return [st1, st2, st3, st4, st5]

    def phase_GLA(self, l):
        P, S, T = self.P, self.S, self.T
        NKT = S // 128
        self.load_vec(l)
        G_ge = self.G_ge
        qsT = P.sb("qsT", [64, S], BF16)
        ksT = P.sb("ksT", [64, S], BF16)
        kt_ = P.sb("ktg", [128, NKT, 64], BF16)
        v_ = P.sb("vg", [128, NKT, 128], BF16)
        eb = P.sb("eb", [64, S // 64], F32)
        self.load_pc()
        selA = self.pcb[:, 64:128]
        selB = self.pcb[:, 128:192]
        gqR = Ring(P, "gq", 2, [128, 2, 2, 512], BF16)
        gtR = Ring(P, "gt", 2, [128, 4, 768], BF16)
        eba = P.sb("eba", [64, 16, T // 64], F32)
        paR = Ring(P, "pa", 2, [128, 512], F32, psum=True)
        nsel = 0
        for s in range(4):
            for kc in range(T // 512):
                c0 = s * T + kc * 512
                gq, gqk = gqR.next()
                P.dma([(lambda e, j=j, gq=gq, s=s, kc=kc: e.dma_start(out=gq[:, j // 2, j % 2, :], in_=self.gf(s, 6 + j)[:, kc * 512:(kc + 1) * 512])) for j in range(4)],
                      (), [gqk], key=gqk)
                for qk_, (dst, dk) in enumerate(((qsT, "qsT"), (ksT, "ksT"))):
                    pss_, pssk = paR.next()
                    self.mm(pss_[0:64, :], selA, gq[:, qk_, 0, :], True, False, [gqk, "pcb"], [pssk])
                    self.mm(pss_[0:64, :], selB, gq[:, qk_, 1, :], False, True, [gqk, "pcb"], [pssk])
                    self.cp("act" if (nsel % 2 == 0) else "dve", dst[0:64, c0:c0 + 512], pss_[0:64, :], [pssk], [dk])
                    nsel += 1
            for j in range(T // 512):
                gt, gtk = gtR.next()
                self.ld(gt[:], self.GTc[j][s * 512:(s + 1) * 512, 256:1024].rearrange("(kt p) c -> p kt c", p=128), [gtk], gtk)
                k0 = (s * T + j * 512) // 128
                ks = slice(k0, k0 + 4)
                self.select(kt_[:, ks, :], [(gt[:, :, h * 64:(h + 1) * 64], self.oh_r[h]) for h in range(4)], [gtk], ["ktg"])
                self.select(v_[:, ks, :], [(gt[:, :, 256 + h * 128:256 + (h + 1) * 128], self.oh_r[h]) for h in range(4)], [gtk], ["vg"])
        self.ld(eba[:], G_ge.ap().rearrange("(a d) c -> d a c", d=64), ["eba"], "eba")
        ebv = eba[:].rearrange("p (s h) c -> p s h c", s=4)
        self.select(eb[:].rearrange("p (s c) -> p s c", s=4), [(ebv[:, :, h, :], self.oh_r[h][0:64, :]) for h in range(4)], ["eba"], ["eb"])
        tri = self.cfv("tri")
        onesf = self.cfv("ones")
        gn = self.vv("gnorm")
        St = P.sb("St", [128, 128], F32)
        SbR = Ring(P, "Sb", 3, [128, 128], BF16)
        AtR = Ring(P, "At", 2, [128, 128], BF16)
        osq = P.sb("osq", [128, 512], F32)
        rs = P.sb("rsg", [128, 512], F32)
        obR = Ring(P, "og", 2, [128, 512], BF16)
        poR = Ring(P, "po", 2, [128, 512], F32, psum=True)
        pkR = Ring(P, "pk", 2, [128, 512], F32, psum=True)
        pn = P.ps("pn", [128, 512])
        self.memset("dve", St[:], 0.0, ["St"])
        Sb, Sbk = SbR.next()
        self.memset("dve", Sb[:], 0.0, [Sbk])
        for tg in range(S // 512):
            po, pok = poR.next()
            for tt_ in range(4):
                ti = tg * 4 + tt_
                c0 = ti * 128
                pa, pak = paR.next()
                self.mm(pa[:, 0:128], ksT[:, c0:c0 + 128], qsT[:, c0:c0 + 128], True, True, ["ksT", "qsT"], [pak])
                At, Atk = AtR.next()
                self.tt("dve", At[:], pa[:, 0:128], tri[:, :], ALU.mult, [pak, "cf"], [Atk])
                for c in range(2):
                    pb = 64 * c
                    cc = slice(c0 + pb, c0 + pb + 64)
                    oc = po[:, tt_ * 128 + pb:tt_ * 128 + pb + 64]
                    self.mm(oc, v_[pb:pb + 64, ti, :], At[pb:pb + 64, pb:pb + 64], True, False, ["vg", Atk], [pok])
                    self.mm(oc, Sb[0:64, :], qsT[:, cc], False, True, [Sbk, "qsT"], [pok])
                    pk_, pkk = pkR.next()
                    self.mm(pk_[0:64, 0:128], kt_[pb:pb + 64, ti, :], v_[pb:pb + 64, ti, :], True, True, ["ktg", "vg"], [pkk])
                    ci = 2 * ti + c
                    self.stt(St[0:64, :], St[0:64, :], eb[:, ci:ci + 1], pk_[0:64, 0:128], ALU.mult, ALU.add, ["St", "eb", pkk], ["St"])
                    Sb, Sbk = SbR.next()
                    self.cp("act", Sb[0:64, :], St[0:64, :], ["St"], [Sbk])
            self.act(osq[:], po[:, :], AF.Square, [pok], ["osq"])
            self.mm(pn[:, :], onesf[:, :], osq[:], True, True, ["osq", "cf"], ["pn"])
            self.act(rs[:], pn[:, :], AF.Ln, ["pn"], ["rsg"], scale=1.0 / 128, bias=self.epsb[:, 0:1])
            self.act(rs[:], rs[:], AF.Exp, ["rsg"], ["rsg"], scale=-0.5)
            ob, obk = obR.next()
            self.stt(ob[:], po[:, :], gn[:, 0:1], rs[:], ALU.mult, ALU.mult, [pok, "vec", "rsg"], [obk])
            t0 = tg * 512
            seg = t0 // T
            lc = t0 % T
            self.stq(self.Yc[1][seg][:, lc:lc + 512], ob[:], [obk], obk + "s")

    def phase_C1(self, l):
        P, S, T = self.P, self.S, self.T
        NG = T // 512
        self.load_vec(l)
        Wb = P.sb("Wb", [128, 3, 4, D], BF16)
        for br in range(3):
            self.ld(Wb[:, br, :, :], self.w_branch[l, br].rearrange("(wc p) d -> p wc d", p=128), ["Wb%d" % br], "Wb%d" % br, queue="pool")
        Wo = P.sb("Wo", [128, 8, D], BF16)
        self.ld(Wo[:], self.w_out[l].rearrange("(k p) d -> p k d", p=128), ["Wo"], "Wo", queue="pool")
        Wp = P.sb("Wpool", [128, 4, 128], BF16)
        self.ld(Wp[:], self.pool_w[l].rearrange("g c d -> c g d"), ["Wpool"], "Wpool", queue="pool")
        self.load_pc()
        pc = self.pc
        ic0 = pc[:, 1:1 + 2048].rearrange("p (g t) -> p g t", g=4)
        uh = P.sb("uh", [128, 4, 4, 16], F32)
        ylR = Ring(P, "yl", 2, [128, 4, 4, 512], BF16)
        hsrc = (self.xT if l == 0 else self.hT).ap().rearrange("(k p) t -> p k t", p=128)
        hdst = self.hT.ap().rearrange("(k p) t -> p k t", p=128)
        uview = self.uT.ap().rearrange("(g p) t -> p g t", p=128)
        hR = Ring(P, "hC", 1, [128, 8, 512], F32)
        uR = Ring(P, "uC", 2, [128, 4, 528], F32)
        ybR = Ring(P, "ybC", 2, [128, 4, 512], BF16)
        ycR = Ring(P, "ycC", 2, [128, 4, 512], BF16)
        rR = Ring(P, "rC", 2, [128, 4, 512], BF16)
        gmR = Ring(P, "gmC", 1, [128, 24, 512], BF16)
        sA = P.sb("sA", [128, 528], F32)
        sB = P.sb("sB", [128, 528], F32)
        pl = P.sb("pl", [128, 4, 512], BF16)
        yaT = P.sb("yaT", [128, 4, 512], BF16)
        ycg = P.sb("ycg", [128, 4, 512], BF16)
        m1 = P.sb("m1", [128, 512], F32)
        m2 = P.sb("m2", [128, 512], F32)
        mb = P.sb("mb", [128, 8, 512], BF16)
        ppR = Ring(P, "ppC", 6, [128, 512], F32, psum=True)
        G_u = self.G_u
        wins = (2, 4, 8, 16)
        for tg in range(NG):
            t0 = tg * 512
            sl = slice(t0, t0 + 512)
            h, hk = hR.next()
            self.ld(h[:], hsrc[:, :, sl], [hk], hk)
            u, uk = uR.next()
            self.ld(u[:, :, 16:528], uview[:, :, sl], [uk], uk + "a")
            if tg == 0:
                self.ld(uh[:], G_u.ap().rearrange("(s g p) t -> p s g t", s=4, g=4), ["uh"], "uh")
                self.select(u[:, :, 0:16], [(uh[:, s_, :, :], self.oh_prev[s_]) for s_ in range(4)], ["uh"], [uk])
            else:
                self.ld(u[:, :, 0:16], uview[:, :, t0 - 16:t0], [uk], uk + "b")
            yb, ybk = ybR.next()
            yc, yck = ycR.next()
            for bc, (yt_, ytk) in enumerate(((yb, ybk), (yc, yck))):
                yl, ylk = ylR.next()
                P.dma([(lambda e, sg=sg, yl=yl, bc=bc, sl=sl: e.dma_start(out=yl[:, sg, :, :], in_=self.GYc[bc][sg][:, sl].rearrange("(r p) t -> p r t", p=128))) for sg in range(4)],
                      (), [ylk], key=ylk)
                for r_ in range(4):
                    self.select(yt_[:, r_, :], [(yl[:, sg, r_, :], self.oh_r[sg]) for sg in range(4)], [ylk], [ytk + "_%d" % r_])
            ybks = [ybk + "_%d" % r_ for r_ in range(4)]
            ycks = [yck + "_%d" % r_ for r_ in range(4)]
            rt, rk = rR.next()
            self.ld(rt[:], self.rT.ap().rearrange("(g p) t -> p g t", p=128)[:, :, sl], [rk], rk)
            gm, gmk = gmR.next()
            self.ld(gm[:], self.gmT.ap().rearrange("(g p) t -> p g t", p=128)[:, :, sl], [gmk], gmk)
            for g in range(4):
                cur, curk = u[:, g, :], uk
                bufs = [(sA, "sA"), (sB, "sB")]
                step = 1
                bi = 0
                while step < wins[g]:
                    nb, nbk = bufs[bi % 2]
                    bi += 1
                    self.tt("dve", nb[:, step:528], cur[:, step:528], cur[:, 0:528 - step], ALU.add, [curk], [nbk])
                    cur, curk = nb[:], nbk
                    step *= 2
                if tg == 0:
                    self.tt("dve", m1[:], cur[:, 16:528], ic0[:, g, :], ALU.mult, [curk, "pc"], ["m1"])
                else:
                    self.ts("dve", m1[:], cur[:, 16:528], 1.0 / wins[g], None, ALU.mult, None, [curk], ["m1"])
                self.tt("dve", pl[:, g, :], m1[:], u[:, g, 16:528], ALU.subtract, ["m1", uk], ["pl"])
            for g in range(4):
                ps, pk = ppR.next()
                self.mm(ps[:, :], Wp[:, g, :], pl[:, g, :], True, True, ["Wpool", "pl"], [pk])
                self.ts("dve", yaT[:, g, :], ps[:, :], self.vv("pscale")[:, g:g + 1], None, ALU.mult, None, [pk, "vec"], ["yaT"])
            self.tt("dve", ycg[:], yc[:], rt[:], ALU.mult, ycks + [rk], ["ycg"])
            ys = [(yaT, ["yaT"]), (yb, ybks), (ycg, ["ycg"])]
            for dc in range(8):
                dsl = slice(dc * 128, (dc + 1) * 128)
                pss = []
                for br in range(3):
                    ps, pk = ppR.next()
                    for wc in range(4):
                        self.mm(ps[:, :], Wb[:, br, wc, dsl], ys[br][0][:, wc, :], wc == 0, wc == 3, ["Wb%d" % br] + ys[br][1], [pk])
                    pss.append((ps, pk))
                self.tt("dve", m1[:], pss[0][0][:, :], gm[:, 0 * 8 + dc, :], ALU.mult, [pss[0][1], gmk], ["m1"])
                self.tt("dve", m2[:], pss[1][0][:, :], gm[:, 1 * 8 + dc, :], ALU.mult, [pss[1][1], gmk], ["m2"])
                self.tt("dve", m1[:], m1[:], m2[:], ALU.add, ["m1", "m2"], ["m1"])
                self.tt("dve", m2[:], pss[2][0][:, :], gm[:, 2 * 8 + dc, :], ALU.mult, [pss[2][1], gmk], ["m2"])
                self.tt("dve", mb[:, dc, :], m1[:], m2[:], ALU.add, ["m1", "m2"], ["mb"])
            for dc in range(8):
                dsl = slice(dc * 128, (dc + 1) * 128)
                ps, pk = ppR.next()
                for k in range(8):
                    self.mm(ps[:, :], Wo[:, k, dsl], mb[:, k, :], k == 0, k == 7, ["Wo", "mb"], [pk])
                self.tt("dve", h[:, dc, :], h[:, dc, :], ps[:, :], ALU.add, [hk, pk], [hk])
            self.stq(hdst[:, :, sl], h[:], [hk], hk + "s")

    def phase_C2(self, l):
        P, T = self.P, self.T
        NG = T // 512
        self.load_vec(l)
        W1 = P.sb("W1", [128, 8, 4 * D], BF16)
        W2 = P.sb("W2", [128, 32, D], BF16)
        P.dma([(lambda e, k=k: e.dma_start(out=W1[:, k, :], in_=self.w_ff1[l, k * 128:(k + 1) * 128, :])) for k in range(8)],
              (), ["W1"], key="W1", queue="pool")
        P.dma([(lambda e, k=k: e.dma_start(out=W2[:, k * 8:(k + 1) * 8, :], in_=self.w_ff2[l, k * 1024:(k + 1) * 1024, :].rearrange("(c p) d -> p c d", p=128))) for k in range(4)],
              (), ["W2"], key="W2", queue="pool")
        hv = self.hT.ap().rearrange("(k p) t -> p k t", p=128)
        hR = Ring(P, "hF", 1, [128, 8, 512], F32)
        sqR = Ring(P, "sqF", 2, [128, 512], F32)
        rs = P.sb("rsF", [128, 512], F32)
        fT = P.sb("fT", [128, 8, 512], BF16)
        uT = P.sb("uF", [128, 32, 512], BF16)
        rl = Ring(P, "rl", 2, [128, 512], F32)
        ssps = P.ps("sspsF", [128, 512])
        ppR = Ring(P, "ppF", 4, [128, 512], F32, psum=True)
        for tg in range(NG):
            sl = slice(tg * 512, (tg + 1) * 512)
            h, hk = hR.next()
            self.ld(h[:], hv[:, :, sl], [hk], hk)
            self.rmsnorm(h, hk, "gffn", fT, "fT", sqR, ssps, rs)
            for fc in range(32):
                ps, pk = ppR.next()
                for k in range(8):
                    self.mm(ps[:, :], W1[:, k, fc * 128:(fc + 1) * 128], fT[:, k, :], k == 0, k == 7, ["W1", "fT"], [pk])
                r_, rk = rl.next()
                self.act(r_[:], ps[:, :], AF.Relu, [pk], [rk])
                self.tt("dve" if fc % 2 == 0 else "pool", uT[:, fc, :], r_[:], r_[:], ALU.mult, [rk], ["uF"])
            for dc in range(8):
                ps, pk = ppR.next()
                for fc in range(32):
                    self.mm(ps[:, :], W2[:, fc, dc * 128:(dc + 1) * 128], uT[:, fc, :], fc == 0, fc == 31, ["W2", "uF"], [pk])
                self.tt("dve", h[:, dc, :], h[:, dc, :], ps[:, :], ALU.add, [hk, pk], [hk])
            self.stq(hv[:, :, sl], h[:], [hk], hk + "s")

    def phase_C3(self, l):
        P, T = self.P, self.T
        NG = T // 512
        last = (l == self.depth - 1)
        self.load_vec(l)
        Wg = P.sb("Wg", [128, 8, D], BF16)
        self.ld(Wg[:], self.w_ple_gate[l].rearrange("(k p) d -> p k d", p=128), ["Wg"], "Wg", queue="pool")
        Wq = P.sb("Wq", [128, 2, D], BF16)
        self.ld(Wq[:], self.w_ple_proj[l].rearrange("(k p) d -> p k d", p=128), ["Wq"], "Wq", queue="pool")
        hv = self.hT.ap().rearrange("(k p) t -> p k t", p=128)
        ov = self.out.ap().rearrange("(k p) t -> p k t", p=128)
        pv = self.pT.ap()
        hR = Ring(P, "hP", 2, [128, 8, 512], F32)
        pR = Ring(P, "pP", 2, [128, 2, 512], BF16)
        sqR = Ring(P, "sqP", 2, [128, 512], F32)
        rs = P.sb("rsP", [128, 512], F32)
        nT = P.sb("nT", [128, 8, 512], BF16)
        gt = P.sb("gt", [128, 512], F32)
        m1 = P.sb("m1P", [128, 512], F32)
        oR = Ring(P, "oP", 2, [128, 8, 512], F32)
        ssps = P.ps("sspsP", [128, 512])
        ppR = Ring(P, "ppP", 4, [128, 512], F32, psum=True)
        for tg in range(NG):
            sl = slice(tg * 512, (tg + 1) * 512)
            h, hk = hR.next()
            self.ld(h[:], hv[:, :, sl], [hk], hk)
            pt, ptk = pR.next()
            self.ld(pt[:], pv[l].rearrange("(k p) t -> p k t", p=128)[:, :, sl], [ptk], ptk, queue="pool")
            self.rmsnorm(h, hk, "gple", nT, "nT", sqR, ssps, rs)
            for dc in range(8):
                dsl = slice(dc * 128, (dc + 1) * 128)
                ps, pk = ppR.next()
                for k in range(8):
                    self.mm(ps[:, :], Wg[:, k, dsl], nT[:, k, :], k == 0, k == 7, ["Wg", "nT"], [pk])
                self.act(gt[:], ps[:, :], AF.Sigmoid, [pk], ["gt"])
                ps2, pk2 = ppR.next()
                for k in range(2):
                    self.mm(ps2[:, :], Wq[:, k, dsl], pt[:, k, :], k == 0, k == 1, ["Wq", ptk], [pk2])
                self.tt("dve", m1[:], gt[:], ps2[:, :], ALU.mult, ["gt", pk2], ["m1P"])
                self.tt("dve", h[:, dc, :], h[:, dc, :], m1[:], ALU.add, [hk, "m1P"], [hk])
            if not last:
                self.stq(hv[:, :, sl], h[:], [hk], hk + "s")
            else:
                o, okk = oR.next()
                self.rmsnorm(h, hk, "gfin", None, None, sqR, ssps, rs)
                g = self.vv("gfin")
                for k in range(8):
                    self.stt(o[:, k, :], h[:, k, :], g[:, k:k + 1], rs[:], ALU.mult, ALU.mult, [hk, "rsP" if False else "rs", "vec"], [okk])
                self.stq(ov[:, :, sl], o[:], [okk], okk + "s")


def vec_pack_offsets():
    dummy = {
        "norm_mix": np.zeros((DEPTH, D), np.float32), "norm_ffn": np.zeros((DEPTH, D), np.float32),
        "norm_ple": np.zeros((DEPTH, D), np.float32), "norm_final": np.zeros((D,), np.float32),
        "pool_scale": np.zeros((DEPTH, 512), np.float32), "gla_norm": np.zeros((DEPTH, 128), np.float32),
        "cmp_pos_k": np.zeros((DEPTH, 32, 64), np.float32), "cmp_pos_v": np.zeros((DEPTH, 32, 64), np.float32),
    }
    return vec_pack(dummy, 0)


def make_in_maps(inp, S, depth=DEPTH):
    T = S // 4
    f = lambda a: np.ascontiguousarray(np.asarray(a, np.float32))
    x = np.asarray(inp["x"], np.float32)
    p = np.asarray(inp["p"], np.float32)
    pos = np.asarray(inp["positions"], np.int32)
    cf = f32_consts(S).array()
    cb = bf_consts(S).array()
    epat = epat_const(S)
    vecs = np.stack([vec_pack(inp, l).array() for l in range(depth)], 0)
    wga = np.zeros((depth, 32, 256), np.float32)
    wga[:, 0:16] = np.asarray(inp["gla_w_gate"], np.float32)[:depth]
    wga[:, 16] = np.asarray(inp["gla_b_gate"], np.float32)[:depth]
    shared = {
        "w_in": f(inp["w_in"][:depth]), "pool_w": f(inp["pool_w"][:depth]),
        "cmp_w_k": f(inp["cmp_w_k"][:depth]), "cmp_w_v": f(inp["cmp_w_v"][:depth]), "wga": wga,
        "w_branch": f(inp["w_branch"][:depth]), "w_out": f(inp["w_out"][:depth]),
        "w_ff1": f(inp["w_ff1"][:depth]), "w_ff2": f(inp["w_ff2"][:depth]),
        "w_ple_gate": f(inp["w_ple_gate"][:depth]), "w_ple_proj": f(inp["w_ple_proj"][:depth]),
        "vecs": f(vecs), "cf": cf, "cb": cb, "epat": epat,
    }
    maps = []
    wins = (2, 4, 8, 16)
    for c in range(8):
        b, seg = c // 4, c % 4
        sl = slice(seg * T, (seg + 1) * T)
        g, hp = seg // 2, seg % 2
        pc = np.zeros((128, NPC), np.float32)
        tglob = seg * T + np.arange(512)
        for gi in range(4):
            pc[:, 1 + gi * 512:1 + (gi + 1) * 512] = (1.0 / np.minimum(tglob + 1, wins[gi]))[None, :]
        pc[:, 2049 + seg] = 1.0
        pc[:, 2053 + g] = 1.0
        pc[:, 2055 + hp] = 1.0
        if seg > 0:
            pc[:, 2057 + seg - 1] = 1.0
        selg = np.zeros((32, 6, 65), np.float32)
        for br in range(3):
            for hh in range(2):
                selg[(4 * g + 2 * hp + hh) * 3 + br, br * 2 + hh, 64] = 1.0
        pc[0:32, 2061:2061 + 390] = selg.reshape(32, 390)
        pcb = np.zeros((128, 192), np.float32)
        dd = np.arange(64)
        pcb[g * 64 + dd, dd] = 1.0
        if seg < 2:
            pcb[seg * 64 + dd, 64 + dd] = 1.0
        else:
            pcb[(seg - 2) * 64 + dd, 128 + dd] = 1.0
        m = dict(shared)
        m["xT"] = np.ascontiguousarray(x[b, sl, :].T)
        m["pT"] = np.ascontiguousarray(np.transpose(p[:depth, b, sl, :], (0, 2, 1)))
        m["pos"] = np.ascontiguousarray(pos[b, sl][None, :])
        m["pc"] = pc
        m["pcb"] = pcb
        maps.append(m)
    return maps


_CACHE = {}


def run(inp, S, depth=DEPTH, debug=False, stop_after=None, trace=False):
    key = (S, depth, debug, stop_after)
    if key not in _CACHE:
        _CACHE[key] = Builder(S, depth, debug, stop_after)
    bld = _CACHE[key]
    maps = make_in_maps(inp, S, depth)
    res = run_bass_kernel_spmd(bld.nc, maps, core_ids=list(range(8)), trace=trace) if trace else \
        run_bass_kernel_spmd(bld.nc, maps, core_ids=list(range(8)))
    return res


def kernel(**inputs):
    S = int(np.asarray(inputs["x"]).shape[1])
    T = S // 4
    res = run(inputs, S)
    out = np.zeros((2, S, D), np.float32)
    for c in range(8):
        b, seg = c // 4, c % 4
        out[b, seg * T:(seg + 1) * T, :] = np.asarray(res.results[c]["outT"]).T
    return out
```
